# Optimizing a Trainium2 kernel written in Bass

```python
import jax
import jax.numpy as jnp
from jax import lax
import numpy as np

D_MODEL = 1024
BATCH = 2
SEQ = 16384
DEPTH = 2

HEAD_DIM = 64
ATTN_BLOCK = 128
DIL_PATTERNS = ((128, 1), (512, 4), (2048, 16))
N_DIL = 3
DIL_HEADS = 6
CONV_WIDTH = 384
CONV_K = 3
NSA_Q_HEADS = 6
NSA_KV_HEADS = 2
NSA_GROUP = NSA_Q_HEADS // NSA_KV_HEADS
CMP_BLOCK = 32
CMP_STRIDE = 16
CMP_HIDDEN = 128
SEL_BLOCK = 64
N_SEL = 16
NSA_WINDOW = 512
N_NSA_BRANCH = 3
SWA_Q_HEADS = 6
SWA_KV_HEADS = 2
SWA_GROUP = SWA_Q_HEADS // SWA_KV_HEADS
SWA_WINDOW = 128
BRANCH_WIDTH = 384
N_BRANCH = 4
D_FF = 2816
N_EXPERTS = 8
TOP_K = 2
D_FF_EXPERT = 3584
PLE_DIM = 256
LN_EPS = 1e-5
ALPHA = (2 * DEPTH) ** 0.25
BETA = (8 * DEPTH) ** -0.25
NEG_INF = -1e30
DIL_WIDTH = N_DIL * DIL_HEADS * HEAD_DIM
NSA_KV_WIDTH = NSA_KV_HEADS * HEAD_DIM
SWA_KV_WIDTH = SWA_KV_HEADS * HEAD_DIM
COLUMN_SIZES = (DIL_WIDTH, DIL_WIDTH, DIL_WIDTH,
                CONV_WIDTH, CONV_WIDTH, CONV_WIDTH,
                NSA_Q_HEADS * HEAD_DIM,
                NSA_KV_WIDTH, NSA_KV_WIDTH, NSA_KV_WIDTH, NSA_KV_WIDTH, NSA_KV_WIDTH, NSA_KV_WIDTH,
                N_NSA_BRANCH * NSA_Q_HEADS,
                SWA_Q_HEADS * HEAD_DIM, SWA_KV_WIDTH, SWA_KV_WIDTH)
N_IN = sum(COLUMN_SIZES)

kernel_name = 'hybrid_dilated_conv_nsa_swa_moe_deepnorm'


def layer_norm(x, g, b):
    xf = x.astype(jnp.float32)
    mu = jnp.mean(xf, axis=-1, keepdims=True)
    var = jnp.mean(jnp.square(xf - mu), axis=-1, keepdims=True)
    return ((xf - mu) * lax.rsqrt(var + LN_EPS) * g + b).astype(x.dtype)


def banded_attention(q, k, v, window):
    b, L, kv, g, hd = q.shape
    nb = -(-L // ATTN_BLOCK)
    lp = nb * ATTN_BLOCK
    n_prev = -(-window // ATTN_BLOCK)
    span = (n_prev + 1) * ATTN_BLOCK
    qb = jnp.pad(q, ((0, 0), (0, lp - L), (0, 0), (0, 0), (0, 0))).reshape(b, nb, ATTN_BLOCK, kv, g, hd)

    def key_windows(t):
        t = jnp.pad(t, ((0, 0), (n_prev * ATTN_BLOCK, lp - L), (0, 0), (0, 0)))
        t = t.reshape(b, nb + n_prev, ATTN_BLOCK, kv, hd)
        return jnp.concatenate([t[:, s:s + nb] for s in range(n_prev + 1)], axis=2)

    kw = key_windows(k)
    vw = key_windows(v)
    s = jnp.einsum('bnqkgd,bnjkd->bnkgqj', qb, kw, preferred_element_type=jnp.float32) * (hd ** -0.5)
    qi = jnp.arange(ATTN_BLOCK)[:, None]
    kj = jnp.arange(span)[None, :]
    dist = n_prev * ATTN_BLOCK + qi - kj
    kpos = (jnp.arange(nb)[:, None, None] - n_prev) * ATTN_BLOCK + kj[None]
    mask = (dist >= 0) & (dist <= window) & (kpos >= 0)
    s = jnp.where(mask[None, :, None, None], s, NEG_INF)
    m = jnp.max(s, axis=-1, keepdims=True)
    e = jnp.exp(s - m)
    l = jnp.sum(e, axis=-1, keepdims=True)
    o = jnp.einsum('bnkgqj,bnjkd->bnqkgd', e / l, vw.astype(jnp.float32))
    lse = (m + jnp.log(l))[..., 0].transpose(0, 1, 4, 2, 3)
    return o.reshape(b, lp, kv, g, hd)[:, :L], lse.reshape(b, lp, kv, g)[:, :L]


def dilated_attention(q, k, v):
    b, S = q.shape[:2]
    outs = []
    lses = []
    for g, (window, dil) in enumerate(DIL_PATTERNS):
        def by_stride(t):
            t = t[:, :, g].reshape(b, S // dil, dil, DIL_HEADS, HEAD_DIM).transpose(0, 2, 1, 3, 4)
            return t.reshape(b * dil, S // dil, DIL_HEADS, HEAD_DIM)
        o, lse = banded_attention(by_stride(q)[:, :, :, None], by_stride(k), by_stride(v), window // dil)
        o = o.reshape(b, dil, S // dil, DIL_HEADS, HEAD_DIM).transpose(0, 2, 1, 3, 4)
        lse = lse.reshape(b, dil, S // dil, DIL_HEADS).transpose(0, 2, 1, 3)
        outs.append(o.reshape(b, S, DIL_HEADS, HEAD_DIM))
        lses.append(lse.reshape(b, S, DIL_HEADS))
    wts = jax.nn.softmax(jnp.stack(lses), axis=0)
    return jnp.sum(wts[..., None] * jnp.stack(outs), axis=0)


def gated_short_conv(gate_b, gate_c, h, conv_w):
    u = gate_c * h
    y = lax.conv_general_dilated(u, conv_w[:, None, :].astype(u.dtype), window_strides=(1,),
                                 padding=((CONV_K - 1, 0),), dimension_numbers=('NWC', 'WIO', 'NWC'),
                                 feature_group_count=u.shape[-1])
    return gate_b * y


def compress_blocks(t, pos, w1, b1, w2, b2):
    b, S, kv, hd = t.shape
    r = CMP_BLOCK // CMP_STRIDE
    nc = S // CMP_STRIDE - r + 1
    chunks = t.reshape(b, S // CMP_STRIDE, CMP_STRIDE, kv, hd)
    blocks = jnp.concatenate([chunks[:, i:i + nc] for i in range(r)], axis=2) + pos[None, None, :, None, :]
    flat = blocks.transpose(0, 1, 3, 2, 4).reshape(b, nc, kv, CMP_BLOCK * hd)
    return jax.nn.gelu(flat @ w1 + b1) @ w2 + b2


def native_sparse_attention(q, k_cmp, v_cmp, k_slc, v_slc, k_win, v_win, gates,
                            cmp_pos, cmp_w1, cmp_b1, cmp_w2, cmp_b2):
    b, S = q.shape[:2]
    kc = compress_blocks(k_cmp, cmp_pos[0], cmp_w1[0], cmp_b1[0], cmp_w2[0], cmp_b2[0])
    vc = compress_blocks(v_cmp, cmp_pos[1], cmp_w1[1], cmp_b1[1], cmp_w2[1], cmp_b2[1])
    nc = kc.shape[1]
    ns = S // SEL_BLOCK
    n_sel = min(N_SEL, ns)
    ksb = k_slc.reshape(b, ns, SEL_BLOCK, NSA_KV_HEADS, HEAD_DIM).transpose(0, 3, 1, 2, 4)
    vsb = v_slc.reshape(b, ns, SEL_BLOCK, NSA_KV_HEADS, HEAD_DIM).transpose(0, 3, 1, 2, 4)
    c_start = jnp.arange(nc) * CMP_STRIDE
    c_end = c_start + CMP_BLOCK - 1
    s_start = jnp.arange(ns) * SEL_BLOCK
    overlap = ((c_start[:, None] < s_start[None, :] + SEL_BLOCK)
               & (c_end[:, None] >= s_start[None, :])).astype(jnp.float32)
    sel_ids = jnp.arange(ns)
    scale = HEAD_DIM ** -0.5
    gather = jax.vmap(jax.vmap(lambda blocks, ix: blocks[ix]))

    def query_block(args):
        qb, i = args
        t = i * ATTN_BLOCK + jnp.arange(ATTN_BLOCK)
        s = jnp.einsum('bqkgd,bckd->bkgqc', qb, kc, preferred_element_type=jnp.float32) * scale
        vis = c_end[None, :] <= t[:, None]
        s = jnp.where(vis, s, NEG_INF)
        e = jnp.exp(s - jnp.max(s, axis=-1, keepdims=True)) * vis
        p_cmp = e / jnp.maximum(jnp.sum(e, axis=-1, keepdims=True), 1e-30)
        o_cmp = jnp.einsum('bkgqc,bckd->bqkgd', p_cmp, vc.astype(jnp.float32))
        imp = jnp.einsum('bkgqc,cj->bkqj', p_cmp, overlap)
        cur = t // SEL_BLOCK
        causal = sel_ids[None, :] <= cur[:, None]
        forced = (sel_ids[None, :] == 0) | (sel_ids[None, :] == cur[:, None]) | (sel_ids[None, :] == cur[:, None] - 1)
        imp = jnp.where(forced, jnp.inf, jnp.where(causal, imp, -jnp.inf))
        top, idx = lax.top_k(imp, n_sel)
        valid = top > -jnp.inf
        kg = gather(ksb, idx)
        vg = gather(vsb, idx)
        s2 = jnp.einsum('bqkgd,bkqnjd->bkgqnj', qb, kg, preferred_element_type=jnp.float32) * scale
        kpos = idx[..., None] * SEL_BLOCK + jnp.arange(SEL_BLOCK)
        m2 = valid[..., None] & (kpos <= t[None, None, :, None, None])
        s2 = jnp.where(m2[:, :, None], s2, NEG_INF)
        sh = s2.shape
        p2 = jax.nn.softmax(s2.reshape(sh[:4] + (n_sel * SEL_BLOCK,)), axis=-1).reshape(sh)
        o_slc = jnp.einsum('bkgqnj,bkqnjd->bqkgd', p2, vg.astype(jnp.float32))
        return o_cmp, o_slc

    nq = S // ATTN_BLOCK
    qblk = q.reshape(b, nq, ATTN_BLOCK, NSA_KV_HEADS, NSA_GROUP, HEAD_DIM).transpose(1, 0, 2, 3, 4, 5)
    o_cmp, o_slc = lax.map(query_block, (qblk, jnp.arange(nq)))

    def unblock(o):
        return o.transpose(1, 0, 2, 3, 4, 5).reshape(b, S, NSA_KV_HEADS, NSA_GROUP, HEAD_DIM)

    o_win, _ = banded_attention(q, k_win, v_win, NSA_WINDOW - 1)
    return (gates[:, :, 0][..., None] * unblock(o_cmp)
            + gates[:, :, 1][..., None] * unblock(o_slc)
            + gates[:, :, 2][..., None] * o_win)


def token_mixers(x, w_in, conv_w, cmp_pos, cmp_w1, cmp_b1, cmp_w2, cmp_b2, sinks):
    b, S, _ = x.shape
    z = x @ w_in
    (qa, ka, va, gb, gc, hb, qc, kcc, vcc, ksc, vsc, kwc, vwc, gn, qd, kd, vd) = jnp.split(
        z, np.cumsum(COLUMN_SIZES)[:-1].tolist(), axis=-1)

    def dil(t):
        return t.reshape(b, S, N_DIL, DIL_HEADS, HEAD_DIM)

    def nkv(t):
        return t.reshape(b, S, NSA_KV_HEADS, HEAD_DIM)

    def skv(t):
        return t.reshape(b, S, SWA_KV_HEADS, HEAD_DIM)

    o_a = dilated_attention(dil(qa), dil(ka), dil(va)).reshape(b, S, BRANCH_WIDTH)
    o_b = gated_short_conv(gb, gc, hb, conv_w)
    nsa_gates = jax.nn.sigmoid(gn.astype(jnp.float32)).reshape(b, S, N_NSA_BRANCH, NSA_KV_HEADS, NSA_GROUP)
    o_c = native_sparse_attention(qc.reshape(b, S, NSA_KV_HEADS, NSA_GROUP, HEAD_DIM),
                                  nkv(kcc), nkv(vcc), nkv(ksc), nkv(vsc), nkv(kwc), nkv(vwc), nsa_gates,
                                  cmp_pos, cmp_w1, cmp_b1, cmp_w2, cmp_b2).reshape(b, S, BRANCH_WIDTH)
    o_d, lse_d = banded_attention(qd.reshape(b, S, SWA_KV_HEADS, SWA_GROUP, HEAD_DIM), skv(kd), skv(vd),
                                  SWA_WINDOW - 1)
    sink_keep = jax.nn.sigmoid(lse_d - sinks.reshape(SWA_KV_HEADS, SWA_GROUP).astype(jnp.float32))
    o_d = (o_d * sink_keep[..., None]).reshape(b, S, BRANCH_WIDTH)
    return [o_a.astype(x.dtype), o_b.astype(x.dtype), o_c.astype(x.dtype), o_d.astype(x.dtype)]


def swiglu(h, w_gate, w_up, w_down):
    return (jax.nn.silu(h @ w_gate) * (h @ w_up)) @ w_down


def moe_swiglu(h, w_router, b_router, w_gate, w_up, w_down):
    logits = (h @ w_router + b_router).astype(jnp.float32)
    top_v, top_i = lax.top_k(logits, TOP_K)
    top_w = jax.nn.softmax(top_v, axis=-1)
    comb = jnp.sum(jax.nn.one_hot(top_i, N_EXPERTS, dtype=jnp.float32) * top_w[..., None], axis=-2)
    comb = comb.astype(h.dtype)
    out = jnp.zeros_like(h)
    for e in range(N_EXPERTS):
        out = out + comb[..., e:e + 1] * swiglu(h, w_gate[e], w_up[e], w_down[e])
    return out


def setup_inputs(seed: int = 0) -> dict:
    key = jax.random.key(seed)
    ks = jax.random.split(key, 32)
    d = D_MODEL
    n_dense = (DEPTH + 1) // 2
    n_moe = DEPTH // 2

    def nrm(k, shape, scale):
        return jax.random.normal(k, shape, jnp.float32) * scale

    return {
        'x': nrm(ks[0], (BATCH, SEQ, d), 1.0),
        'p': nrm(ks[1], (DEPTH, BATCH, SEQ, PLE_DIM), 1.0),
        'w_in': nrm(ks[2], (DEPTH, d, N_IN), d ** -0.5),
        'conv_w': nrm(ks[3], (DEPTH, CONV_K, CONV_WIDTH), CONV_K ** -0.5),
        'cmp_pos': nrm(ks[4], (DEPTH, 2, CMP_BLOCK, HEAD_DIM), 0.1),
        'cmp_w1': nrm(ks[5], (DEPTH, 2, CMP_BLOCK * HEAD_DIM, CMP_HIDDEN), (CMP_BLOCK * HEAD_DIM) ** -0.5),
        'cmp_b1': nrm(ks[6], (DEPTH, 2, CMP_HIDDEN), 0.01),
        'cmp_w2': nrm(ks[7], (DEPTH, 2, CMP_HIDDEN, HEAD_DIM), CMP_HIDDEN ** -0.5),
        'cmp_b2': nrm(ks[8], (DEPTH, 2, HEAD_DIM), 0.01),
        'sinks': nrm(ks[9], (DEPTH, SWA_Q_HEADS), 1.0),
        'w_branch': nrm(ks[10], (DEPTH, N_BRANCH, BRANCH_WIDTH, d), BRANCH_WIDTH ** -0.5 * BETA),
        'w_merge_gate': nrm(ks[11], (DEPTH, N_BRANCH, d, d), d ** -0.5),
        'b_merge_gate': nrm(ks[12], (DEPTH, N_BRANCH, d), 0.01),
        'w_out': nrm(ks[13], (DEPTH, d, d), d ** -0.5 * BETA),
        'ln_mix_g': 1.0 + nrm(ks[14], (DEPTH, d), 0.01),
        'ln_mix_b': nrm(ks[15], (DEPTH, d), 0.01),
        'ffn_w_gate': nrm(ks[16], (n_dense, d, D_FF), d ** -0.5),
        'ffn_w_up': nrm(ks[17], (n_dense, d, D_FF), d ** -0.5 * BETA),
        'ffn_w_down': nrm(ks[18], (n_dense, D_FF, d), D_FF ** -0.5 * BETA),
        'w_router': nrm(ks[19], (n_moe, d, N_EXPERTS), d ** -0.5),
        'b_router': nrm(ks[20], (n_moe, N_EXPERTS), 0.01),
        'moe_w_gate': nrm(ks[21], (n_moe, N_EXPERTS, d, D_FF_EXPERT), d ** -0.5),
        'moe_w_up': nrm(ks[22], (n_moe, N_EXPERTS, d, D_FF_EXPERT), d ** -0.5 * BETA),
        'moe_w_down': nrm(ks[23], (n_moe, N_EXPERTS, D_FF_EXPERT, d), D_FF_EXPERT ** -0.5 * BETA),
        'ple_w': nrm(ks[24], (DEPTH, PLE_DIM, d), PLE_DIM ** -0.5),
        'ple_gate_w': nrm(ks[25], (DEPTH, d, d), d ** -0.5),
        'ple_gate_b': nrm(ks[26], (DEPTH, d), 0.01),
        'ln_ffn_g': 1.0 + nrm(ks[27], (DEPTH, d), 0.01),
        'ln_ffn_b': nrm(ks[28], (DEPTH, d), 0.01),
    }


def reference(x, p, w_in, conv_w, cmp_pos, cmp_w1, cmp_b1, cmp_w2, cmp_b2, sinks, w_branch,
              w_merge_gate, b_merge_gate, w_out, ln_mix_g, ln_mix_b, ffn_w_gate, ffn_w_up, ffn_w_down,
              w_router, b_router, moe_w_gate, moe_w_up, moe_w_down, ple_w, ple_gate_w, ple_gate_b,
              ln_ffn_g, ln_ffn_b):
    for i in range(DEPTH):
        branches = token_mixers(x, w_in[i], conv_w[i], cmp_pos[i], cmp_w1[i], cmp_b1[i], cmp_w2[i],
                                cmp_b2[i], sinks[i])
        merged = jnp.zeros_like(x)
        for m in range(N_BRANCH):
            gate = jax.nn.sigmoid(x @ w_merge_gate[i, m] + b_merge_gate[i, m])
            merged = merged + gate * (branches[m] @ w_branch[i, m])
        x = layer_norm(ALPHA * x + merged @ w_out[i], ln_mix_g[i], ln_mix_b[i])
        j = i // 2
        if i % 2 == 0:
            f = swiglu(x, ffn_w_gate[j], ffn_w_up[j], ffn_w_down[j])
        else:
            f = moe_swiglu(x, w_router[j], b_router[j], moe_w_gate[j], moe_w_up[j], moe_w_down[j])
        ple = jax.nn.sigmoid(x @ ple_gate_w[i] + ple_gate_b[i]) * (p[i] @ ple_w[i])
        x = layer_norm(ALPHA * x + f + ple, ln_ffn_g[i], ln_ffn_b[i])
    return x
```

```python
import numpy as np
import ml_dtypes
import concourse.bass as bass
import concourse.mybir as mybir
from concourse.bass_utils import run_bass_kernel_spmd

F32 = mybir.dt.float32
BF16 = mybir.dt.bfloat16
I32 = mybir.dt.int32
AF = mybir.ActivationFunctionType
ALU = mybir.AluOpType
AX = mybir.AxisListType
_DSZ = {F32: 4, BF16: 2, I32: 4}

D_MODEL = 1024
HD = 64
ALPHA = 4 ** 0.25
LN_EPS = 1e-5
D_FF = 2816
D_FFE = 3584
NEXP = 8
NEG = -30000.0


class Sched:
    def __init__(self, nc, n_dma_sems=24):
        self.nc = nc
        self.e = dict(pe=nc.tensor, act=nc.scalar, dve=nc.vector, pool=nc.gpsimd, sp=nc.sync)
        self.sem = {k: nc.alloc_semaphore(name=f"sem_{k}") for k in ('pe', 'act', 'dve', 'pool')}
        self.cnt = {k: 0 for k in self.sem}
        self.dsem = [nc.alloc_semaphore(name=f"dsem{i}") for i in range(n_dma_sems)]
        self.dcnt = [0] * n_dma_sems
        self.dnext = 0
        self.waited = {k: {} for k in self.e}
        self.acc = {}
        self.nins = 0

    @staticmethod
    def box(ap):
        dims = ap.ap
        name = ap.tensor.name
        sz = _DSZ.get(ap.dtype, 4)
        if str(ap.space) == 'DRAM':
            lo = ap.offset
            hi = lo + sum((c - 1) * abs(s) for s, c in dims) + 1
            return name, (0, 1, lo * sz, hi * sz)
        pstep, pcount = dims[0]
        sp = ap.start_partition()
        flo = ap.offset - sp * pstep
        fhi = flo + sum((c - 1) * abs(s) for s, c in dims[1:]) + 1
        return name, (sp, sp + pcount, flo * sz, fhi * sz)

    @staticmethod
    def _ov(a, b):
        return a[0] < b[1] and b[0] < a[1] and a[2] < b[3] and b[2] < a[3]

    @staticmethod
    def _contains(a, b):
        return a[0] <= b[0] and b[1] <= a[1] and a[2] <= b[2] and b[3] <= a[3]

    def _deps(self, boxes):
        toks = set()
        for name, bx, isw in boxes:
            for (rbx, risw, rtok) in self.acc.get(name, ()):
                if (isw or risw) and self._ov(bx, rbx):
                    toks.add(rtok)
        return toks

    def _record(self, boxes, tok):
        for name, bx, isw in boxes:
            lst = self.acc.setdefault(name, [])
            if isw:
                lst[:] = [r for r in lst if not self._contains(bx, r[0])]
            else:
                lst[:] = [r for r in lst if not (not r[1] and r[2][0] == tok[0] and self._contains(bx, r[0]))]
            lst.append((bx, isw, tok))

    def _wait(self, eng, tok):
        key, val = tok
        if key == eng and eng == 'pe':
            return
        if self.waited[eng].get(key, 0) >= val:
            return
        self.waited[eng][key] = val
        semobj = self.sem[key] if isinstance(key, str) else self.dsem[key[1]]
        self.e[eng].wait_ge(semobj, val)
        self.nins += 1

    def _boxes(self, reads, writes):
        out = []
        for r in reads:
            if r is None or isinstance(r, (int, float)):
                continue
            n, b = self.box(r)
            out.append((n, b, False))
        for w in writes:
            n, b = self.box(w)
            out.append((n, b, True))
        return out

    def op(self, eng, fn, *args, reads=(), writes=(), **kw):
        boxes = self._boxes(reads, writes)
        for t in sorted(self._deps(boxes), key=lambda t: (str(t[0]), t[1])):
            self._wait(eng, t)
        ins = fn(*args, **kw)
        self.cnt[eng] += 1
        self.nins += 1
        ins.then_inc(self.sem[eng], 1)
        self._record(boxes, (eng, self.cnt[eng]))
        return ins

    def mm(self, out, lhsT, rhs, start=True, stop=True):
        return self.op('pe', self.nc.tensor.matmul, out, lhsT, rhs, start=start, stop=stop,
                       skip_group_check=True, reads=[lhsT, rhs], writes=[out])

    def transpose(self, out, in_, ident):
        return self.op('pe', self.nc.tensor.transpose, out, in_, ident, reads=[in_, ident], writes=[out])

    def act(self, out, in_, func, bias=None, scale=None, accum_out=None):
        kw = {}
        rd = [in_]
        wr = [out]
        if bias is not None:
            kw['bias'] = bias
            rd.append(bias)
        if scale is not None:
            kw['scale'] = scale
            rd.append(scale)
        if accum_out is not None:
            kw['accum_out'] = accum_out
            wr.append(accum_out)
        return self.op('act', self.nc.scalar.activation, out, in_, func, reads=rd, writes=wr, **kw)

    def tt(self, eng, out, in0, in1, op):
        return self.op(eng, self.e[eng].tensor_tensor, out, in0, in1, op, reads=[in0, in1], writes=[out])

    def ts(self, eng, out, in0, s1, s2, op0, op1=None, accum_out=None):
        kw = {}
        wr = [out]
        if op1 is not None:
            kw['op1'] = op1
        if accum_out is not None:
            kw['accum_out'] = accum_out
            wr.append(accum_out)
        return self.op(eng, self.e[eng].tensor_scalar, out, in0, s1, s2, op0, reads=[in0, s1, s2], writes=wr, **kw)

    def stt(self, eng, out, in0, scalar, in1, op0, op1):
        return self.op(eng, self.e[eng].scalar_tensor_tensor, out, in0, scalar, in1, op0, op1,
                       reads=[in0, scalar, in1], writes=[out])

    def copy(self, eng, out, in_):
        if eng == 'act':
            return self.op('act', self.nc.scalar.copy, out, in_, reads=[in_], writes=[out])
        return self.op(eng, self.e[eng].tensor_copy, out, in_, reads=[in_], writes=[out])

    def memset(self, eng, ap, val):
        return self.op(eng, self.e[eng].memset, ap, val, reads=[], writes=[ap])

    def dma(self, q, out, in_):
        boxes = self._boxes([in_], [out])
        toks = self._deps(boxes)
        i = self.dnext
        self.dnext = (self.dnext + 1) % len(self.dsem)
        if self.dcnt[i] > 0:
            toks.add((('d', i), 16 * self.dcnt[i]))
        for t in sorted(toks, key=lambda t: (str(t[0]), t[1])):
            self._wait(q, t)
        ins = self.e[q].dma_start(out=out, in_=in_)
        self.dcnt[i] += 1
        self.nins += 1
        ins.then_inc(self.dsem[i], 16)
        self._record(boxes, (('d', i), 16 * self.dcnt[i]))

    def finish(self):
        for i, c in enumerate(self.dcnt):
            if c:
                self._wait('sp', (('d', i), 16 * c))
        for k, c in self.cnt.items():
            if c:
                self._wait('sp', (k, c))


class PsumPool:
    def __init__(self, nc):
        self.t = nc.alloc_psum_tensor("psum_all", [128, 8 * 512], F32)
        self.rr = {}

    def bank(self, role, banks):
        i = self.rr.get(role, 0)
        self.rr[role] = i + 1
        b = banks[i % len(banks)]
        return self.t[:, b * 512:(b + 1) * 512]

    def wide(self, role, pairs):
        i = self.rr.get(role, 0)
        self.rr[role] = i + 1
        b = pairs[i % len(pairs)]
        return self.t[:, b * 512:(b + 2) * 512]


class Prog:
    def __init__(self, NM, moe, mixers=(0, 1, 2, 3)):
        self.NM = NM
        self.T = NM * 512
        self.SB = NM * 2048
        self.moe = moe
        self.mixers = mixers
        self.nc = bass.Bass("TRN2", target_bir_lowering=False)
        self.S = Sched(self.nc)
        self.ps = PsumPool(self.nc)
        self.din = {}
        self._n = 0

    def inp(self, name, shape, dt=F32):
        t = self.nc.dram_tensor(name, list(shape), dt, kind="ExternalInput")
        self.din[name] = (tuple(shape), dt)
        return t.ap()

    def outp(self, name, shape, dt=F32):
        return self.nc.dram_tensor(name, list(shape), dt, kind="ExternalOutput").ap()

    def scratch(self, name, shape, dt):
        return self.nc.dram_tensor(name, list(shape), dt, kind="Internal").ap()

    def sb(self, name, shape, dt):
        return self.nc.alloc_sbuf_tensor(name, list(shape), dt)

    def layer_norm(self, s, g_bc, b_bc, out_ap, tmp):
        S, nc = self.S, self.nc
        st = tmp['st']
        mv = tmp['mv']
        for j in range(2):
            S.op('dve', nc.vector.bn_stats, out=st[:, j, :], in_=s[:, j * 512:(j + 1) * 512],
                 reads=[s[:, j * 512:(j + 1) * 512]], writes=[st[:, j, :]])
        S.op('dve', nc.vector.bn_aggr, out=mv[:, 0:2], in_=st[:, :, :], reads=[st[:, :, :]], writes=[mv[:, 0:2]])
        S.ts('pool', mv[:, 2:3], mv[:, 1:2], LN_EPS, None, ALU.add)
        S.tt('pool', mv[:, 3:4], mv[:, 2:3], self.neghalf[:, 0:1], ALU.pow)
        S.ts('dve', s[:, :], s[:, :], mv[:, 0:1], mv[:, 3:4], ALU.subtract, ALU.mult)
        S.tt('pool', s[:, :], s[:, :], g_bc[:, :], ALU.mult)
        S.tt('pool', out_ap, s[:, :], b_bc[:, :], ALU.add)

    def setup_common(self):
        S, nc = self.S, self.nc
        self.ident = self.sb("ident", [128, 128], F32)
        S.dma('sp', self.ident[:, :], self.inp("c_ident", [128, 128]))
        self.neghalf = self.sb("neghalf", [128, 1], F32)
        S.memset('pool', self.neghalf[:, :], -0.5)
        self.ones_bf = self.sb("ones_bf", [128, 512], BF16)
        S.memset('pool', self.ones_bf[:, :], 1.0)
        self.lnp_d = self.inp("ln_params", [128, 4, 1024])
        self.lntmp = dict(st=self.sb("ln_st", [128, 2, 6], F32), mv=self.sb("ln_mv", [128, 4], F32))

    def mix_tail(self, m, mergedT, wout, xmid_d, xmT_d, x_own):
        S, nc = self.S, self.nc
        for tt in range(4):
            tok = m * 512 + tt * 128
            xt = self.tl_x[tt % len(self.tl_x)]
            S.dma('sp', xt[:, :], x_own[tok:tok + 128, :])
            s = self.tl_s[tt % len(self.tl_s)]
            if mergedT is not None:
                yps = self.ps.wide('y', [0, 2])
                for half in range(2):
                    for k in range(8):
                        S.mm(yps[:, half * 512:(half + 1) * 512], mergedT[:, k, tt * 128:(tt + 1) * 128],
                             wout[:, k, half * 512:(half + 1) * 512], start=(k == 0), stop=(k == 7))
                S.stt('dve', s[:, :], xt[:, :], ALPHA, yps, ALU.mult, ALU.add)
            else:
                S.ts('dve', s[:, :], xt[:, :], ALPHA, None, ALU.mult)
            xm = self.tl_xm[tt % len(self.tl_xm)]
            self.layer_norm(s, self.lnp[:, 0, :], self.lnp[:, 1, :], xm[:, :], self.lntmp)
            S.dma('sp', xmid_d[tok:tok + 128, :], xm[:, :])
            xmt = self.tl_xmt[tt % len(self.tl_xmt)]
            for half in range(2):
                tp = self.ps.bank('tp', [4, 5])
                for j in range(4):
                    fc = half * 4 + j
                    S.mm(tp[:, j * 128:(j + 1) * 128], xm[:, fc * 128:(fc + 1) * 128], self.ident[:, :])
                S.copy('act', xmt[:, half * 4:(half + 1) * 4, :],
                       tp.rearrange("p (j t) -> p j t", j=4))
            S.dma('sp', xmT_d[:, :, tok:tok + 128], xmt[:, :, :])

    def stage_f(self, xmid_d, xmT_d, y_out):
        S, nc = self.S, self.nc
        T, NT = self.T, self.T // 128
        moe = self.moe
        NE = NEXP if moe else 1
        FF = D_FFE if moe else D_FF
        GW = 256
        NG = FF // GW
        if moe:
            wg_d = self.inp("moe_w_gate", [NEXP, 1024, FF])
            wu_d = self.inp("moe_w_up", [NEXP, 1024, FF])
            wd_d = self.inp("moe_w_down", [NEXP, FF, 1024])
            wr_d = self.inp("w_router", [1024, NEXP])
            br_d = self.inp("b_router", [1, NEXP])
        else:
            wg_d = self.inp("ffn_w_gate", [1, 1024, FF])
            wu_d = self.inp("ffn_w_up", [1, 1024, FF])
            wd_d = self.inp("ffn_w_down", [1, FF, 1024])
        plew_d = self.inp("ple_w", [256, 1024])
        plegw_d = self.inp("ple_gate_w", [1024, 1024])
        plegb_d = self.inp("ple_gate_b", [1, 1024])
        pT_d = self.inp("pT_own", [256, T])

        HT = min(16, NT)
        xmT = self.sb("f_xmT", [128, 8, HT * 128], BF16)
        acc = self.sb("f_acc", [128, HT, 1024], F32)
        comb = self.sb("f_comb", [128, NT, 8], F32)
        wgb = [self.sb(f"f_wg{i}", [128, 8, GW], BF16) for i in range(2)]
        wub = [self.sb(f"f_wu{i}", [128, 8, GW], BF16) for i in range(2)]
        wdb = [self.sb(f"f_wd{i}", [128, GW // 128, 1024], BF16) for i in range(2)]
        hT = [self.sb(f"f_hT{i}", [128, GW // 128, 512], BF16) for i in range(2)]
        sg = [self.sb(f"f_sg{i}", [128, 512], F32) for i in range(2)]
        plegw = self.sb("f_plegw", [128, 8, 1024], BF16)
        plew = self.sb("f_plew", [128, 2, 1024], BF16)
        plegb = self.sb("f_plegb", [1, 1024], BF16)
        pT = self.sb("f_pT", [128, 2, HT * 128], BF16)
        S.dma('pool', plegw[:, :, :], plegw_d.rearrange("(k p) n -> p k n", p=128))
        S.dma('pool', plew[:, :, :], plew_d.rearrange("(k p) n -> p k n", p=128))
        S.dma('pool', plegb[:, :], plegb_d)
        sig = [self.sb(f"f_sig{i}", [128, 1024], F32) for i in range(1)]

        if moe:
            wr = self.sb("f_wr", [128, 8, NEXP], BF16)
            brt = self.sb("f_br", [1, NEXP], BF16)
            S.dma('pool', wr[:, :, :], wr_d.rearrange("(k p) n -> p k n", p=128))
            S.dma('pool', brt[:, :], br_d)
            lg = self.sb("f_lg", [128, 8], F32)
            m8 = self.sb("f_m8", [128, 8], F32)
            ex = self.sb("f_ex", [128, 8], F32)
            msk = self.sb("f_msk", [128, 8], F32)
            den = self.sb("f_den", [128, 2], F32)
        else:
            S.memset('pool', comb[:, :, :], 1.0)

        def router(h0):
            for tl in range(HT):
                t = h0 + tl
                lp = self.ps.bank('rt', [6, 7])
                for k in range(8):
                    S.mm(lp[:, 0:NEXP], xmT[:, k, tl * 128:(tl + 1) * 128], wr[:, k, :], start=(k == 0), stop=False)
                S.mm(lp[:, 0:NEXP], self.ones_bf[0:1, 0:128], brt[0:1, :], start=False, stop=True)
                S.copy('dve', lg[:, :], lp[:, 0:NEXP])
                S.op('dve', nc.vector.max, out=m8[:, :], in_=lg[:, :], reads=[lg[:, :]], writes=[m8[:, :]])
                S.ts('dve', msk[:, :], lg[:, :], m8[:, 1:2], None, ALU.is_ge)
                S.ts('dve', den[:, 0:1], m8[:, 0:1], -1.0, None, ALU.mult)
                S.act(ex[:, :], lg[:, :], AF.Exp, bias=den[:, 0:1])
                S.tt('dve', ex[:, :], ex[:, :], msk[:, :], ALU.mult)
                S.op('dve', nc.vector.tensor_reduce, out=den[:, 1:2], in_=ex[:, :], axis=AX.X, op=ALU.add,
                     reads=[ex[:, :]], writes=[den[:, 1:2]])
                S.op('dve', nc.vector.reciprocal, out=den[:, 1:2], in_=den[:, 1:2], reads=[den[:, 1:2]], writes=[den[:, 1:2]])
                S.ts('dve', comb[:, t, :], ex[:, :], den[:, 1:2], None, ALU.mult)

        gi = 0
        for h0 in range(0, NT, HT):
            S.dma('sp', xmT[:, :, :], xmT_d[:, :, h0 * 128:(h0 + HT) * 128])
            S.dma('pool', pT[:, :, :], pT_d[:, h0 * 128:(h0 + HT) * 128].rearrange("(k p) n -> p k n", p=128))
            if moe:
                router(h0)
            for tl in range(HT):
                t = h0 + tl
                gps = self.ps.wide('y', [0, 2])
                pps = self.ps.wide('pp', [4, 6])
                for half in range(2):
                    cs = slice(half * 512, (half + 1) * 512)
                    for k in range(8):
                        S.mm(gps[:, cs], xmT[:, k, tl * 128:(tl + 1) * 128], plegw[:, k, cs], start=(k == 0), stop=False)
                    S.mm(gps[:, cs], self.ones_bf[0:1, 0:128], plegb[0:1, cs], start=False, stop=True)
                    for k in range(2):
                        S.mm(pps[:, cs], pT[:, k, tl * 128:(tl + 1) * 128], plew[:, k, cs], start=(k == 0), stop=(k == 1))
                sg_ = sig[0]
                S.act(sg_[:, :], gps, AF.Sigmoid)
                S.tt('dve', acc[:, tl, :], sg_[:, :], pps, ALU.mult)
            nchunk = HT // 4
            for e in range(NE):
                for g in range(NG):
                    b = gi % 2
                    gi += 1
                    S.dma('pool', wgb[b][:, :, :], wg_d[e, :, g * GW:(g + 1) * GW].rearrange("(k p) n -> p k n", p=128))
                    S.dma('pool', wub[b][:, :, :], wu_d[e, :, g * GW:(g + 1) * GW].rearrange("(k p) n -> p k n", p=128))
                    S.dma('pool', wdb[b][:, :, :], wd_d[e, g * GW:(g + 1) * GW, :].rearrange("(k p) n -> p k n", p=128))
                    for c in range(nchunk):
                        tok0 = (c * 4) * 128
                        hb = hT[c % 2]
                        for j in range(GW // 128):
                            hg = self.ps.bank('hg', [0, 1])
                            hu = self.ps.bank('hu', [2, 3])
                            for k in range(8):
                                S.mm(hg, wgb[b][:, k, j * 128:(j + 1) * 128], xmT[:, k, tok0:tok0 + 512], start=(k == 0), stop=(k == 7))
                            for k in range(8):
                                S.mm(hu, wub[b][:, k, j * 128:(j + 1) * 128], xmT[:, k, tok0:tok0 + 512], start=(k == 0), stop=(k == 7))
                            s_ = sg[j % 2]
                            S.act(s_[:, :], hg, AF.Silu)
                            S.tt('dve', hb[:, j, :], s_[:, :], hu, ALU.mult)
                        for tl4 in range(4):
                            tl = c * 4 + tl4
                            fps = self.ps.wide('pp', [4, 6])
                            for half in range(2):
                                cs = slice(half * 512, (half + 1) * 512)
                                for j in range(GW // 128):
                                    S.mm(fps[:, cs], hb[:, j, tl4 * 128:(tl4 + 1) * 128], wdb[b][:, j, cs],
                                         start=(j == 0), stop=(j == GW // 128 - 1))
                            S.stt('dve', acc[:, tl, :], fps, comb[:, h0 + tl, e:e + 1], acc[:, tl, :], ALU.mult, ALU.add)
            for tl in range(HT):
                t = h0 + tl
                xm = self.tl_xm[tl % len(self.tl_xm)]
                S.dma('sp', xm[:, :], xmid_d[t * 128:(t + 1) * 128, :])
                s = self.tl_s[tl % len(self.tl_s)]
                S.stt('dve', s[:, :], xm[:, :], ALPHA, acc[:, tl, :], ALU.mult, ALU.add)
                o = self.tl_x[tl % len(self.tl_x)]
                self.layer_norm(s, self.lnp[:, 0, :], self.lnp[:, 1, :], o[:, :], self.lntmp)
                S.dma('sp', y_out[t * 128:(t + 1) * 128, :], o[:, :])

    def load_lnp(self, a):
        self.lnp = self.sb(f"lnp{a}", [128, 2, 1024], F32)
        self.S.dma('sp', self.lnp[:, :, :], self.lnp_d[:, a:a + 2, :])

    def alloc_tiles(self):
        self.tl_x = [self.sb(f"tl_x{i}", [128, 1024], F32) for i in range(1)]
        self.tl_s = [self.sb(f"tl_s{i}", [128, 1024], F32) for i in range(1)]
        self.tl_xm = [self.sb(f"tl_xm{i}", [128, 1024], F32) for i in range(1)]
        self.tl_xmt = [self.sb(f"tl_xmt{i}", [128, 8, 128], BF16) for i in range(2)]

    def build(self):
        S = self.S
        T = self.T
        self.stk = None
        self.setup_common()
        x_own = self.inp("x_own", [T, 1024])
        y_out = self.outp("y", [T, 1024])
        xmid_d = self.scratch("xmid_d", [T, 1024], F32)
        xmT_d = self.scratch("xmT_d", [128, 8, T], BF16)
        from contextlib import ExitStack
        if self.mixers:
            self.stage_m(x_own, xmid_d, xmT_d)
        else:
            self.stk = ExitStack()
            self.alloc_tiles()
            self.load_lnp(0)
            for m in range(self.NM):
                self.mix_tail(m, None, None, xmid_d, xmT_d, x_own)
            barrier(self)
            self.stk.close()
        self.stk = ExitStack()
        self.alloc_tiles()
        self.load_lnp(2)
        self.stage_f(xmid_d, xmT_d, y_out)
        S.finish()
        return self.nc


def own_positions(NM, qt):
    return np.concatenate([np.arange(512) + 2048 * m + 512 * qt for m in range(NM)])


def prep_core_inputs(prog, layer, xl, inp, core, consts):
    NM = prog.NM
    b, qt = core // 4, core % 4
    pos = own_positions(NM, qt)
    d = {}
    need = prog.din
    j = layer // 2
    for name in need:
        if name == "x_own":
            d[name] = np.ascontiguousarray(xl[b, pos, :])
        elif name == "pT_own":
            d[name] = np.ascontiguousarray(inp['p'][layer, b, pos, :].T)
        elif name == "ln_params":
            rows = np.stack([inp['ln_mix_g'][layer], inp['ln_mix_b'][layer], inp['ln_ffn_g'][layer], inp['ln_ffn_b'][layer]])
            d[name] = np.ascontiguousarray(np.broadcast_to(rows[None], (128, 4, 1024)))
        elif name in ("ffn_w_gate", "ffn_w_up", "ffn_w_down"):
            d[name] = np.ascontiguousarray(inp[name][j:j + 1])
        elif name in ("moe_w_gate", "moe_w_up", "moe_w_down", "w_router"):
            d[name] = np.ascontiguousarray(inp[name][j])
        elif name == "b_router":
            d[name] = np.ascontiguousarray(inp[name][j][None, :])
        elif name in ("ple_w", "ple_gate_w"):
            d[name] = np.ascontiguousarray(inp[name][layer])
        elif name == "ple_gate_b":
            d[name] = np.ascontiguousarray(inp[name][layer][None, :])
        elif name in consts:
            d[name] = consts[name]
        else:
            d[name] = prep_mixer_input(prog, name, layer, xl, inp, b, qt)
        shape, dt = need[name]
        assert tuple(d[name].shape) == tuple(shape), (name, d[name].shape, shape)
    return d


def make_consts(prog):
    c = {"c_ident": np.eye(128, dtype=np.float32)}
    c.update(make_mixer_consts(prog))
    return c


_PROGS = {}
DEBUG = False
LAST = {}


def run_layer(layer, xl, inp, NM, moe, mixers=(0, 1, 2, 3)):
    key = (NM, moe, tuple(mixers))
    if key not in _PROGS:
        p = Prog(NM, moe, mixers)
        p.debug = DEBUG
        p.build()
        _PROGS[key] = (p, make_consts(p))
    prog, consts = _PROGS[key]
    in_maps = [prep_core_inputs(prog, layer, xl, inp, c, consts) for c in range(8)]
    res = run_bass_kernel_spmd(prog.nc, in_maps, core_ids=list(range(8)))
    LAST['res'] = res
    B, S_, _ = xl.shape
    out = np.empty_like(xl)
    for c in range(8):
        b, qt = c // 4, c % 4
        out[b, own_positions(NM, qt), :] = res.results[c]["y"]
    return out


def kernel(**inputs):
    inp = {k: np.asarray(v) for k, v in inputs.items()}
    x = inp['x'].astype(np.float32, copy=False)
    B, S_, _ = x.shape
    NM = S_ // 2048
    depth = inp['w_in'].shape[0]
    for layer in range(depth):
        x = run_layer(layer, x, inp, NM, moe=(layer % 2 == 1))
    return x


U_QA, U_KA, U_QC, U_QD, U_KWC, U_KD, U_KCC, U_KSC, U_VCC, U_GN = 0, 18, 36, 42, 48, 50, 52, 54, 56, 58
C_CONV, C_VA, C_VWC, C_VD, C_VSC, NCOLS = 4864, 6016, 7168, 7296, 7424, 7552
WIN = 2560
DILS = ((128, 1), (512, 4), (2048, 16))
BIGB = 1000.0


def win_perm():
    sizes = [1152, 1152, 1152, 384, 384, 384, 384, 128, 128, 128, 128, 128, 128, 18, 384, 128, 128]
    names = ['qa', 'ka', 'va', 'gb', 'gc', 'hb', 'qc', 'kcc', 'vcc', 'ksc', 'vsc', 'kwc', 'vwc', 'gn', 'qd', 'kd', 'vd']
    off = dict(zip(names, np.cumsum([0] + sizes[:-1]).tolist()))
    r = lambda a, n: list(range(a, a + n))
    cols = []
    cols += r(off['qa'], 1152) + r(off['ka'], 1152) + r(off['qc'], 384) + r(off['qd'], 384)
    cols += r(off['kwc'], 128) + r(off['kd'], 128) + r(off['kcc'], 128) + r(off['ksc'], 128) + r(off['vcc'], 128)
    for hq in range(6):
        for br in range(3):
            cols += [off['gn'] + br * 6 + hq] * 64
    cols += r(off['gb'], 1152)
    cols += r(off['va'], 1152) + r(off['vwc'], 128) + r(off['vd'], 128) + r(off['vsc'], 128)
    assert len(cols) == NCOLS
    return np.array(cols)


def _sb(self, name, shape, dt):
    used = self.__dict__.setdefault('_names', {})
    used[name] = used.get(name, 0) + 1
    if used[name] > 1:
        name = f"{name}_r{used[name]}"
    if getattr(self, 'stk', None) is not None:
        return self.stk.enter_context(self.nc.sbuf_tensor(name, list(shape), dt))
    return self.nc.alloc_sbuf_tensor(name, list(shape), dt)


def barrier(self):
    S = self.S
    for eng in ('pe', 'act', 'dve', 'pool', 'sp'):
        for i, c in enumerate(S.dcnt):
            if c:
                S._wait(eng, (('d', i), 16 * c))
        for k, c in S.cnt.items():
            if c and k != eng:
                S._wait(eng, (k, c))
    S.acc.clear()


def get_mask(self, allowed):
    nk, n = allowed.shape
    key = (nk, n, allowed.tobytes())
    if key not in self.mask_idx:
        off = self.mask_used
        assert off + n <= self.MCAP, "mask bank full"
        self.mask_np[:nk, off:off + n] = allowed.astype(np.float32)
        self.mask_idx[key] = off
        self.mask_used += n
    off = self.mask_idx[key]
    return self.maskbank[0:nk, off:off + n]


def wplan(self, groups):
    self.wg, self.wi, self.wissued = groups, 0, 0


def wget(self, c0, n):
    i = self.wi
    assert self.wg[i] == (c0, n), (i, self.wg[i], c0, n)
    self.wi += 1
    while self.wissued < min(len(self.wg), i + 2):
        j = self.wissued
        cc, nn = self.wg[j]
        self.S.dma('pool', self.wbufs[j % 3][:, :, 0:nn], self.w_in_d[:, cc:cc + nn].rearrange("(k p) n -> p k n", p=128))
        self.wissued += 1
    return self.wbufs[i % 3]


def evac(self, dst, src):
    self._n += 1
    self.S.copy('act' if self._n % 2 else 'dve', dst, src)


def proj_fm(self, w, wcol, dst, rhs_fn, ncols, rows=64):
    S = self.S
    for c0 in range(0, ncols, 512):
        n = min(512, ncols - c0)
        ps = self.ps.bank('pj', [6, 7])
        for k in range(8):
            S.mm(ps[0:rows, 0:n], w[:, k, wcol:wcol + rows], rhs_fn(k, c0, n), start=(k == 0), stop=(k == 7))
        evac(self, dst[0:rows, c0:c0 + n], ps[0:rows, 0:n])


def band_attn(self, o_ps_fn, qT, nq, uq0, kT_fn, v_fn, uk0, nk, wd, first):
    S = self.S
    nt = (nk + 127) // 128
    for i in range(nt):
        nki = min(128, nk - 128 * i)
        k_lo = uk0 + 128 * i
        k_hi = k_lo + nki - 1
        c0 = max(k_lo, uq0) - uq0
        c1 = min(k_hi + wd, uq0 + nq - 1) - uq0 + 1
        if c1 <= c0:
            continue
        n = c1 - c0
        kk = np.arange(k_lo, k_lo + nki)[:, None]
        qq = np.arange(uq0 + c0, uq0 + c1)[None, :]
        allowed = (kk <= qq) & (kk >= qq - wd)
        s_ps = self.ps.bank('sc', [0, 1, 2])
        S.mm(s_ps[0:nki, 0:n], kT_fn(i, nki), qT[:, c0:c1])
        p = self.pbufs[self._pn % len(self.pbufs)]
        self._pn += 1
        S.act(p[0:nki, 0:n], s_ps[0:nki, 0:n], AF.Exp, scale=0.125)
        if not allowed.all():
            bad = np.where(~allowed.all(axis=0))[0]
            a, b = int(bad.min()), int(bad.max()) + 1
            mk = get_mask(self, allowed[:, a:b])
            eng = 'dve' if self._pn % 2 else 'pool'
            S.tt(eng, p[0:nki, a:b], p[0:nki, a:b], mk, ALU.mult)
        S.mm(o_ps_fn(c0, c1), v_fn(i, nki), p[0:nki, 0:n], start=first[0], stop=False)
        first[0] = False


def vsel(vt, h, nb):
    return vt[:, h, :]


def carve(ar, off, p0, p1, shape):
    n = int(np.prod(shape))
    v = ar[p0:p1, off:off + n]
    if len(shape) == 2:
        v = v.rearrange("p (a b) -> p a b", a=shape[0])
    elif len(shape) == 3:
        v = v.rearrange("p (a b c) -> p a b c", a=shape[0], b=shape[1])
    return v, off + n


def normalize(self, o_src, dst, extra=None, clamp=False):
    S, nc = self.S, self.nc
    rl = self.rlb[self._n % 2]
    self._n += 1
    if extra is not None:
        S.ts('dve', rl[:, :], o_src[64:128, :], extra, None, ALU.add)
        src = rl[:, :]
    elif clamp:
        S.ts('dve', rl[:, :], o_src[64:128, :], 1e-30, None, ALU.max)
        src = rl[:, :]
    else:
        S.ts('dve', rl[:, :], o_src[64:128, :], 0.0, None, ALU.add)
        src = rl[:, :]
    S.op('dve', nc.vector.reciprocal, out=rl[:, :], in_=src, reads=[src], writes=[rl[:, :]])
    S.tt('dve', dst, o_src[0:64, :], rl[:, :], ALU.mult)


def stage_m_setup(self):
    S, nc = self.S, self.nc
    NS, NKT = self.SB // 64, self.SB // 2048
    self.MCAP = 2048
    self.mask_np = np.zeros((128, self.MCAP), np.float32)
    self.mask_idx, self.mask_used = {}, 0
    self.maskbank = self.sb("maskbank", [128, self.MCAP], BF16)
    S.dma('pool', self.maskbank[:, :], self.inp("c_masks", [128, self.MCAP]))
    self.convw = self.sb("convw", [128, 3, 3], F32)
    S.dma('sp', self.convw[:, :, :], self.inp("conv_w_l", [128, 3, 3]))
    self.sinkexp = self.sb("sinkexp", [128, 6], F32)
    S.dma('sp', self.sinkexp[:, :], self.inp("sinks_rep", [128, 6]))
    S.act(self.sinkexp[:, :], self.sinkexp[:, :], AF.Exp)
    self.bmg = self.sb("bmg", [128, 4, 8], F32)
    S.dma('sp', self.bmg[:, :, :], self.inp("b_merge_l", [128, 4, 8]))
    self.pbufs = [self.sb(f"pbuf{i}", [128, 512], BF16) for i in range(4)]
    self._pn = 0
    self.wbufs = [self.sb(f"wbuf{i}", [128, 8, 384], BF16) for i in range(3)]
    self.oT = self.sb("oT", [64, 18, 512], BF16)
    self.oTb = self.sb("oTb", [128, 3, 512], BF16)
    self.xw = self.sb("xw", [128, 8, 1536], BF16)
    self.arb = self.sb("arb", [128, 29184], BF16)
    self.arf = self.sb("arf", [128, 6144], F32)
    self.load_lnp(0)
    self.rlb = [self.sb(f"rlb{i}", [64, 512], F32) for i in range(2)]
    if 2 in self.mixers:
        self.Eq = self.sb("Eq", [128, 32, 128], BF16)
        S.dma('pool', self.Eq[:, :, :], self.inp("c_Eq", [128, 32, 128]))
        self.ovl = self.sb("ovl", [128, NKT, NS + 1], BF16)
        S.dma('pool', self.ovl[:, :, :], self.inp("c_ovl", [128, NKT, NS + 1]))
        self.dqk = self.sb("dqk", [128, 2, 512], F32)
        S.dma('sp', self.dqk[:, :, :], self.inp("c_dqk", [128, 2, 512]))
        self.thr = self.sb("thr", [128, 18], F32)
        S.dma('sp', self.thr[:, :], self.inp("pc_thr", [128, 18]))
        self.negselT = self.sb("negselT", [64, 2, self.NS // 64, 512], BF16)
        self.ibias = self.sb("ibias", [128, 4, NS], F32)
        self.selw = self.sb("selw", [128, NS], F32)
        self.m8 = self.sb("m8", [128, 24], F32)


def stage_k(self):
    from contextlib import ExitStack
    S, nc = self.S, self.nc
    SB, NKT = self.SB, self.NKT
    xTf = self.inp("xT_full", [1024, SB])
    self.kslc_d = self.scratch("kslc_d", [2, 64, SB], BF16)
    self.vslc_d = self.scratch("vslc_d", [SB, 128], BF16)
    kcmp_d = self.scratch("kcmp_d", [4, 64, SB + 32], BF16)
    w1_d = self.inp("cmp_w1_l", [64, 2, 32, 128])
    pos_d = self.inp("cmp_pos_l", [64, 2, 32])
    b1_d = self.inp("cmp_b1_l", [128, 2])
    w2_d = self.inp("cmp_w2_l", [128, 2, 64])
    b2k_d = self.inp("cmp_b2k_l", [64, 1])
    b2v_d = self.inp("cmp_b2v_l", [1, 64])
    self.stk = ExitStack()
    wk = self.sb("k_w", [128, 8, 512], BF16)
    S.dma('pool', wk[:, :, 0:384], self.w_in_d[:, 64 * U_KCC:64 * U_KCC + 384].rearrange("(k p) n -> p k n", p=128))
    S.dma('pool', wk[:, :, 384:512], self.w_in_d[:, C_VSC:C_VSC + 128].rearrange("(k p) n -> p k n", p=128))
    xp = [self.sb(f"k_xp{i}", [128, 8, 512], BF16) for i in range(2)]
    ut = [self.sb(f"k_ut{i}", [64, 6, 512], BF16) for i in range(2)]
    vt = [self.sb(f"k_vt{i}", [128, 4, 128], BF16) for i in range(2)]
    zt = self.sb("k_zero", [64, 32], BF16)
    S.memset('pool', zt[:, :], 0.0)
    for s in range(4):
        S.dma('sp', kcmp_d[s, :, SB:SB + 32], zt[:, :])
    for j in range(SB // 512):
        x_ = xp[j % 2]
        S.dma('pool', x_[:, :, :], xTf[:, 512 * j:512 * j + 512].rearrange("(k p) n -> p k n", p=128))
        u_ = ut[j % 2]
        for u in range(6):
            proj_fm(self, wk, 64 * u, u_[:, u, :], lambda k, c0, n: x_[:, k, c0:c0 + n], 512)
        for s, u in enumerate((0, 1, 4, 5)):
            S.dma('sp', kcmp_d[s, :, 512 * j:512 * j + 512], u_[:, u, :])
        for kv in range(2):
            S.dma('sp', self.kslc_d[kv, :, 512 * j:512 * j + 512], u_[:, 2 + kv, :])
        v_ = vt[j % 2]
        for t in range(4):
            ps = self.ps.bank('pj', [6, 7])
            for k in range(8):
                S.mm(ps[:, 0:128], x_[:, k, 128 * t:128 * t + 128], wk[:, k, 384:512], start=(k == 0), stop=(k == 7))
            evac(self, v_[:, t, :], ps[:, 0:128])
        S.dma('sp', self.vslc_d[512 * j:512 * j + 512, :].rearrange("(t p) c -> p t c", p=128), v_[:, :, :])
    w1 = self.sb("k_w1", [64, 2, 32, 128], BF16)
    S.dma('pool', w1[:, :, :, :], w1_d)
    pos = self.sb("k_pos", [64, 2, 32], BF16)
    S.dma('pool', pos[:, :, :], pos_d)
    b1 = self.sb("k_b1", [128, 2], F32)
    S.dma('sp', b1[:, :], b1_d)
    w2 = self.sb("k_w2", [128, 2, 64], BF16)
    S.dma('pool', w2[:, :, :], w2_d)
    b2k = self.sb("k_b2k", [64, 1], F32)
    S.dma('sp', b2k[:, :], b2k_d)
    b2v = self.sb("k_b2v", [1, 64], BF16)
    S.dma('pool', b2v[:, :], b2v_d)
    beff = self.sb("k_beff", [128, 2], F32)
    for wh in range(2):
        ps = self.ps.bank('pj', [6, 7])
        for p_ in range(32):
            S.mm(ps[:, 0:1], w1[:, wh, p_, :], pos[:, wh, p_:p_ + 1], start=(p_ == 0), stop=(p_ == 31))
        S.tt('dve', beff[:, wh:wh + 1], ps[:, 0:1], b1[:, wh:wh + 1], ALU.add)
    kk = [self.sb(f"k_kk{i}", [64, 8192 + 32], BF16) for i in range(2)]
    hx = self.sb("k_hx", [128, 512], F32)
    hu = self.sb("k_hu", [128, 512], F32)
    hT = self.sb("k_hT", [128, 512], BF16)
    S.memset('pool', self.vc[:, :, :, 64:128], 1.0)
    it = 0
    for wh in range(2):
        for kv in range(2):
            s = wh * 2 + kv
            for bg in range((NKT * 128 + 511) // 512):
                nb = min(512, NKT * 128 - 512 * bg)
                k_ = kk[it % 2]
                it += 1
                ntok = nb * 16 + 16
                S.dma('sp', k_[:, 0:ntok], kcmp_d[s, :, 8192 * bg:8192 * bg + ntok])
                hp = self.ps.bank('sc', [0, 1, 2])
                for p_ in range(32):
                    S.mm(hp[:, 0:nb], w1[:, wh, p_, :], k_[:, p_:p_ + 16 * nb:16], start=(p_ == 0), stop=(p_ == 31))
                S.act(hx[:, 0:nb], hp[:, 0:nb], AF.Identity, bias=beff[:, wh:wh + 1])
                S.tt('dve', hu[:, 0:nb], hx[:, 0:nb], hx[:, 0:nb], ALU.mult)
                S.ts('dve', hu[:, 0:nb], hu[:, 0:nb], 0.044715, 1.0, ALU.mult, ALU.add)
                S.tt('dve', hu[:, 0:nb], hu[:, 0:nb], hx[:, 0:nb], ALU.mult)
                S.act(hu[:, 0:nb], hu[:, 0:nb], AF.Sigmoid, scale=1.5957691216057308)
                S.tt('dve', hT[:, 0:nb], hx[:, 0:nb], hu[:, 0:nb], ALU.mult)
                if wh == 0:
                    kp = self.ps.bank('pj', [6, 7])
                    S.mm(kp[0:64, 0:nb], w2[:, 0, :], hT[:, 0:nb])
                    S.act(self.kcT[:, kv, 512 * bg:512 * bg + nb], kp[0:64, 0:nb], AF.Identity, bias=b2k[:, 0:1])
                else:
                    for t in range(nb // 128):
                        vp = self.ps.bank('pj', [6, 7])
                        S.mm(vp[:, 0:64], hT[:, 128 * t:128 * t + 128], w2[:, 1, :], start=True, stop=False)
                        S.mm(vp[:, 0:64], self.ones_bf[0:1, 0:128], b2v[0:1, :], start=False, stop=True)
                        evac(self, self.vc[:, bg * 4 + t, kv, 0:64], vp[:, 0:64])
    barrier(self)
    self.stk.close()
    self.stk = None


Prog.sb = _sb
Prog.stage_k = stage_k
Prog.stage_m_setup = stage_m_setup


def stage_m(self, x_own, xmid_d, xmT_d):
    from contextlib import ExitStack
    S, nc = self.S, self.nc
    NM, NS, NKT, NH = self.NM, self.SB // 64, self.SB // 2048, (self.SB // 64 + 127) // 128
    mix = self.mixers
    self.stk = ExitStack()
    self.NS, self.NKT, self.NH = NS, NKT, NH
    self.w_in_d = self.inp("w_in_u", [1024, NCOLS])
    if 2 in mix:
        self.kcT = self.sb("kcT", [64, 2, NKT * 128], BF16)
        self.vc = self.sb("vc", [128, NKT, 2, 128], BF16)
        stk_m = self.stk
        stage_k(self)
        self.stk = stk_m
    stage_m_setup(self)
    xTw_d = self.inp("xT_win", [NM, 1024, WIN])
    pad_d = self.inp("padrow", [NM, 1, WIN])
    wg_d = self.inp("w_mgate_l", [8, 128, 4, 8, 128])
    wb_d = self.inp("w_branch_l", [8, 64, 18, 128])
    wbc_d = self.inp("w_branchc_l", [8, 128, 3, 128])
    wout_d = self.inp("w_out_l", [1024, 1024])
    if 2 in mix:
        ibias_d = self.inp("pc_impbias", [NM, 128, 4, NS])
    arb, arf, xw = self.arb, self.arf, self.xw
    o = 0
    qbuf, o = carve(arb, o, 0, 65, (6, 512))
    kbuf, o = carve(arb, o, 0, 65, (2, WIN))
    vbuf, o = carve(arb, o, 0, 128, (32, 2, 128))
    o_x = o
    xh, _ = carve(arb, o_x, 0, 128, (8, 1024))
    gbuf, o = carve(arb, o, 0, 64, (9, 512))
    ET, _ = carve(arb, o, 0, 128, (8, 512))
    kslab = []
    vslab = []
    for i in range(2):
        a, o = carve(arb, o, 0, 64, (2048,))
        kslab.append(a)
    for i in range(2):
        a, o = carve(arb, o, 0, 128, (16, 2, 64))
        vslab.append(a)
    assert o <= 29184, o
    f = 0
    Oacc, f = carve(arf, f, 0, 128, (6, 512))
    onsa, _ = carve(arf, 0, 0, 64, (6, 512))
    cva, _ = carve(arf, 0, 0, 128, (514,))
    cvb, _ = carve(arf, 514, 0, 128, (514,))
    imp, f = carve(arf, f, 0, 128, (4, 2, NS))
    tmpn = []
    for i in range(2):
        a, f = carve(arf, f, 0, 64, (512,))
        tmpn.append(a)
    assert f <= 6144, f
    o = 0
    wgb, wbb, wbcb = [], [], []
    for i in range(2):
        a, o = carve(arb, o, 0, 128, (4, 8, 128))
        wgb.append(a)
    for i in range(2):
        a, o = carve(arb, o, 0, 64, (18, 128))
        wbb.append(a)
    for i in range(2):
        a, o = carve(arb, o, 0, 128, (3, 128))
        wbcb.append(a)
    mergedT, o = carve(arb, o, 0, 128, (8, 512))
    wout, o = carve(arb, o, 0, 128, (8, 1024))
    assert o <= 27136, o
    f = 0
    gsb, f = carve(arf, f, 0, 128, (512,))
    macc, f = carve(arf, f, 0, 128, (512,))
    mtmp, f = carve(arf, f, 0, 128, (512,))
    tls = []
    for i in range(4):
        a, f = carve(arf, f, 0, 128, (1024,))
        tls.append(a)
    self.tl_x, self.tl_s, self.tl_xm = tls[0:1], tls[1:2], tls[2:4]
    self.tl_xmt = [self.sb(f"tl_xmt{i}", [128, 8, 128], BF16) for i in range(2)]
    assert f <= 6144, f

    def xcol(k, a, b, step=1):
        if a >= 1024:
            return xw[:, k, a - 1024:b - 1024:step]
        assert b <= 1024 + step - 1, (a, b)
        return xh[:, k, a:min(b, 1024):step]

    xrhs = lambda k, c0, n: xcol(k, 2048 + c0, 2048 + c0 + n)

    for m in range(NM):
        S.dma('pool', xw[:, :, :], xTw_d[m][:, 1024:WIN].rearrange("(k p) n -> p k n", p=128))
        for j in range(2):
            S.dma('pool', kbuf[64:65, j, :], pad_d[m])
        S.memset('pool', qbuf[64:65, :, :], 1.0)
        S.memset('pool', vbuf[:, :, :, 64:128], 1.0)
        for i in range(2):
            S.memset('pool', vslab[i][:, :, 1, :], 1.0)
        groups = []
        if 1 in mix:
            groups += [(C_CONV, 384), (C_CONV + 384, 384), (C_CONV + 768, 384)]
        if 0 in mix:
            for g in range(3):
                groups += [(64 * (U_QA + 6 * g), 384), (64 * (U_KA + 6 * g), 384), (C_VA + 384 * g, 384)]
        if 3 in mix:
            groups += [(64 * U_QD, 384), (64 * U_KD, 128), (C_VD, 128)]
        if 2 in mix:
            groups += [(64 * U_QC, 384), (64 * U_KWC, 128), (C_VWC, 128)]
            for kv in range(2):
                groups += [(64 * (U_GN + 9 * kv), 384), (64 * (U_GN + 9 * kv + 6), 192)]
        wplan(self, groups)

        if 1 in mix:
            wgb_ = wget(self, C_CONV, 384)
            for c in range(3):
                p3 = self.ps.bank('pj', [6, 7])
                for k in range(8):
                    S.mm(p3[:, :], wgb_[:, k, 128 * c:128 * c + 128], xcol(k, 2048, 2560), start=(k == 0), stop=(k == 7))
                evac(self, self.oTb[:, c, :], p3[:, :])
            wgc_ = wget(self, C_CONV + 384, 384)
            whb = wget(self, C_CONV + 768, 384)
            for c in range(3):
                u = cva
                for (c0, n, dcol) in ((2046, 2, 0), (2048, 512, 2)):
                    p1 = self.ps.bank('pj', [6, 7])
                    p2 = self.ps.bank('pj', [6, 7])
                    for k in range(8):
                        S.mm(p1[:, 0:n], wgc_[:, k, 128 * c:128 * c + 128], xcol(k, c0, c0 + n), start=(k == 0), stop=(k == 7))
                    for k in range(8):
                        S.mm(p2[:, 0:n], whb[:, k, 128 * c:128 * c + 128], xcol(k, c0, c0 + n), start=(k == 0), stop=(k == 7))
                    S.copy('act', cvb[:, dcol:dcol + n], p1[:, 0:n])
                    S.tt('dve', u[:, dcol:dcol + n], cvb[:, dcol:dcol + n], p2[:, 0:n], ALU.mult)
                S.ts('dve', cvb[:, 0:512], u[:, 0:512], self.convw[:, c, 0:1], None, ALU.mult)
                S.stt('dve', cvb[:, 0:512], u[:, 1:513], self.convw[:, c, 1:2], cvb[:, 0:512], ALU.mult, ALU.add)
                S.stt('dve', cvb[:, 0:512], u[:, 2:514], self.convw[:, c, 2:3], cvb[:, 0:512], ALU.mult, ALU.add)
                S.tt('pool', self.oTb[:, c, :], cvb[:, 0:512], self.oTb[:, c, :], ALU.mult)

        if 0 in mix:
            for g in range(3):
                D = DILS[g][1]
                lo = 2048 - DILS[g][0]
                nq = 512 // D
                uq0 = 2048 // D
                uk0 = uq0 - 128
                nk = 128 + nq
                ntile = (nk + 127) // 128
                if lo < 1024:
                    S.dma('pool', xh[:, :, :], xTw_d[m][:, 0:1024].rearrange("(k p) n -> p k n", p=128))
                wq = wget(self, 64 * (U_QA + 6 * g), 384)
                for h in range(6):
                    proj_fm(self, wq, 64 * h, qbuf[:, h, :], xrhs, 512)
                wk = wget(self, 64 * (U_KA + 6 * g), 384)
                wv = wget(self, C_VA + 384 * g, 384)
                for hp in range(3):
                    for j in range(2):
                        proj_fm(self, wk, 64 * (2 * hp + j), kbuf[:, j, lo:WIN],
                                lambda k, c0, n: xcol(k, lo + c0, lo + c0 + n), WIN - lo)
                    for r in range(D):
                        for i in range(ntile):
                            nki = min(128, nk - 128 * i)
                            st = r + D * (uk0 + 128 * i)
                            vp = self.ps.bank('pj', [6, 7])
                            en = st + D * (nki - 1) + 1
                            if st < 1024 < en:
                                n1 = (1024 - st + D - 1) // D
                                parts = [(0, n1, st, st + D * (n1 - 1) + 1), (n1, nki, st + D * n1, en)]
                            else:
                                parts = [(0, nki, st, en)]
                            for (r0, r1, a_, b_) in parts:
                                assert r0 in (0, 32, 64), r0
                                for k in range(8):
                                    S.mm(vp[r0:r1, 0:128], xcol(k, a_, b_, D), wv[:, k, 128 * hp:128 * hp + 128],
                                         start=(k == 0), stop=(k == 7))
                            evac(self, vbuf[0:nki, r * ntile + i, :, 0:64], vp[0:nki, 0:128].rearrange("p (a b) -> p a b", a=2))
                    for j in range(2):
                        h = 2 * hp + j
                        o_ps = self.ps.bank('o', [3, 4, 5])
                        first = [True]
                        for r in range(D):
                            band_attn(self,
                                      lambda c0, c1: o_ps[:, r + D * c0:r + D * (c1 - 1) + 1:D],
                                      qbuf[:, h, r:512:D], nq, uq0,
                                      lambda i, n: kbuf[:, j, r + D * (uk0 + 128 * i):r + D * (uk0 + 128 * i) + D * (n - 1) + 1:D],
                                      lambda i, n: vsel(vbuf[0:n, r * ntile + i, :, :], j, 2),
                                      uk0, nk, 128, first)
                        if g == 0:
                            S.copy('act', Oacc[:, h, :], o_ps)
                        else:
                            S.tt('dve', Oacc[:, h, :], Oacc[:, h, :], o_ps, ALU.add)
            for h in range(6):
                normalize(self, Oacc[:, h, :], self.oT[:, h, :])

        if 3 in mix:
            lo = 2048 - 128
            wq = wget(self, 64 * U_QD, 384)
            for h in range(6):
                proj_fm(self, wq, 64 * h, qbuf[:, h, :], xrhs, 512)
            wk = wget(self, 64 * U_KD, 128)
            for j in range(2):
                proj_fm(self, wk, 64 * j, kbuf[:, j, lo:WIN], lambda k, c0, n: xcol(k, lo + c0, lo + c0 + n), WIN - lo)
            wv = wget(self, C_VD, 128)
            for i in range(5):
                vp = self.ps.bank('pj', [6, 7])
                for k in range(8):
                    S.mm(vp[:, 0:128], xcol(k, lo + 128 * i, lo + 128 * i + 128), wv[:, k, 0:128], start=(k == 0), stop=(k == 7))
                evac(self, vbuf[:, i, :, 0:64], vp[:, 0:128].rearrange("p (a b) -> p a b", a=2))
            for h in range(6):
                kv = h // 3
                o_ps = self.ps.bank('o', [3, 4, 5])
                band_attn(self, lambda c0, c1: o_ps[:, c0:c1], qbuf[:, h, :], 512, 2048,
                          lambda i, n: kbuf[:, kv, lo + 128 * i:lo + 128 * i + n],
                          lambda i, n: vsel(vbuf[0:n, i, :, :], kv, 2), lo, 640, 127, [True])
                normalize(self, o_ps, self.oT[:, 12 + h, :], extra=self.sinkexp[64:128, h:h + 1])

        if 2 in mix:
            nsa_chunk(self, m, locals())

        if getattr(self, 'debug', False) and m == NM - 1:
            dbg = self.outp("dbg_oT", [64, 18, 512], BF16)
            S.dma('sp', dbg, self.oT[:, :, :])
        S.dma('pool', wout[:, :, :], wout_d.rearrange("(k p) n -> p k n", p=128))
        for fc in range(8):
            b = fc % 2
            S.dma('pool', wgb[b][:, :, :, :], wg_d[fc])
            S.dma('pool', wbb[b][:, :, :], wb_d[fc])
            S.dma('pool', wbcb[b][:, :, :], wbc_d[fc])
            nmix = len(mix)
            for idx, mi in enumerate(mix):
                bp = self.ps.bank('sc', [0, 1, 2])
                if mi == 1:
                    for c in range(3):
                        S.mm(bp[:, :], wbcb[b][:, c, :], self.oTb[:, c, :], start=(c == 0), stop=(c == 2))
                else:
                    sl = {0: 0, 2: 6, 3: 12}[mi]
                    for h in range(6):
                        S.mm(bp[:, :], wbb[b][:, sl + h, :], self.oT[:, sl + h, :], start=(h == 0), stop=(h == 5))
                gp = self.ps.bank('o', [3, 4, 5])
                for k in range(8):
                    S.mm(gp[:, :], wgb[b][:, mi, k, :], xcol(k, 2048, 2560), start=(k == 0), stop=(k == 7))
                S.act(gsb[:, :], gp[:, :], AF.Sigmoid, bias=self.bmg[:, mi, fc:fc + 1])
                last = (idx == nmix - 1)
                if idx == 0:
                    S.tt('dve', mergedT[:, fc, :] if last else macc[:, :], gsb[:, :], bp[:, :], ALU.mult)
                else:
                    S.tt('dve', mtmp[:, :], gsb[:, :], bp[:, :], ALU.mult)
                    S.tt('pool', mergedT[:, fc, :] if last else macc[:, :], macc[:, :], mtmp[:, :], ALU.add)
        self.mix_tail(m, mergedT, wout, xmid_d, xmT_d, x_own)
    barrier(self)
    self.stk.close()
    self.stk = None


Prog.stage_m = stage_m


_PERM = win_perm()


def prep_mixer_input(prog, name, layer, xl, inp, b, qt):
    NM, SB = prog.NM, prog.SB
    NS = SB // 64
    f32 = np.float32
    if name == "w_in_u":
        return np.ascontiguousarray(inp['w_in'][layer][:, _PERM])
    if name == "xT_win":
        out = np.zeros((NM, 1024, WIN), f32)
        for m in range(NM):
            t0 = 2048 * m + 512 * qt
            lo = t0 - 2048
            a = max(lo, 0)
            out[m, :, a - lo:] = xl[b, a:t0 + 512, :].T
        return out
    if name == "padrow":
        out = np.zeros((NM, 1, WIN), f32)
        for m in range(NM):
            pos = 2048 * m + 512 * qt - 2048 + np.arange(WIN)
            out[m, 0, pos < 0] = NEG
        return out
    if name == "xT_full":
        return np.ascontiguousarray(xl[b].T)
    if name == "conv_w_l":
        return np.ascontiguousarray(inp['conv_w'][layer].reshape(3, 3, 128).transpose(2, 1, 0))
    if name == "sinks_rep":
        return np.ascontiguousarray(np.broadcast_to(inp['sinks'][layer][None, :], (128, 6)))
    if name == "b_merge_l":
        return np.ascontiguousarray(inp['b_merge_gate'][layer].reshape(4, 8, 128).transpose(2, 0, 1))
    if name == "w_mgate_l":
        w = inp['w_merge_gate'][layer].reshape(4, 8, 128, 8, 128)
        return np.ascontiguousarray(w.transpose(3, 2, 0, 1, 4))
    if name == "w_branch_l":
        w = inp['w_branch'][layer][[0, 2, 3]].reshape(3, 6, 64, 8, 128)
        return np.ascontiguousarray(w.transpose(3, 2, 0, 1, 4).reshape(8, 64, 18, 128))
    if name == "w_branchc_l":
        w = inp['w_branch'][layer][1].reshape(3, 128, 8, 128)
        return np.ascontiguousarray(w.transpose(2, 1, 0, 3))
    if name == "w_out_l":
        return np.ascontiguousarray(inp['w_out'][layer])
    if name == "cmp_w1_l":
        w = inp['cmp_w1'][layer].reshape(2, 32, 64, 128)
        return np.ascontiguousarray(w.transpose(2, 0, 1, 3))
    if name == "cmp_pos_l":
        return np.ascontiguousarray(inp['cmp_pos'][layer].transpose(2, 0, 1))
    if name == "cmp_b1_l":
        return np.ascontiguousarray(inp['cmp_b1'][layer].T)
    if name == "cmp_w2_l":
        return np.ascontiguousarray(inp['cmp_w2'][layer].transpose(1, 0, 2))
    if name == "cmp_b2k_l":
        return np.ascontiguousarray(inp['cmp_b2'][layer][0][:, None])
    if name == "cmp_b2v_l":
        return np.ascontiguousarray(inp['cmp_b2'][layer][1][None, :])
    if name == "pc_thr":
        row = np.zeros(18, f32)
        row[0:16] = 128 * np.arange(16) - 512 * qt
        row[16] = -2048 + 31 - 512 * qt
        row[17] = 31 - 512 * qt
        return np.ascontiguousarray(np.broadcast_to(row[None, :], (128, 18)))
    if name == "pc_impbias":
        out = np.zeros((NM, 128, 4, NS), f32)
        jj = np.arange(NS)[None, :]
        for m in range(NM):
            for qi in range(4):
                t = 2048 * m + 512 * qt + 128 * qi + np.arange(128)
                cur = (t // 64)[:, None]
                forced = (jj == 0) | (jj == cur) | (jj == cur - 1)
                out[m, :, qi, :] = np.where(forced, BIGB, np.where(jj <= cur, 0.0, -BIGB))
        return out
    raise KeyError(name)


def make_mixer_consts(prog):
    c = {}
    if not prog.mixers:
        return c
    c["c_masks"] = prog.mask_np
    if 2 in prog.mixers:
        NS, NKT = prog.SB // 64, prog.SB // 2048
        p = np.arange(128)[:, None, None]
        r = np.arange(16)[None, :, None]
        j = np.arange(128)[None, None, :]
        e1 = ((p % 32) == 2 * r + (j >= 64)).astype(np.float32)
        e2 = e1 * ((p % 64) >= 32)
        c["c_Eq"] = np.ascontiguousarray(np.concatenate([e1, e2], axis=1))
        cc = (np.arange(NKT)[None, :, None] * 128 + np.arange(128)[:, None, None])
        jb = np.arange(NS)[None, None, :]
        ov = ((16 * cc < 64 * jb + 64) & (16 * cc + 31 >= 64 * jb)).astype(np.float32)
        c["c_ovl"] = np.concatenate([ov, np.ones((128, NKT, 1), np.float32)], axis=2)
        q = np.arange(512)[None, :]
        jl = np.arange(128)[:, None]
        c["c_dqk"] = np.ascontiguousarray(np.stack([q - jl, q - 16 * jl], axis=1).astype(np.float32))
    return c


def nsa_chunk(self, m, L):
    S, nc = self.S, self.nc
    NS, NKT, NH = self.NS, self.NKT, self.NH
    qbuf, kbuf, vbuf, gbuf, ET = L['qbuf'], L['kbuf'], L['vbuf'], L['gbuf'], L['ET']
    kslab, vslab, onsa, imp, tmpn = L['kslab'], L['vslab'], L['onsa'], L['imp'], L['tmpn']
    xcol, xrhs, ibias_d = L['xcol'], L['xrhs'], L['ibias_d']
    lo = 2048 - 512
    S.dma('sp', self.ibias[:, :, :], ibias_d[m])
    wq = wget(self, 64 * U_QC, 384)
    for h in range(6):
        proj_fm(self, wq, 64 * h, qbuf[:, h, :], xrhs, 512)
    wk = wget(self, 64 * U_KWC, 128)
    for j in range(2):
        proj_fm(self, wk, 64 * j, kbuf[:, j, lo:WIN], lambda k, c0, n: xcol(k, lo + c0, lo + c0 + n), WIN - lo)
    wv = wget(self, C_VWC, 128)
    for i in range(8):
        vp = self.ps.bank('pj', [6, 7])
        for k in range(8):
            S.mm(vp[:, 0:128], xcol(k, lo + 128 * i, lo + 128 * i + 128), wv[:, k, 0:128], start=(k == 0), stop=(k == 7))
        evac(self, vbuf[:, i, :, 0:64], vp[:, 0:128].rearrange("p (a b) -> p a b", a=2))
    nkt = min(m + 1, NKT)
    for kv in range(2):
        wga = wget(self, 64 * (U_GN + 9 * kv), 384)
        wgb_ = wget(self, 64 * (U_GN + 9 * kv + 6), 192)
        for u in range(9):
            w_, wc = (wga, 64 * u) if u < 6 else (wgb_, 64 * (u - 6))
            ps = self.ps.bank('pj', [6, 7])
            for k in range(8):
                S.mm(ps[0:64, :], w_[:, k, wc:wc + 64], xcol(k, 2048, 2560), start=(k == 0), stop=(k == 7))
            S.act(gbuf[:, u, :], ps[0:64, :], AF.Sigmoid)
        for hl in range(3):
            hq = 3 * kv + hl
            o_ps = self.ps.bank('o', [3, 4, 5])
            band_attn(self, lambda c0, c1: o_ps[:, c0:c1], qbuf[:, hq, :], 512, 2048,
                      lambda i, n: kbuf[:, kv, lo + 128 * i:lo + 128 * i + n],
                      lambda i, n: vsel(vbuf[0:n, i, :, :], kv, 2), lo, 1024, 511, [True])
            t_ = tmpn[0]
            normalize(self, o_ps, t_[:, :])
            S.tt('pool', onsa[:, hq, :], t_[:, :], gbuf[:, 3 * hl + 2, :], ALU.mult)
            o_ps = self.ps.bank('o', [3, 4, 5])
            for kt in range(nkt):
                s_ps = self.ps.bank('sc', [0, 1, 2])
                S.mm(s_ps[:, :], self.kcT[:, kv, 128 * kt:128 * kt + 128], qbuf[0:64, hq, :])
                S.act(ET[:, kt, :], s_ps[:, :], AF.Exp, scale=0.125)
                if kt >= m - 1:
                    S.stt('dve', ET[:, kt, :], self.dqk[:, 1, :], self.thr[:, 16 + (kt - (m - 1)):17 + (kt - (m - 1))],
                          ET[:, kt, :], ALU.is_ge, ALU.mult)
                S.mm(o_ps[:, :], self.vc[:, kt, kv, :], ET[:, kt, :], start=(kt == 0), stop=(kt == nkt - 1))
            t_ = tmpn[1]
            normalize(self, o_ps, t_[:, :], clamp=True)
            S.tt('pool', t_[:, :], t_[:, :], gbuf[:, 3 * hl + 0, :], ALU.mult)
            S.tt('pool', onsa[:, hq, :], onsa[:, hq, :], t_[:, :], ALU.add)
            for qi in range(4):
                ip = self.ps.bank('pj', [6, 7])
                for kt in range(nkt):
                    S.mm(ip[:, 0:NS + 1], ET[:, kt, 128 * qi:128 * qi + 128], self.ovl[:, kt, :], start=(kt == 0), stop=(kt == nkt - 1))
                rd = self.m8[:, 16 + qi:17 + qi]
                S.ts('dve', rd, ip[:, NS:NS + 1], 1e-30, None, ALU.max)
                S.op('dve', nc.vector.reciprocal, out=rd, in_=rd, reads=[rd], writes=[rd])
                if hl == 0:
                    S.ts('dve', imp[:, qi, kv, :], ip[:, 0:NS], rd, None, ALU.mult)
                else:
                    S.stt('dve', imp[:, qi, kv, :], ip[:, 0:NS], rd, imp[:, qi, kv, :], ALU.mult, ALU.add)
        for qi in range(4):
            ib = imp[:, qi, kv, :]
            S.tt('dve', ib, ib, self.ibias[:, qi, :], ALU.add)
            S.op('dve', nc.vector.max, out=self.m8[:, 0:8], in_=ib, reads=[ib], writes=[self.m8[:, 0:8]])
            S.op('dve', nc.vector.match_replace, out=self.selw[:, :], in_to_replace=self.m8[:, 0:8], in_values=ib,
                 imm_value=-3.0 * BIGB, reads=[self.m8[:, 0:8], ib], writes=[self.selw[:, :]])
            S.op('dve', nc.vector.max, out=self.m8[:, 8:16], in_=self.selw[:, :], reads=[self.selw[:, :]], writes=[self.m8[:, 8:16]])
            S.ts('dve', self.m8[:, 20:21], self.m8[:, 15:16], -0.5 * BIGB, None, ALU.max)
            S.ts('dve', self.selw[:, :], ib, self.m8[:, 20:21], None, ALU.is_ge)
            S.ts('dve', self.selw[:, :], self.selw[:, :], -1.0, -NEG, ALU.add, ALU.mult)
            for quarter in range(NS // 64):
                tp = self.ps.bank('pj', [6, 7])
                S.mm(tp[0:64, 0:128], self.selw[:, 64 * quarter:64 * quarter + 64], self.ident[:, :])
                evac(self, self.negselT[:, kv, quarter, 128 * qi:128 * qi + 128], tp[0:64, 0:128])
        obanks = [self.ps.t[:, (3 + hl) * 512:(4 + hl) * 512] for hl in range(3)]
        nslab = m + 1
        for i in range(nslab):
            ks, vs = kslab[i % 2], vslab[i % 2]
            S.dma('sp', ks[:, :], self.kslc_d[kv, :, 2048 * i:2048 * i + 2048])
            S.dma('sp', vs[:, :, 0, :], self.vslc_d[2048 * i:2048 * i + 2048, 64 * kv:64 * kv + 64].rearrange("(t p) c -> p t c", p=128))
            for hl in range(3):
                hq = 3 * kv + hl
                for t in range(16):
                    kt = 16 * i + t
                    row = (2 * kt) % 64
                    quarter = (2 * kt) // 64
                    q32, r = row // 32, (row % 32) // 2
                    s_ps = self.ps.bank('sc', [0, 1, 2])
                    S.mm(s_ps[:, :], ks[:, 128 * t:128 * t + 128], qbuf[0:64, hq, :], start=True, stop=False)
                    S.mm(s_ps[:, :], self.Eq[32 * q32:32 * q32 + 32, r, :], self.negselT[32 * q32:32 * q32 + 32, kv, quarter, :],
                         start=False, stop=True)
                    p = self.pbufs[self._pn % len(self.pbufs)]
                    self._pn += 1
                    S.act(p[:, :], s_ps[:, :], AF.Exp, scale=0.125)
                    if i == m:
                        S.stt('dve', p[:, :], self.dqk[:, 0, :], self.thr[:, t:t + 1], p[:, :], ALU.is_ge, ALU.mult)
                    S.mm(obanks[hl], vs[:, t, :, :].rearrange("p a b -> p (a b)"), p[:, :], start=(kt == 0), stop=(kt == 16 * nslab - 1))
        for hl in range(3):
            hq = 3 * kv + hl
            t_ = tmpn[hl % 2]
            normalize(self, obanks[hl], t_[:, :])
            S.tt('pool', t_[:, :], t_[:, :], gbuf[:, 3 * hl + 1, :], ALU.mult)
            S.tt('pool', self.oT[:, 6 + hq, :], onsa[:, hq, :], t_[:, :], ALU.add)
```

```python
import numpy as np
import ml_dtypes
import concourse.bass as bass
import concourse.mybir as mybir
from concourse.bass_utils import run_bass_kernel_spmd

F32 = mybir.dt.float32
BF16 = mybir.dt.bfloat16
I32 = mybir.dt.int32
AF = mybir.ActivationFunctionType
ALU = mybir.AluOpType
AX = mybir.AxisListType
_DSZ = {F32: 4, BF16: 2, I32: 4}

D_MODEL = 1024
HD = 64
ALPHA = 4 ** 0.25
LN_EPS = 1e-5
D_FF = 2816
D_FFE = 3584
NEXP = 8
NEG = -30000.0


class Sched:
    def __init__(self, nc, n_dma_sems=24):
        self.nc = nc
        self.e = dict(pe=nc.tensor, act=nc.scalar, dve=nc.vector, pool=nc.gpsimd, sp=nc.sync)
        self.sem = {k: nc.alloc_semaphore(name=f"sem_{k}") for k in ('pe', 'act', 'dve', 'pool')}
        self.cnt = {k: 0 for k in self.sem}
        self.dsem = [nc.alloc_semaphore(name=f"dsem{i}") for i in range(n_dma_sems)]
        self.dcnt = [0] * n_dma_sems
        self.dnext = 0
        self.waited = {k: {} for k in self.e}
        self.acc = {}
        self.nins = 0

    @staticmethod
    def box(ap):
        dims = ap.ap
        name = ap.tensor.name
        sz = _DSZ.get(ap.dtype, 4)
        if str(ap.space) == 'DRAM':
            lo = ap.offset
            hi = lo + sum((c - 1) * abs(s) for s, c in dims) + 1
            return name, (0, 1, lo * sz, hi * sz)
        pstep, pcount = dims[0]
        sp = ap.start_partition()
        flo = ap.offset - sp * pstep
        fhi = flo + sum((c - 1) * abs(s) for s, c in dims[1:]) + 1
        return name, (sp, sp + pcount, flo * sz, fhi * sz)

    @staticmethod
    def _ov(a, b):
        return a[0] < b[1] and b[0] < a[1] and a[2] < b[3] and b[2] < a[3]

    @staticmethod
    def _contains(a, b):
        return a[0] <= b[0] and b[1] <= a[1] and a[2] <= b[2] and b[3] <= a[3]

    def _deps(self, boxes):
        toks = set()
        for name, bx, isw in boxes:
            for (rbx, risw, rtok) in self.acc.get(name, ()):
                if (isw or risw) and self._ov(bx, rbx):
                    toks.add(rtok)
        return toks

    def _record(self, boxes, tok):
        for name, bx, isw in boxes:
            lst = self.acc.setdefault(name, [])
            if isw:
                lst[:] = [r for r in lst if not self._contains(bx, r[0])]
            else:
                lst[:] = [r for r in lst if not (not r[1] and r[2][0] == tok[0] and self._contains(bx, r[0]))]
            lst.append((bx, isw, tok))

    def _wait(self, eng, tok):
        key, val = tok
        if key == eng and eng == 'pe':
            return
        if self.waited[eng].get(key, 0) >= val:
            return
        self.waited[eng][key] = val
        semobj = self.sem[key] if isinstance(key, str) else self.dsem[key[1]]
        self.e[eng].wait_ge(semobj, val)
        self.nins += 1

    def _boxes(self, reads, writes):
        out = []
        for r in reads:
            if r is None or isinstance(r, (int, float)):
                continue
            n, b = self.box(r)
            out.append((n, b, False))
        for w in writes:
            n, b = self.box(w)
            out.append((n, b, True))
        return out

    def op(self, eng, fn, *args, reads=(), writes=(), **kw):
        boxes = self._boxes(reads, writes)
        for t in sorted(self._deps(boxes), key=lambda t: (str(t[0]), t[1])):
            self._wait(eng, t)
        ins = fn(*args, **kw)
        self.cnt[eng] += 1
        self.nins += 1
        ins.then_inc(self.sem[eng], 1)
        self._record(boxes, (eng, self.cnt[eng]))
        return ins

    def mm(self, out, lhsT, rhs, start=True, stop=True):
        return self.op('pe', self.nc.tensor.matmul, out, lhsT, rhs, start=start, stop=stop,
                       skip_group_check=True, reads=[lhsT, rhs], writes=[out])

    def transpose(self, out, in_, ident):
        return self.op('pe', self.nc.tensor.transpose, out, in_, ident, reads=[in_, ident], writes=[out])

    def act(self, out, in_, func, bias=None, scale=None, accum_out=None):
        kw = {}
        rd = [in_]
        wr = [out]
        if bias is not None:
            kw['bias'] = bias
            rd.append(bias)
        if scale is not None:
            kw['scale'] = scale
            rd.append(scale)
        if accum_out is not None:
            kw['accum_out'] = accum_out
            wr.append(accum_out)
        return self.op('act', self.nc.scalar.activation, out, in_, func, reads=rd, writes=wr, **kw)

    def tt(self, eng, out, in0, in1, op):
        return self.op(eng, self.e[eng].tensor_tensor, out, in0, in1, op, reads=[in0, in1], writes=[out])

    def ts(self, eng, out, in0, s1, s2, op0, op1=None, accum_out=None):
        kw = {}
        wr = [out]
        if op1 is not None:
            kw['op1'] = op1
        if accum_out is not None:
            kw['accum_out'] = accum_out
            wr.append(accum_out)
        return self.op(eng, self.e[eng].tensor_scalar, out, in0, s1, s2, op0, reads=[in0, s1, s2], writes=wr, **kw)

    def stt(self, eng, out, in0, scalar, in1, op0, op1):
        return self.op(eng, self.e[eng].scalar_tensor_tensor, out, in0, scalar, in1, op0, op1,
                       reads=[in0, scalar, in1], writes=[out])

    def copy(self, eng, out, in_):
        if eng == 'act':
            return self.op('act', self.nc.scalar.copy, out, in_, reads=[in_], writes=[out])
        return self.op(eng, self.e[eng].tensor_copy, out, in_, reads=[in_], writes=[out])

    def memset(self, eng, ap, val):
        return self.op(eng, self.e[eng].memset, ap, val, reads=[], writes=[ap])

    def dma(self, q, out, in_):
        boxes = self._boxes([in_], [out])
        toks = self._deps(boxes)
        i = self.dnext
        self.dnext = (self.dnext + 1) % len(self.dsem)
        if self.dcnt[i] > 0:
            toks.add((('d', i), 16 * self.dcnt[i]))
        for t in sorted(toks, key=lambda t: (str(t[0]), t[1])):
            self._wait(q, t)
        ins = self.e[q].dma_start(out=out, in_=in_)
        self.dcnt[i] += 1
        self.nins += 1
        ins.then_inc(self.dsem[i], 16)
        self._record(boxes, (('d', i), 16 * self.dcnt[i]))

    def finish(self):
        for i, c in enumerate(self.dcnt):
            if c:
                self._wait('sp', (('d', i), 16 * c))
        for k, c in self.cnt.items():
            if c:
                self._wait('sp', (k, c))


class PsumPool:
    def __init__(self, nc):
        self.t = nc.alloc_psum_tensor("psum_all", [128, 8 * 512], F32)
        self.rr = {}

    def bank(self, role, banks):
        i = self.rr.get(role, 0)
        self.rr[role] = i + 1
        b = banks[i % len(banks)]
        return self.t[:, b * 512:(b + 1) * 512]

    def wide(self, role, pairs):
        i = self.rr.get(role, 0)
        self.rr[role] = i + 1
        b = pairs[i % len(pairs)]
        return self.t[:, b * 512:(b + 2) * 512]


class Prog:
    def __init__(self, NM, moe, mixers=(0, 1, 2, 3)):
        self.NM = NM
        self.T = NM * 512
        self.SB = NM * 2048
        self.moe = moe
        self.mixers = mixers
        self.nc = bass.Bass("TRN2", target_bir_lowering=False)
        self.S = Sched(self.nc)
        self.ps = PsumPool(self.nc)
        self.din = {}
        self._n = 0

    def inp(self, name, shape, dt=F32):
        t = self.nc.dram_tensor(name, list(shape), dt, kind="ExternalInput")
        self.din[name] = (tuple(shape), dt)
        return t.ap()

    def outp(self, name, shape, dt=F32):
        return self.nc.dram_tensor(name, list(shape), dt, kind="ExternalOutput").ap()

    def scratch(self, name, shape, dt):
        return self.nc.dram_tensor(name, list(shape), dt, kind="Internal").ap()

    def sb(self, name, shape, dt):
        return self.nc.alloc_sbuf_tensor(name, list(shape), dt)

    def layer_norm(self, s, g_bc, b_bc, out_ap, tmp):
        S, nc = self.S, self.nc
        st = tmp['st']
        mv = tmp['mv']
        for j in range(2):
            S.op('dve', nc.vector.bn_stats, out=st[:, j, :], in_=s[:, j * 512:(j + 1) * 512],
                 reads=[s[:, j * 512:(j + 1) * 512]], writes=[st[:, j, :]])
        S.op('dve', nc.vector.bn_aggr, out=mv[:, 0:2], in_=st[:, :, :], reads=[st[:, :, :]], writes=[mv[:, 0:2]])
        S.ts('pool', mv[:, 2:3], mv[:, 1:2], LN_EPS, None, ALU.add)
        S.tt('pool', mv[:, 3:4], mv[:, 2:3], self.neghalf[:, 0:1], ALU.pow)
        S.ts('dve', s[:, :], s[:, :], mv[:, 0:1], mv[:, 3:4], ALU.subtract, ALU.mult)
        S.tt('pool', s[:, :], s[:, :], g_bc[:, :], ALU.mult)
        S.tt('pool', out_ap, s[:, :], b_bc[:, :], ALU.add)

    def setup_common(self):
        S, nc = self.S, self.nc
        self.ident = self.sb("ident", [128, 128], F32)
        S.dma('sp', self.ident[:, :], self.inp("c_ident", [128, 128]))
        self.neghalf = self.sb("neghalf", [128, 1], F32)
        S.memset('pool', self.neghalf[:, :], -0.5)
        self.ones_bf = self.sb("ones_bf", [128, 512], BF16)
        S.memset('pool', self.ones_bf[:, :], 1.0)
        self.lnp_d = self.inp("ln_params", [128, 4, 1024])
        self.lntmp = dict(st=self.sb("ln_st", [128, 2, 6], F32), mv=self.sb("ln_mv", [128, 4], F32))

    def mix_tail(self, m, mergedT, wout, xmid_d, xmT_d, x_own):
        S, nc = self.S, self.nc
        for tt in range(4):
            tok = m * 512 + tt * 128
            xt = self.tl_x[tt % len(self.tl_x)]
            S.dma('sp', xt[:, :], x_own[tok:tok + 128, :])
            s = self.tl_s[tt % len(self.tl_s)]
            if mergedT is not None:
                yps = self.ps.wide('y', [0, 2])
                for half in range(2):
                    for k in range(8):
                        S.mm(yps[:, half * 512:(half + 1) * 512], mergedT[:, k, tt * 128:(tt + 1) * 128],
                             wout[:, k, half * 512:(half + 1) * 512], start=(k == 0), stop=(k == 7))
                S.stt('dve', s[:, :], xt[:, :], ALPHA, yps, ALU.mult, ALU.add)
            else:
                S.ts('dve', s[:, :], xt[:, :], ALPHA, None, ALU.mult)
            xm = self.tl_xm[tt % len(self.tl_xm)]
            self.layer_norm(s, self.lnp[:, 0, :], self.lnp[:, 1, :], xm[:, :], self.lntmp)
            S.dma('sp', xmid_d[tok:tok + 128, :], xm[:, :])
            xmt = self.tl_xmt[tt % len(self.tl_xmt)]
            for half in range(2):
                tp = self.ps.bank('tp', [4, 5])
                for j in range(4):
                    fc = half * 4 + j
                    S.mm(tp[:, j * 128:(j + 1) * 128], xm[:, fc * 128:(fc + 1) * 128], self.ident[:, :])
                S.copy('act', xmt[:, half * 4:(half + 1) * 4, :],
                       tp.rearrange("p (j t) -> p j t", j=4))
            S.dma('sp', xmT_d[:, :, tok:tok + 128], xmt[:, :, :])

    def stage_f(self, xmid_d, xmT_d, y_out):
        S, nc = self.S, self.nc
        T, NT = self.T, self.T // 128
        moe = self.moe
        NE = NEXP if moe else 1
        FF = D_FFE if moe else D_FF
        GW = 256
        NG = FF // GW
        if moe:
            wg_d = self.inp("moe_w_gate", [NEXP, 1024, FF])
            wu_d = self.inp("moe_w_up", [NEXP, 1024, FF])
            wd_d = self.inp("moe_w_down", [NEXP, FF, 1024])
            wr_d = self.inp("w_router", [1024, NEXP])
            br_d = self.inp("b_router", [1, NEXP])
        else:
            wg_d = self.inp("ffn_w_gate", [1, 1024, FF])
            wu_d = self.inp("ffn_w_up", [1, 1024, FF])
            wd_d = self.inp("ffn_w_down", [1, FF, 1024])
        plew_d = self.inp("ple_w", [256, 1024])
        plegw_d = self.inp("ple_gate_w", [1024, 1024])
        plegb_d = self.inp("ple_gate_b", [1, 1024])
        pT_d = self.inp("pT_own", [256, T])

        HT = min(16, NT)
        xmT = self.sb("f_xmT", [128, 8, HT * 128], BF16)
        acc = self.sb("f_acc", [128, HT, 1024], F32)
        comb = self.sb("f_comb", [128, NT, 8], F32)
        wgb = [self.sb(f"f_wg{i}", [128, 8, GW], BF16) for i in range(2)]
        wub = [self.sb(f"f_wu{i}", [128, 8, GW], BF16) for i in range(2)]
        wdb = [self.sb(f"f_wd{i}", [128, GW // 128, 1024], BF16) for i in range(2)]
        hT = [self.sb(f"f_hT{i}", [128, GW // 128, 512], BF16) for i in range(2)]
        sg = [self.sb(f"f_sg{i}", [128, 512], F32) for i in range(2)]
        plegw = self.sb("f_plegw", [128, 8, 1024], BF16)
        plew = self.sb("f_plew", [128, 2, 1024], BF16)
        plegb = self.sb("f_plegb", [1, 1024], BF16)
        pT = self.sb("f_pT", [128, 2, HT * 128], BF16)
        S.dma('pool', plegw[:, :, :], plegw_d.rearrange("(k p) n -> p k n", p=128))
        S.dma('pool', plew[:, :, :], plew_d.rearrange("(k p) n -> p k n", p=128))
        S.dma('pool', plegb[:, :], plegb_d)
        sig = [self.sb(f"f_sig{i}", [128, 1024], F32) for i in range(1)]

        if moe:
            wr = self.sb("f_wr", [128, 8, NEXP], BF16)
            brt = self.sb("f_br", [1, NEXP], BF16)
            S.dma('pool', wr[:, :, :], wr_d.rearrange("(k p) n -> p k n", p=128))
            S.dma('pool', brt[:, :], br_d)
            lg = self.sb("f_lg", [128, 8], F32)
            m8 = self.sb("f_m8", [128, 8], F32)
            ex = self.sb("f_ex", [128, 8], F32)
            msk = self.sb("f_msk", [128, 8], F32)
            den = self.sb("f_den", [128, 2], F32)
        else:
            S.memset('pool', comb[:, :, :], 1.0)

        def router(h0):
            for tl in range(HT):
                t = h0 + tl
                lp = self.ps.bank('rt', [6, 7])
                for k in range(8):
                    S.mm(lp[:, 0:NEXP], xmT[:, k, tl * 128:(tl + 1) * 128], wr[:, k, :], start=(k == 0), stop=False)
                S.mm(lp[:, 0:NEXP], self.ones_bf[0:1, 0:128], brt[0:1, :], start=False, stop=True)
                S.copy('dve', lg[:, :], lp[:, 0:NEXP])
                S.op('dve', nc.vector.max, out=m8[:, :], in_=lg[:, :], reads=[lg[:, :]], writes=[m8[:, :]])
                S.ts('dve', msk[:, :], lg[:, :], m8[:, 1:2], None, ALU.is_ge)
                S.ts('dve', den[:, 0:1], m8[:, 0:1], -1.0, None, ALU.mult)
                S.act(ex[:, :], lg[:, :], AF.Exp, bias=den[:, 0:1])
                S.tt('dve', ex[:, :], ex[:, :], msk[:, :], ALU.mult)
                S.op('dve', nc.vector.tensor_reduce, out=den[:, 1:2], in_=ex[:, :], axis=AX.X, op=ALU.add,
                     reads=[ex[:, :]], writes=[den[:, 1:2]])
                S.op('dve', nc.vector.reciprocal, out=den[:, 1:2], in_=den[:, 1:2], reads=[den[:, 1:2]], writes=[den[:, 1:2]])
                S.ts('dve', comb[:, t, :], ex[:, :], den[:, 1:2], None, ALU.mult)

        gi = 0
        for h0 in range(0, NT, HT):
            S.dma('sp', xmT[:, :, :], xmT_d[:, :, h0 * 128:(h0 + HT) * 128])
            S.dma('pool', pT[:, :, :], pT_d[:, h0 * 128:(h0 + HT) * 128].rearrange("(k p) n -> p k n", p=128))
            if moe:
                router(h0)
            for tl in range(HT):
                t = h0 + tl
                gps = self.ps.wide('y', [0, 2])
                pps = self.ps.wide('pp', [4, 6])
                for half in range(2):
                    cs = slice(half * 512, (half + 1) * 512)
                    for k in range(8):
                        S.mm(gps[:, cs], xmT[:, k, tl * 128:(tl + 1) * 128], plegw[:, k, cs], start=(k == 0), stop=False)
                    S.mm(gps[:, cs], self.ones_bf[0:1, 0:128], plegb[0:1, cs], start=False, stop=True)
                    for k in range(2):
                        S.mm(pps[:, cs], pT[:, k, tl * 128:(tl + 1) * 128], plew[:, k, cs], start=(k == 0), stop=(k == 1))
                sg_ = sig[0]
                S.act(sg_[:, :], gps, AF.Sigmoid)
                S.tt('dve', acc[:, tl, :], sg_[:, :], pps, ALU.mult)
            nchunk = HT // 4
            for e in range(NE):
                for g in range(NG):
                    b = gi % 2
                    gi += 1
                    S.dma('pool', wgb[b][:, :, :], wg_d[e, :, g * GW:(g + 1) * GW].rearrange("(k p) n -> p k n", p=128))
                    S.dma('pool', wub[b][:, :, :], wu_d[e, :, g * GW:(g + 1) * GW].rearrange("(k p) n -> p k n", p=128))
                    S.dma('pool', wdb[b][:, :, :], wd_d[e, g * GW:(g + 1) * GW, :].rearrange("(k p) n -> p k n", p=128))
                    for c in range(nchunk):
                        tok0 = (c * 4) * 128
                        hb = hT[c % 2]
                        for j in range(GW // 128):
                            hg = self.ps.bank('hg', [0, 1])
                            hu = self.ps.bank('hu', [2, 3])
                            for k in range(8):
                                S.mm(hg, wgb[b][:, k, j * 128:(j + 1) * 128], xmT[:, k, tok0:tok0 + 512], start=(k == 0), stop=(k == 7))
                            for k in range(8):
                                S.mm(hu, wub[b][:, k, j * 128:(j + 1) * 128], xmT[:, k, tok0:tok0 + 512], start=(k == 0), stop=(k == 7))
                            s_ = sg[j % 2]
                            S.act(s_[:, :], hg, AF.Silu)
                            S.tt('dve', hb[:, j, :], s_[:, :], hu, ALU.mult)
                        for tl4 in range(4):
                            tl = c * 4 + tl4
                            fps = self.ps.wide('pp', [4, 6])
                            for half in range(2):
                                cs = slice(half * 512, (half + 1) * 512)
                                for j in range(GW // 128):
                                    S.mm(fps[:, cs], hb[:, j, tl4 * 128:(tl4 + 1) * 128], wdb[b][:, j, cs],
                                         start=(j == 0), stop=(j == GW // 128 - 1))
                            S.stt('dve', acc[:, tl, :], fps, comb[:, h0 + tl, e:e + 1], acc[:, tl, :], ALU.mult, ALU.add)
            for tl in range(HT):
                t = h0 + tl
                xm = self.tl_xm[tl % len(self.tl_xm)]
                S.dma('sp', xm[:, :], xmid_d[t * 128:(t + 1) * 128, :])
                s = self.tl_s[tl % len(self.tl_s)]
                S.stt('dve', s[:, :], xm[:, :], ALPHA, acc[:, tl, :], ALU.mult, ALU.add)
                o = self.tl_x[tl % len(self.tl_x)]
                self.layer_norm(s, self.lnp[:, 0, :], self.lnp[:, 1, :], o[:, :], self.lntmp)
                S.dma('sp', y_out[t * 128:(t + 1) * 128, :], o[:, :])

    def load_lnp(self, a):
        self.lnp = self.sb(f"lnp{a}", [128, 2, 1024], F32)
        self.S.dma('sp', self.lnp[:, :, :], self.lnp_d[:, a:a + 2, :])

    def alloc_tiles(self):
        self.tl_x = [self.sb(f"tl_x{i}", [128, 1024], F32) for i in range(1)]
        self.tl_s = [self.sb(f"tl_s{i}", [128, 1024], F32) for i in range(1)]
        self.tl_xm = [self.sb(f"tl_xm{i}", [128, 1024], F32) for i in range(1)]
        self.tl_xmt = [self.sb(f"tl_xmt{i}", [128, 8, 128], BF16) for i in range(2)]

    def build(self):
        S = self.S
        T = self.T
        self.stk = None
        self.setup_common()
        x_own = self.inp("x_own", [T, 1024])
        y_out = self.outp("y", [T, 1024])
        xmid_d = self.scratch("xmid_d", [T, 1024], F32)
        xmT_d = self.scratch("xmT_d", [128, 8, T], BF16)
        from contextlib import ExitStack
        if self.mixers:
            self.stage_m(x_own, xmid_d, xmT_d)
        else:
            self.stk = ExitStack()
            self.alloc_tiles()
            self.load_lnp(0)
            for m in range(self.NM):
                self.mix_tail(m, None, None, xmid_d, xmT_d, x_own)
            barrier(self)
            self.stk.close()
        self.stk = ExitStack()
        self.alloc_tiles()
        self.load_lnp(2)
        self.stage_f(xmid_d, xmT_d, y_out)
        S.finish()
        return self.nc


def own_positions(NM, qt):
    return np.concatenate([np.arange(512) + 2048 * m + 512 * qt for m in range(NM)])


def prep_core_inputs(prog, layer, xl, inp, core, consts):
    NM = prog.NM
    b, qt = core // 4, core % 4
    pos = own_positions(NM, qt)
    d = {}
    need = prog.din
    j = layer // 2
    for name in need:
        if name == "x_own":
            d[name] = np.ascontiguousarray(xl[b, pos, :])
        elif name == "pT_own":
            d[name] = np.ascontiguousarray(inp['p'][layer, b, pos, :].T)
        elif name == "ln_params":
            rows = np.stack([inp['ln_mix_g'][layer], inp['ln_mix_b'][layer], inp['ln_ffn_g'][layer], inp['ln_ffn_b'][layer]])
            d[name] = np.ascontiguousarray(np.broadcast_to(rows[None], (128, 4, 1024)))
        elif name in ("ffn_w_gate", "ffn_w_up", "ffn_w_down"):
            d[name] = np.ascontiguousarray(inp[name][j:j + 1])
        elif name in ("moe_w_gate", "moe_w_up", "moe_w_down", "w_router"):
            d[name] = np.ascontiguousarray(inp[name][j])
        elif name == "b_router":
            d[name] = np.ascontiguousarray(inp[name][j][None, :])
        elif name in ("ple_w", "ple_gate_w"):
            d[name] = np.ascontiguousarray(inp[name][layer])
        elif name == "ple_gate_b":
            d[name] = np.ascontiguousarray(inp[name][layer][None, :])
        elif name in consts:
            d[name] = consts[name]
        else:
            d[name] = prep_mixer_input(prog, name, layer, xl, inp, b, qt)
        shape, dt = need[name]
        assert tuple(d[name].shape) == tuple(shape), (name, d[name].shape, shape)
    return d


def make_consts(prog):
    c = {"c_ident": np.eye(128, dtype=np.float32)}
    c.update(make_mixer_consts(prog))
    return c


_PROGS = {}
DEBUG = False
LAST = {}


def run_layer(layer, xl, inp, NM, moe, mixers=(0, 1, 2, 3)):
    key = (NM, moe, tuple(mixers))
    if key not in _PROGS:
        p = Prog(NM, moe, mixers)
        p.debug = DEBUG
        p.build()
        _PROGS[key] = (p, make_consts(p))
    prog, consts = _PROGS[key]
    in_maps = [prep_core_inputs(prog, layer, xl, inp, c, consts) for c in range(8)]
    res = run_bass_kernel_spmd(prog.nc, in_maps, core_ids=list(range(8)))
    LAST['res'] = res
    B, S_, _ = xl.shape
    out = np.empty_like(xl)
    for c in range(8):
        b, qt = c // 4, c % 4
        out[b, own_positions(NM, qt), :] = res.results[c]["y"]
    return out


def kernel(**inputs):
    inp = {k: np.asarray(v) for k, v in inputs.items()}
    x = inp['x'].astype(np.float32, copy=False)
    B, S_, _ = x.shape
    NM = S_ // 2048
    depth = inp['w_in'].shape[0]
    for layer in range(depth):
        x = run_layer(layer, x, inp, NM, moe=(layer % 2 == 1))
    return x


U_QA, U_KA, U_QC, U_QD, U_KWC, U_KD, U_KCC, U_KSC, U_VCC, U_GN = 0, 18, 36, 42, 48, 50, 52, 54, 56, 58
C_CONV, C_VA, C_VWC, C_VD, C_VSC, NCOLS = 4864, 6016, 7168, 7296, 7424, 7552
WIN = 2560
DILS = ((128, 1), (512, 4), (2048, 16))
BIGB = 1000.0


def win_perm():
    sizes = [1152, 1152, 1152, 384, 384, 384, 384, 128, 128, 128, 128, 128, 128, 18, 384, 128, 128]
    names = ['qa', 'ka', 'va', 'gb', 'gc', 'hb', 'qc', 'kcc', 'vcc', 'ksc', 'vsc', 'kwc', 'vwc', 'gn', 'qd', 'kd', 'vd']
    off = dict(zip(names, np.cumsum([0] + sizes[:-1]).tolist()))
    r = lambda a, n: list(range(a, a + n))
    cols = []
    cols += r(off['qa'], 1152) + r(off['ka'], 1152) + r(off['qc'], 384) + r(off['qd'], 384)
    cols += r(off['kwc'], 128) + r(off['kd'], 128) + r(off['kcc'], 128) + r(off['ksc'], 128) + r(off['vcc'], 128)
    for hq in range(6):
        for br in range(3):
            cols += [off['gn'] + br * 6 + hq] * 64
    cols += r(off['gb'], 1152)
    cols += r(off['va'], 1152) + r(off['vwc'], 128) + r(off['vd'], 128) + r(off['vsc'], 128)
    assert len(cols) == NCOLS
    return np.array(cols)


def _sb(self, name, shape, dt):
    used = self.__dict__.setdefault('_names', {})
    used[name] = used.get(name, 0) + 1
    if used[name] > 1:
        name = f"{name}_r{used[name]}"
    if getattr(self, 'stk', None) is not None:
        return self.stk.enter_context(self.nc.sbuf_tensor(name, list(shape), dt))
    return self.nc.alloc_sbuf_tensor(name, list(shape), dt)


def barrier(self):
    S = self.S
    for eng in ('pe', 'act', 'dve', 'pool', 'sp'):
        for i, c in enumerate(S.dcnt):
            if c:
                S._wait(eng, (('d', i), 16 * c))
        for k, c in S.cnt.items():
            if c and k != eng:
                S._wait(eng, (k, c))
    S.acc.clear()


def get_mask(self, allowed):
    nk, n = allowed.shape
    key = (nk, n, allowed.tobytes())
    if key not in self.mask_idx:
        off = self.mask_used
        assert off + n <= self.MCAP, "mask bank full"
        self.mask_np[:nk, off:off + n] = allowed.astype(np.float32)
        self.mask_idx[key] = off
        self.mask_used += n
    off = self.mask_idx[key]
    return self.maskbank[0:nk, off:off + n]


def wplan(self, groups):
    self.wg, self.wi, self.wissued = groups, 0, 0


def wget(self, c0, n):
    i = self.wi
    assert self.wg[i] == (c0, n), (i, self.wg[i], c0, n)
    self.wi += 1
    while self.wissued < min(len(self.wg), i + 2):
        j = self.wissued
        cc, nn = self.wg[j]
        self.S.dma('pool', self.wbufs[j % 3][:, :, 0:nn], self.w_in_d[:, cc:cc + nn].rearrange("(k p) n -> p k n", p=128))
        self.wissued += 1
    return self.wbufs[i % 3]


def evac(self, dst, src):
    self._n += 1
    self.S.copy('act' if self._n % 2 else 'dve', dst, src)


def proj_fm(self, w, wcol, dst, rhs_fn, ncols, rows=64):
    S = self.S
    for c0 in range(0, ncols, 512):
        n = min(512, ncols - c0)
        ps = self.ps.bank('pj', [6, 7])
        for k in range(8):
            S.mm(ps[0:rows, 0:n], w[:, k, wcol:wcol + rows], rhs_fn(k, c0, n), start=(k == 0), stop=(k == 7))
        evac(self, dst[0:rows, c0:c0 + n], ps[0:rows, 0:n])


PV_DEPTH = 3


def pv_defer(self, fn):
    q = self.__dict__.setdefault('pvq', [])
    q.append(fn)
    while len(q) > PV_DEPTH:
        q.pop(0)()


def pv_flush(self):
    q = self.__dict__.setdefault('pvq', [])
    while q:
        q.pop(0)()


def band_attn(self, o_ps_fn, qT, nq, uq0, kT_fn, v_fn, uk0, nk, wd, first):
    S = self.S
    nt = (nk + 127) // 128
    for i in range(nt):
        nki = min(128, nk - 128 * i)
        k_lo = uk0 + 128 * i
        k_hi = k_lo + nki - 1
        c0 = max(k_lo, uq0) - uq0
        c1 = min(k_hi + wd, uq0 + nq - 1) - uq0 + 1
        if c1 <= c0:
            continue
        n = c1 - c0
        kk = np.arange(k_lo, k_lo + nki)[:, None]
        qq = np.arange(uq0 + c0, uq0 + c1)[None, :]
        allowed = (kk <= qq) & (kk >= qq - wd)
        s_ps = self.ps.bank('sc', [0, 1, 2])
        S.mm(s_ps[0:nki, 0:n], kT_fn(i, nki), qT[:, c0:c1])
        p = self.pbufs[self._pn % len(self.pbufs)]
        self._pn += 1
        S.act(p[0:nki, 0:n], s_ps[0:nki, 0:n], AF.Exp, scale=0.125)
        if not allowed.all():
            bad = np.where(~allowed.all(axis=0))[0]
            a, b = int(bad.min()), int(bad.max()) + 1
            mk = get_mask(self, allowed[:, a:b])
            S.tt('dve', p[0:nki, a:b], p[0:nki, a:b], mk, ALU.mult)
        pv_defer(self, lambda o_=o_ps_fn(c0, c1), v_=v_fn(i, nki), p_=p[0:nki, 0:n], st_=first[0]:
                 S.mm(o_, v_, p_, start=st_, stop=False))
        first[0] = False


def vsel(vt, h, nb):
    return vt[:, h, :]


def carve(ar, off, p0, p1, shape):
    n = int(np.prod(shape))
    v = ar[p0:p1, off:off + n]
    if len(shape) == 2:
        v = v.rearrange("p (a b) -> p a b", a=shape[0])
    elif len(shape) == 3:
        v = v.rearrange("p (a b c) -> p a b c", a=shape[0], b=shape[1])
    return v, off + n


def normalize(self, o_src, dst, extra=None, clamp=False):
    S, nc = self.S, self.nc
    rl = self.rlb[self._n % 2]
    self._n += 1
    if extra is not None:
        S.ts('dve', rl[:, :], o_src[64:128, :], extra, None, ALU.add)
        src = rl[:, :]
    elif clamp:
        S.ts('dve', rl[:, :], o_src[64:128, :], 1e-30, None, ALU.max)
        src = rl[:, :]
    else:
        S.ts('dve', rl[:, :], o_src[64:128, :], 0.0, None, ALU.add)
        src = rl[:, :]
    S.op('dve', nc.vector.reciprocal, out=rl[:, :], in_=src, reads=[src], writes=[rl[:, :]])
    S.tt('dve', dst, o_src[0:64, :], rl[:, :], ALU.mult)


def stage_m_setup(self):
    S, nc = self.S, self.nc
    NS, NKT = self.SB // 64, self.SB // 2048
    self.MCAP = 2048
    self.mask_np = np.zeros((128, self.MCAP), np.float32)
    self.mask_idx, self.mask_used = {}, 0
    self.maskbank = self.sb("maskbank", [128, self.MCAP], BF16)
    S.dma('pool', self.maskbank[:, :], self.inp("c_masks", [128, self.MCAP]))
    self.convw = self.sb("convw", [128, 3, 3], F32)
    S.dma('sp', self.convw[:, :, :], self.inp("conv_w_l", [128, 3, 3]))
    self.sinkexp = self.sb("sinkexp", [128, 6], F32)
    S.dma('sp', self.sinkexp[:, :], self.inp("sinks_rep", [128, 6]))
    S.act(self.sinkexp[:, :], self.sinkexp[:, :], AF.Exp)
    self.bmg = self.sb("bmg", [128, 4, 8], F32)
    S.dma('sp', self.bmg[:, :, :], self.inp("b_merge_l", [128, 4, 8]))
    self.pbufs = [self.sb(f"pbuf{i}", [128, 512], BF16) for i in range(4)]
    self._pn = 0
    self.wbufs = [self.sb(f"wbuf{i}", [128, 8, 384], BF16) for i in range(3)]
    self.oT = self.sb("oT", [64, 18, 512], BF16)
    self.oTb = self.sb("oTb", [128, 3, 512], BF16)
    self.xw = self.sb("xw", [128, 8, 1536], BF16)
    self.arb = self.sb("arb", [128, 29184], BF16)
    self.arf = self.sb("arf", [128, 6144], F32)
    self.load_lnp(0)
    self.rlb = [self.sb(f"rlb{i}", [64, 512], F32) for i in range(2)]
    if 2 in self.mixers:
        self.Eq = self.sb("Eq", [128, 32, 128], BF16)
        S.dma('pool', self.Eq[:, :, :], self.inp("c_Eq", [128, 32, 128]))
        self.ovl = self.sb("ovl", [128, NKT, NS + 1], BF16)
        S.dma('pool', self.ovl[:, :, :], self.inp("c_ovl", [128, NKT, NS + 1]))
        self.dqk = self.sb("dqk", [128, 2, 512], F32)
        S.dma('sp', self.dqk[:, :, :], self.inp("c_dqk", [128, 2, 512]))
        self.thr = self.sb("thr", [128, 18], F32)
        S.dma('sp', self.thr[:, :], self.inp("pc_thr", [128, 18]))
        self.negselT = self.sb("negselT", [64, 2, self.NS // 64, 512], BF16)
        self.ibias = self.sb("ibias", [128, 4, NS], F32)
        self.selw = self.sb("selw", [128, NS], F32)
        self.m8 = self.sb("m8", [128, 24], F32)


def stage_k(self):
    from contextlib import ExitStack
    S, nc = self.S, self.nc
    SB, NKT = self.SB, self.NKT
    xTf = self.inp("xT_full", [1024, SB])
    self.kslc_d = self.scratch("kslc_d", [2, 64, SB], BF16)
    self.vslc_d = self.scratch("vslc_d", [SB, 128], BF16)
    kcmp_d = self.scratch("kcmp_d", [4, 64, SB + 32], BF16)
    w1_d = self.inp("cmp_w1_l", [64, 2, 32, 128])
    pos_d = self.inp("cmp_pos_l", [64, 2, 32])
    b1_d = self.inp("cmp_b1_l", [128, 2])
    w2_d = self.inp("cmp_w2_l", [128, 2, 64])
    b2k_d = self.inp("cmp_b2k_l", [64, 1])
    b2v_d = self.inp("cmp_b2v_l", [1, 64])
    self.stk = ExitStack()
    wk = self.sb("k_w", [128, 8, 512], BF16)
    S.dma('pool', wk[:, :, 0:384], self.w_in_d[:, 64 * U_KCC:64 * U_KCC + 384].rearrange("(k p) n -> p k n", p=128))
    S.dma('pool', wk[:, :, 384:512], self.w_in_d[:, C_VSC:C_VSC + 128].rearrange("(k p) n -> p k n", p=128))
    xp = [self.sb(f"k_xp{i}", [128, 8, 512], BF16) for i in range(2)]
    ut = [self.sb(f"k_ut{i}", [64, 6, 512], BF16) for i in range(2)]
    vt = [self.sb(f"k_vt{i}", [128, 4, 128], BF16) for i in range(2)]
    zt = self.sb("k_zero", [64, 32], BF16)
    S.memset('pool', zt[:, :], 0.0)
    for s in range(4):
        S.dma('sp', kcmp_d[s, :, SB:SB + 32], zt[:, :])
    for j in range(SB // 512):
        x_ = xp[j % 2]
        S.dma('pool', x_[:, :, :], xTf[:, 512 * j:512 * j + 512].rearrange("(k p) n -> p k n", p=128))
        u_ = ut[j % 2]
        for u in range(6):
            proj_fm(self, wk, 64 * u, u_[:, u, :], lambda k, c0, n: x_[:, k, c0:c0 + n], 512)
        for s, u in enumerate((0, 1, 4, 5)):
            S.dma('sp', kcmp_d[s, :, 512 * j:512 * j + 512], u_[:, u, :])
        for kv in range(2):
            S.dma('sp', self.kslc_d[kv, :, 512 * j:512 * j + 512], u_[:, 2 + kv, :])
        v_ = vt[j % 2]
        for t in range(4):
            ps = self.ps.bank('pj', [6, 7])
            for k in range(8):
                S.mm(ps[:, 0:128], x_[:, k, 128 * t:128 * t + 128], wk[:, k, 384:512], start=(k == 0), stop=(k == 7))
            evac(self, v_[:, t, :], ps[:, 0:128])
        S.dma('sp', self.vslc_d[512 * j:512 * j + 512, :].rearrange("(t p) c -> p t c", p=128), v_[:, :, :])
    w1 = self.sb("k_w1", [64, 2, 32, 128], BF16)
    S.dma('pool', w1[:, :, :, :], w1_d)
    pos = self.sb("k_pos", [64, 2, 32], BF16)
    S.dma('pool', pos[:, :, :], pos_d)
    b1 = self.sb("k_b1", [128, 2], F32)
    S.dma('sp', b1[:, :], b1_d)
    w2 = self.sb("k_w2", [128, 2, 64], BF16)
    S.dma('pool', w2[:, :, :], w2_d)
    b2k = self.sb("k_b2k", [64, 1], F32)
    S.dma('sp', b2k[:, :], b2k_d)
    b2v = self.sb("k_b2v", [1, 64], BF16)
    S.dma('pool', b2v[:, :], b2v_d)
    beff = self.sb("k_beff", [128, 2], F32)
    for wh in range(2):
        ps = self.ps.bank('pj', [6, 7])
        for p_ in range(32):
            S.mm(ps[:, 0:1], w1[:, wh, p_, :], pos[:, wh, p_:p_ + 1], start=(p_ == 0), stop=(p_ == 31))
        S.tt('dve', beff[:, wh:wh + 1], ps[:, 0:1], b1[:, wh:wh + 1], ALU.add)
    kk = [self.sb(f"k_kk{i}", [64, 8192 + 32], BF16) for i in range(2)]
    hx = self.sb("k_hx", [128, 512], F32)
    hu = self.sb("k_hu", [128, 512], F32)
    hT = self.sb("k_hT", [128, 512], BF16)
    S.memset('pool', self.vc[:, :, :, 64:128], 1.0)
    it = 0
    for wh in range(2):
        for kv in range(2):
            s = wh * 2 + kv
            for bg in range((NKT * 128 + 511) // 512):
                nb = min(512, NKT * 128 - 512 * bg)
                k_ = kk[it % 2]
                it += 1
                ntok = nb * 16 + 16
                S.dma('sp', k_[:, 0:ntok], kcmp_d[s, :, 8192 * bg:8192 * bg + ntok])
                hp = self.ps.bank('sc', [0, 1, 2])
                for p_ in range(32):
                    S.mm(hp[:, 0:nb], w1[:, wh, p_, :], k_[:, p_:p_ + 16 * nb:16], start=(p_ == 0), stop=(p_ == 31))
                S.act(hx[:, 0:nb], hp[:, 0:nb], AF.Identity, bias=beff[:, wh:wh + 1])
                S.tt('dve', hu[:, 0:nb], hx[:, 0:nb], hx[:, 0:nb], ALU.mult)
                S.ts('dve', hu[:, 0:nb], hu[:, 0:nb], 0.044715, 1.0, ALU.mult, ALU.add)
                S.tt('dve', hu[:, 0:nb], hu[:, 0:nb], hx[:, 0:nb], ALU.mult)
                S.act(hu[:, 0:nb], hu[:, 0:nb], AF.Sigmoid, scale=1.5957691216057308)
                S.tt('dve', hT[:, 0:nb], hx[:, 0:nb], hu[:, 0:nb], ALU.mult)
                if wh == 0:
                    kp = self.ps.bank('pj', [6, 7])
                    S.mm(kp[0:64, 0:nb], w2[:, 0, :], hT[:, 0:nb])
                    S.act(self.kcT[:, kv, 512 * bg:512 * bg + nb], kp[0:64, 0:nb], AF.Identity, bias=b2k[:, 0:1])
                else:
                    for t in range(nb // 128):
                        vp = self.ps.bank('pj', [6, 7])
                        S.mm(vp[:, 0:64], hT[:, 128 * t:128 * t + 128], w2[:, 1, :], start=True, stop=False)
                        S.mm(vp[:, 0:64], self.ones_bf[0:1, 0:128], b2v[0:1, :], start=False, stop=True)
                        evac(self, self.vc[:, bg * 4 + t, kv, 0:64], vp[:, 0:64])
    barrier(self)
    self.stk.close()
    self.stk = None


Prog.sb = _sb
Prog.stage_k = stage_k
Prog.stage_m_setup = stage_m_setup


def stage_m(self, x_own, xmid_d, xmT_d):
    from contextlib import ExitStack
    S, nc = self.S, self.nc
    NM, NS, NKT, NH = self.NM, self.SB // 64, self.SB // 2048, (self.SB // 64 + 127) // 128
    mix = self.mixers
    self.stk = ExitStack()
    self.NS, self.NKT, self.NH = NS, NKT, NH
    self.w_in_d = self.inp("w_in_u", [1024, NCOLS])
    if 2 in mix:
        self.kcT = self.sb("kcT", [64, 2, NKT * 128], BF16)
        self.vc = self.sb("vc", [128, NKT, 2, 128], BF16)
        stk_m = self.stk
        stage_k(self)
        self.stk = stk_m
    stage_m_setup(self)
    xTw_d = self.inp("xT_win", [NM, 1024, WIN])
    pad_d = self.inp("padrow", [NM, 1, WIN])
    wg_d = self.inp("w_mgate_l", [8, 128, 4, 8, 128])
    wb_d = self.inp("w_branch_l", [8, 64, 18, 128])
    wbc_d = self.inp("w_branchc_l", [8, 128, 3, 128])
    wout_d = self.inp("w_out_l", [1024, 1024])
    if 2 in mix:
        ibias_d = self.inp("pc_impbias", [NM, 128, 4, NS])
    arb, arf, xw = self.arb, self.arf, self.xw
    o = 0
    qbuf, o = carve(arb, o, 0, 65, (6, 512))
    kbuf, o = carve(arb, o, 0, 65, (2, WIN))
    vbuf, o = carve(arb, o, 0, 128, (32, 2, 128))
    o_x = o
    xh, _ = carve(arb, o_x, 0, 128, (8, 1024))
    gbuf, o = carve(arb, o, 0, 64, (9, 512))
    ET, _ = carve(arb, o, 0, 128, (8, 512))
    kslab = []
    vslab = []
    for i in range(2):
        a, o = carve(arb, o, 0, 64, (2048,))
        kslab.append(a)
    for i in range(2):
        a, o = carve(arb, o, 0, 128, (16, 2, 64))
        vslab.append(a)
    assert o <= 29184, o
    f = 0
    Oacc, f = carve(arf, f, 0, 128, (6, 512))
    onsa, _ = carve(arf, 0, 0, 64, (6, 512))
    cva, _ = carve(arf, 0, 0, 128, (514,))
    cvb, _ = carve(arf, 514, 0, 128, (514,))
    imp, f = carve(arf, f, 0, 128, (4, 2, NS))
    tmpn = []
    for i in range(2):
        a, f = carve(arf, f, 0, 64, (512,))
        tmpn.append(a)
    assert f <= 6144, f
    o = 0
    wgb, wbb, wbcb = [], [], []
    for i in range(2):
        a, o = carve(arb, o, 0, 128, (4, 8, 128))
        wgb.append(a)
    for i in range(2):
        a, o = carve(arb, o, 0, 64, (18, 128))
        wbb.append(a)
    for i in range(2):
        a, o = carve(arb, o, 0, 128, (3, 128))
        wbcb.append(a)
    mergedT, o = carve(arb, o, 0, 128, (8, 512))
    wout, o = carve(arb, o, 0, 128, (8, 1024))
    assert o <= 27136, o
    f = 0
    gsb, f = carve(arf, f, 0, 128, (512,))
    macc, f = carve(arf, f, 0, 128, (512,))
    mtmp, f = carve(arf, f, 0, 128, (512,))
    tls = []
    for i in range(4):
        a, f = carve(arf, f, 0, 128, (1024,))
        tls.append(a)
    self.tl_x, self.tl_s, self.tl_xm = tls[0:1], tls[1:2], tls[2:4]
    self.tl_xmt = [self.sb(f"tl_xmt{i}", [128, 8, 128], BF16) for i in range(2)]
    assert f <= 6144, f

    def xcol(k, a, b, step=1):
        if a >= 1024:
            return xw[:, k, a - 1024:b - 1024:step]
        assert b <= 1024 + step - 1, (a, b)
        return xh[:, k, a:min(b, 1024):step]

    xrhs = lambda k, c0, n: xcol(k, 2048 + c0, 2048 + c0 + n)

    for m in range(NM):
        S.dma('pool', xw[:, :, :], xTw_d[m][:, 1024:WIN].rearrange("(k p) n -> p k n", p=128))
        for j in range(2):
            S.dma('pool', kbuf[64:65, j, :], pad_d[m])
        S.memset('pool', qbuf[64:65, :, :], 1.0)
        S.memset('pool', vbuf[:, :, :, 64:128], 1.0)
        for i in range(2):
            S.memset('pool', vslab[i][:, :, 1, :], 1.0)
        groups = []
        if 1 in mix:
            groups += [(C_CONV, 384), (C_CONV + 384, 384), (C_CONV + 768, 384)]
        if 0 in mix:
            for g in range(3):
                groups += [(64 * (U_QA + 6 * g), 384), (64 * (U_KA + 6 * g), 384), (C_VA + 384 * g, 384)]
        if 3 in mix:
            groups += [(64 * U_QD, 384), (64 * U_KD, 128), (C_VD, 128)]
        if 2 in mix:
            groups += [(64 * U_QC, 384), (64 * U_KWC, 128), (C_VWC, 128)]
            for kv in range(2):
                groups += [(64 * (U_GN + 9 * kv), 384), (64 * (U_GN + 9 * kv + 6), 192)]
        wplan(self, groups)

        if 1 in mix:
            wgb_ = wget(self, C_CONV, 384)
            for c in range(3):
                p3 = self.ps.bank('pj', [6, 7])
                for k in range(8):
                    S.mm(p3[:, :], wgb_[:, k, 128 * c:128 * c + 128], xcol(k, 2048, 2560), start=(k == 0), stop=(k == 7))
                evac(self, self.oTb[:, c, :], p3[:, :])
            wgc_ = wget(self, C_CONV + 384, 384)
            whb = wget(self, C_CONV + 768, 384)
            for c in range(3):
                u = cva
                for (c0, n, dcol) in ((2046, 2, 0), (2048, 512, 2)):
                    p1 = self.ps.bank('pj', [6, 7])
                    p2 = self.ps.bank('pj', [6, 7])
                    for k in range(8):
                        S.mm(p1[:, 0:n], wgc_[:, k, 128 * c:128 * c + 128], xcol(k, c0, c0 + n), start=(k == 0), stop=(k == 7))
                    for k in range(8):
                        S.mm(p2[:, 0:n], whb[:, k, 128 * c:128 * c + 128], xcol(k, c0, c0 + n), start=(k == 0), stop=(k == 7))
                    S.copy('act', cvb[:, dcol:dcol + n], p1[:, 0:n])
                    S.tt('dve', u[:, dcol:dcol + n], cvb[:, dcol:dcol + n], p2[:, 0:n], ALU.mult)
                S.ts('dve', cvb[:, 0:512], u[:, 0:512], self.convw[:, c, 0:1], None, ALU.mult)
                S.stt('dve', cvb[:, 0:512], u[:, 1:513], self.convw[:, c, 1:2], cvb[:, 0:512], ALU.mult, ALU.add)
                S.stt('dve', cvb[:, 0:512], u[:, 2:514], self.convw[:, c, 2:3], cvb[:, 0:512], ALU.mult, ALU.add)
                S.tt('pool', self.oTb[:, c, :], cvb[:, 0:512], self.oTb[:, c, :], ALU.mult)

        if 0 in mix:
            for g in range(3):
                D = DILS[g][1]
                lo = 2048 - DILS[g][0]
                nq = 512 // D
                uq0 = 2048 // D
                uk0 = uq0 - 128
                nk = 128 + nq
                ntile = (nk + 127) // 128
                if lo < 1024:
                    S.dma('pool', xh[:, :, :], xTw_d[m][:, 0:1024].rearrange("(k p) n -> p k n", p=128))
                wq = wget(self, 64 * (U_QA + 6 * g), 384)
                for h in range(6):
                    proj_fm(self, wq, 64 * h, qbuf[:, h, :], xrhs, 512)
                wk = wget(self, 64 * (U_KA + 6 * g), 384)
                wv = wget(self, C_VA + 384 * g, 384)
                for hp in range(3):
                    for j in range(2):
                        proj_fm(self, wk, 64 * (2 * hp + j), kbuf[:, j, lo:WIN],
                                lambda k, c0, n: xcol(k, lo + c0, lo + c0 + n), WIN - lo)
                    for r in range(D):
                        for i in range(ntile):
                            nki = min(128, nk - 128 * i)
                            st = r + D * (uk0 + 128 * i)
                            vp = self.ps.bank('pj', [6, 7])
                            en = st + D * (nki - 1) + 1
                            if st < 1024 < en:
                                n1 = (1024 - st + D - 1) // D
                                parts = [(0, n1, st, st + D * (n1 - 1) + 1), (n1, nki, st + D * n1, en)]
                            else:
                                parts = [(0, nki, st, en)]
                            for (r0, r1, a_, b_) in parts:
                                assert r0 in (0, 32, 64), r0
                                for k in range(8):
                                    S.mm(vp[r0:r1, 0:128], xcol(k, a_, b_, D), wv[:, k, 128 * hp:128 * hp + 128],
                                         start=(k == 0), stop=(k == 7))
                            evac(self, vbuf[0:nki, r * ntile + i, :, 0:64], vp[0:nki, 0:128].rearrange("p (a b) -> p a b", a=2))
                    for j in range(2):
                        h = 2 * hp + j
                        o_ps = self.ps.bank('o', [3, 4, 5])
                        first = [True]
                        for r in range(D):
                            band_attn(self,
                                      lambda c0, c1: o_ps[:, r + D * c0:r + D * (c1 - 1) + 1:D],
                                      qbuf[:, h, r:512:D], nq, uq0,
                                      lambda i, n: kbuf[:, j, r + D * (uk0 + 128 * i):r + D * (uk0 + 128 * i) + D * (n - 1) + 1:D],
                                      lambda i, n: vsel(vbuf[0:n, r * ntile + i, :, :], j, 2),
                                      uk0, nk, 128, first)
                        pv_flush(self)
                        if g == 0:
                            S.copy('act', Oacc[:, h, :], o_ps)
                        else:
                            S.tt('dve', Oacc[:, h, :], Oacc[:, h, :], o_ps, ALU.add)
            for h in range(6):
                normalize(self, Oacc[:, h, :], self.oT[:, h, :])

        if 3 in mix:
            lo = 2048 - 128
            wq = wget(self, 64 * U_QD, 384)
            for h in range(6):
                proj_fm(self, wq, 64 * h, qbuf[:, h, :], xrhs, 512)
            wk = wget(self, 64 * U_KD, 128)
            for j in range(2):
                proj_fm(self, wk, 64 * j, kbuf[:, j, lo:WIN], lambda k, c0, n: xcol(k, lo + c0, lo + c0 + n), WIN - lo)
            wv = wget(self, C_VD, 128)
            for i in range(5):
                vp = self.ps.bank('pj', [6, 7])
                for k in range(8):
                    S.mm(vp[:, 0:128], xcol(k, lo + 128 * i, lo + 128 * i + 128), wv[:, k, 0:128], start=(k == 0), stop=(k == 7))
                evac(self, vbuf[:, i, :, 0:64], vp[:, 0:128].rearrange("p (a b) -> p a b", a=2))
            for h in range(6):
                kv = h // 3
                o_ps = self.ps.bank('o', [3, 4, 5])
                band_attn(self, lambda c0, c1: o_ps[:, c0:c1], qbuf[:, h, :], 512, 2048,
                          lambda i, n: kbuf[:, kv, lo + 128 * i:lo + 128 * i + n],
                          lambda i, n: vsel(vbuf[0:n, i, :, :], kv, 2), lo, 640, 127, [True])
                pv_flush(self)
                normalize(self, o_ps, self.oT[:, 12 + h, :], extra=self.sinkexp[64:128, h:h + 1])

        if 2 in mix:
            nsa_chunk(self, m, locals())

        if getattr(self, 'debug', False) and m == NM - 1:
            dbg = self.outp("dbg_oT", [64, 18, 512], BF16)
            S.dma('sp', dbg, self.oT[:, :, :])
        S.dma('pool', wout[:, :, :], wout_d.rearrange("(k p) n -> p k n", p=128))
        def load_fc(fc_):
            b_ = fc_ % 2
            S.dma('pool', wgb[b_][:, :, :, :], wg_d[fc_])
            S.dma('pool', wbb[b_][:, :, :], wb_d[fc_])
            S.dma('pool', wbcb[b_][:, :, :], wbc_d[fc_])
        load_fc(0)
        for fc in range(8):
            b = fc % 2
            if fc + 1 < 8:
                load_fc(fc + 1)
            nmix = len(mix)
            for idx, mi in enumerate(mix):
                bp = self.ps.bank('sc', [0, 1, 2])
                if mi == 1:
                    for c in range(3):
                        S.mm(bp[:, :], wbcb[b][:, c, :], self.oTb[:, c, :], start=(c == 0), stop=(c == 2))
                else:
                    sl = {0: 0, 2: 6, 3: 12}[mi]
                    for h in range(6):
                        S.mm(bp[:, :], wbb[b][:, sl + h, :], self.oT[:, sl + h, :], start=(h == 0), stop=(h == 5))
                gp = self.ps.bank('o', [3, 4, 5])
                for k in range(8):
                    S.mm(gp[:, :], wgb[b][:, mi, k, :], xcol(k, 2048, 2560), start=(k == 0), stop=(k == 7))
                S.act(gsb[:, :], gp[:, :], AF.Sigmoid, bias=self.bmg[:, mi, fc:fc + 1])
                last = (idx == nmix - 1)
                if idx == 0:
                    S.tt('dve', mergedT[:, fc, :] if last else macc[:, :], gsb[:, :], bp[:, :], ALU.mult)
                else:
                    S.tt('dve', mtmp[:, :], gsb[:, :], bp[:, :], ALU.mult)
                    S.tt('pool', mergedT[:, fc, :] if last else macc[:, :], macc[:, :], mtmp[:, :], ALU.add)
        self.mix_tail(m, mergedT, wout, xmid_d, xmT_d, x_own)
    barrier(self)
    self.stk.close()
    self.stk = None


Prog.stage_m = stage_m


_PERM = win_perm()


def prep_mixer_input(prog, name, layer, xl, inp, b, qt):
    NM, SB = prog.NM, prog.SB
    NS = SB // 64
    f32 = np.float32
    if name == "w_in_u":
        return np.ascontiguousarray(inp['w_in'][layer][:, _PERM])
    if name == "xT_win":
        out = np.zeros((NM, 1024, WIN), f32)
        for m in range(NM):
            t0 = 2048 * m + 512 * qt
            lo = t0 - 2048
            a = max(lo, 0)
            out[m, :, a - lo:] = xl[b, a:t0 + 512, :].T
        return out
    if name == "padrow":
        out = np.zeros((NM, 1, WIN), f32)
        for m in range(NM):
            pos = 2048 * m + 512 * qt - 2048 + np.arange(WIN)
            out[m, 0, pos < 0] = NEG
        return out
    if name == "xT_full":
        return np.ascontiguousarray(xl[b].T)
    if name == "conv_w_l":
        return np.ascontiguousarray(inp['conv_w'][layer].reshape(3, 3, 128).transpose(2, 1, 0))
    if name == "sinks_rep":
        return np.ascontiguousarray(np.broadcast_to(inp['sinks'][layer][None, :], (128, 6)))
    if name == "b_merge_l":
        return np.ascontiguousarray(inp['b_merge_gate'][layer].reshape(4, 8, 128).transpose(2, 0, 1))
    if name == "w_mgate_l":
        w = inp['w_merge_gate'][layer].reshape(4, 8, 128, 8, 128)
        return np.ascontiguousarray(w.transpose(3, 2, 0, 1, 4))
    if name == "w_branch_l":
        w = inp['w_branch'][layer][[0, 2, 3]].reshape(3, 6, 64, 8, 128)
        return np.ascontiguousarray(w.transpose(3, 2, 0, 1, 4).reshape(8, 64, 18, 128))
    if name == "w_branchc_l":
        w = inp['w_branch'][layer][1].reshape(3, 128, 8, 128)
        return np.ascontiguousarray(w.transpose(2, 1, 0, 3))
    if name == "w_out_l":
        return np.ascontiguousarray(inp['w_out'][layer])
    if name == "cmp_w1_l":
        w = inp['cmp_w1'][layer].reshape(2, 32, 64, 128)
        return np.ascontiguousarray(w.transpose(2, 0, 1, 3))
    if name == "cmp_pos_l":
        return np.ascontiguousarray(inp['cmp_pos'][layer].transpose(2, 0, 1))
    if name == "cmp_b1_l":
        return np.ascontiguousarray(inp['cmp_b1'][layer].T)
    if name == "cmp_w2_l":
        return np.ascontiguousarray(inp['cmp_w2'][layer].transpose(1, 0, 2))
    if name == "cmp_b2k_l":
        return np.ascontiguousarray(inp['cmp_b2'][layer][0][:, None])
    if name == "cmp_b2v_l":
        return np.ascontiguousarray(inp['cmp_b2'][layer][1][None, :])
    if name == "pc_thr":
        row = np.zeros(18, f32)
        row[0:16] = 128 * np.arange(16) - 512 * qt
        row[16] = -2048 + 31 - 512 * qt
        row[17] = 31 - 512 * qt
        return np.ascontiguousarray(np.broadcast_to(row[None, :], (128, 18)))
    if name == "pc_impbias":
        out = np.zeros((NM, 128, 4, NS), f32)
        jj = np.arange(NS)[None, :]
        for m in range(NM):
            for qi in range(4):
                t = 2048 * m + 512 * qt + 128 * qi + np.arange(128)
                cur = (t // 64)[:, None]
                forced = (jj == 0) | (jj == cur) | (jj == cur - 1)
                out[m, :, qi, :] = np.where(forced, BIGB, np.where(jj <= cur, 0.0, -BIGB))
        return out
    raise KeyError(name)


def make_mixer_consts(prog):
    c = {}
    if not prog.mixers:
        return c
    c["c_masks"] = prog.mask_np
    if 2 in prog.mixers:
        NS, NKT = prog.SB // 64, prog.SB // 2048
        p = np.arange(128)[:, None, None]
        r = np.arange(16)[None, :, None]
        j = np.arange(128)[None, None, :]
        e1 = ((p % 32) == 2 * r + (j >= 64)).astype(np.float32)
        e2 = e1 * ((p % 64) >= 32)
        c["c_Eq"] = np.ascontiguousarray(np.concatenate([e1, e2], axis=1))
        cc = (np.arange(NKT)[None, :, None] * 128 + np.arange(128)[:, None, None])
        jb = np.arange(NS)[None, None, :]
        ov = ((16 * cc < 64 * jb + 64) & (16 * cc + 31 >= 64 * jb)).astype(np.float32)
        c["c_ovl"] = np.concatenate([ov, np.ones((128, NKT, 1), np.float32)], axis=2)
        q = np.arange(512)[None, :]
        jl = np.arange(128)[:, None]
        c["c_dqk"] = np.ascontiguousarray(np.stack([q - jl, q - 16 * jl], axis=1).astype(np.float32))
    return c


def nsa_chunk(self, m, L):
    S, nc = self.S, self.nc
    NS, NKT, NH = self.NS, self.NKT, self.NH
    qbuf, kbuf, vbuf, gbuf, ET = L['qbuf'], L['kbuf'], L['vbuf'], L['gbuf'], L['ET']
    kslab, vslab, onsa, imp, tmpn = L['kslab'], L['vslab'], L['onsa'], L['imp'], L['tmpn']
    xcol, xrhs, ibias_d = L['xcol'], L['xrhs'], L['ibias_d']
    lo = 2048 - 512
    S.dma('sp', self.ibias[:, :, :], ibias_d[m])
    wq = wget(self, 64 * U_QC, 384)
    for h in range(6):
        proj_fm(self, wq, 64 * h, qbuf[:, h, :], xrhs, 512)
    wk = wget(self, 64 * U_KWC, 128)
    for j in range(2):
        proj_fm(self, wk, 64 * j, kbuf[:, j, lo:WIN], lambda k, c0, n: xcol(k, lo + c0, lo + c0 + n), WIN - lo)
    wv = wget(self, C_VWC, 128)
    for i in range(8):
        vp = self.ps.bank('pj', [6, 7])
        for k in range(8):
            S.mm(vp[:, 0:128], xcol(k, lo + 128 * i, lo + 128 * i + 128), wv[:, k, 0:128], start=(k == 0), stop=(k == 7))
        evac(self, vbuf[:, i, :, 0:64], vp[:, 0:128].rearrange("p (a b) -> p a b", a=2))
    nkt = min(m + 1, NKT)
    for kv in range(2):
        wga = wget(self, 64 * (U_GN + 9 * kv), 384)
        wgb_ = wget(self, 64 * (U_GN + 9 * kv + 6), 192)
        for u in range(9):
            w_, wc = (wga, 64 * u) if u < 6 else (wgb_, 64 * (u - 6))
            ps = self.ps.bank('pj', [6, 7])
            for k in range(8):
                S.mm(ps[0:64, :], w_[:, k, wc:wc + 64], xcol(k, 2048, 2560), start=(k == 0), stop=(k == 7))
            S.act(gbuf[:, u, :], ps[0:64, :], AF.Sigmoid)
        for hl in range(3):
            hq = 3 * kv + hl
            o_ps = self.ps.bank('o', [3, 4, 5])
            band_attn(self, lambda c0, c1: o_ps[:, c0:c1], qbuf[:, hq, :], 512, 2048,
                      lambda i, n: kbuf[:, kv, lo + 128 * i:lo + 128 * i + n],
                      lambda i, n: vsel(vbuf[0:n, i, :, :], kv, 2), lo, 1024, 511, [True])
            pv_flush(self)
            t_ = tmpn[0]
            normalize(self, o_ps, t_[:, :])
            S.tt('pool', onsa[:, hq, :], t_[:, :], gbuf[:, 3 * hl + 2, :], ALU.mult)
            o_ps = self.ps.bank('o', [3, 4, 5])
            for kt in range(nkt):
                s_ps = self.ps.bank('sc', [0, 1, 2])
                S.mm(s_ps[:, :], self.kcT[:, kv, 128 * kt:128 * kt + 128], qbuf[0:64, hq, :])
                S.act(ET[:, kt, :], s_ps[:, :], AF.Exp, scale=0.125)
                if kt >= m - 1:
                    S.stt('dve', ET[:, kt, :], self.dqk[:, 1, :], self.thr[:, 16 + (kt - (m - 1)):17 + (kt - (m - 1))],
                          ET[:, kt, :], ALU.is_ge, ALU.mult)
                pv_defer(self, lambda o_=o_ps[:, :], v_=self.vc[:, kt, kv, :], p_=ET[:, kt, :], st_=(kt == 0), sp_=(kt == nkt - 1):
                         S.mm(o_, v_, p_, start=st_, stop=sp_))
            pv_flush(self)
            t_ = tmpn[1]
            normalize(self, o_ps, t_[:, :], clamp=True)
            S.tt('pool', t_[:, :], t_[:, :], gbuf[:, 3 * hl + 0, :], ALU.mult)
            S.tt('pool', onsa[:, hq, :], onsa[:, hq, :], t_[:, :], ALU.add)
            for qi in range(4):
                ip = self.ps.bank('pj', [6, 7])
                for kt in range(nkt):
                    S.mm(ip[:, 0:NS + 1], ET[:, kt, 128 * qi:128 * qi + 128], self.ovl[:, kt, :], start=(kt == 0), stop=(kt == nkt - 1))
                rd = self.m8[:, 16 + qi:17 + qi]
                S.ts('dve', rd, ip[:, NS:NS + 1], 1e-30, None, ALU.max)
                S.op('dve', nc.vector.reciprocal, out=rd, in_=rd, reads=[rd], writes=[rd])
                if hl == 0:
                    S.ts('dve', imp[:, qi, kv, :], ip[:, 0:NS], rd, None, ALU.mult)
                else:
                    S.stt('dve', imp[:, qi, kv, :], ip[:, 0:NS], rd, imp[:, qi, kv, :], ALU.mult, ALU.add)
        for qi in range(4):
            ib = imp[:, qi, kv, :]
            S.tt('dve', ib, ib, self.ibias[:, qi, :], ALU.add)
            S.op('dve', nc.vector.max, out=self.m8[:, 0:8], in_=ib, reads=[ib], writes=[self.m8[:, 0:8]])
            S.op('dve', nc.vector.match_replace, out=self.selw[:, :], in_to_replace=self.m8[:, 0:8], in_values=ib,
                 imm_value=-3.0 * BIGB, reads=[self.m8[:, 0:8], ib], writes=[self.selw[:, :]])
            S.op('dve', nc.vector.max, out=self.m8[:, 8:16], in_=self.selw[:, :], reads=[self.selw[:, :]], writes=[self.m8[:, 8:16]])
            S.ts('dve', self.m8[:, 20:21], self.m8[:, 15:16], -0.5 * BIGB, None, ALU.max)
            S.ts('dve', self.selw[:, :], ib, self.m8[:, 20:21], None, ALU.is_ge)
            S.ts('dve', self.selw[:, :], self.selw[:, :], -1.0, -NEG, ALU.add, ALU.mult)
            for quarter in range(NS // 64):
                tp = self.ps.bank('pj', [6, 7])
                S.mm(tp[0:64, 0:128], self.selw[:, 64 * quarter:64 * quarter + 64], self.ident[:, :])
                evac(self, self.negselT[:, kv, quarter, 128 * qi:128 * qi + 128], tp[0:64, 0:128])
        obanks = [self.ps.t[:, (3 + hl) * 512:(4 + hl) * 512] for hl in range(3)]
        nslab = m + 1
        for i in range(nslab):
            ks, vs = kslab[i % 2], vslab[i % 2]
            S.dma('sp', ks[:, :], self.kslc_d[kv, :, 2048 * i:2048 * i + 2048])
            S.dma('sp', vs[:, :, 0, :], self.vslc_d[2048 * i:2048 * i + 2048, 64 * kv:64 * kv + 64].rearrange("(t p) c -> p t c", p=128))
            for hl in range(3):
                hq = 3 * kv + hl
                for t in range(16):
                    kt = 16 * i + t
                    row = (2 * kt) % 64
                    quarter = (2 * kt) // 64
                    q32, r = row // 32, (row % 32) // 2
                    s_ps = self.ps.bank('sc', [0, 1, 2])
                    S.mm(s_ps[:, :], ks[:, 128 * t:128 * t + 128], qbuf[0:64, hq, :], start=True, stop=False)
                    S.mm(s_ps[:, :], self.Eq[32 * q32:32 * q32 + 32, r, :], self.negselT[32 * q32:32 * q32 + 32, kv, quarter, :],
                         start=False, stop=True)
                    p = self.pbufs[self._pn % len(self.pbufs)]
                    self._pn += 1
                    S.act(p[:, :], s_ps[:, :], AF.Exp, scale=0.125)
                    if i == m:
                        S.stt('dve', p[:, :], self.dqk[:, 0, :], self.thr[:, t:t + 1], p[:, :], ALU.is_ge, ALU.mult)
                    pv_defer(self, lambda o_=obanks[hl], v_=vs[:, t, :, :].rearrange("p a b -> p (a b)"), p_=p[:, :], st_=(kt == 0), sp_=(kt == 16 * nslab - 1):
                             S.mm(o_, v_, p_, start=st_, stop=sp_))
        pv_flush(self)
        for hl in range(3):
            hq = 3 * kv + hl
            t_ = tmpn[hl % 2]
            normalize(self, obanks[hl], t_[:, :])
            S.tt('pool', t_[:, :], t_[:, :], gbuf[:, 3 * hl + 1, :], ALU.mult)
            S.tt('pool', self.oT[:, 6 + hq, :], onsa[:, hq, :], t_[:, :], ALU.add)
```

```python
import numpy as np
import ml_dtypes
import concourse.bass as bass
import concourse.mybir as mybir
from concourse.bass_utils import run_bass_kernel_spmd

F32 = mybir.dt.float32
BF16 = mybir.dt.bfloat16
I32 = mybir.dt.int32
AF = mybir.ActivationFunctionType
ALU = mybir.AluOpType
AX = mybir.AxisListType
_DSZ = {F32: 4, BF16: 2, I32: 4}

D_MODEL = 1024
HD = 64
ALPHA = 4 ** 0.25
LN_EPS = 1e-5
D_FF = 2816
D_FFE = 3584
NEXP = 8
NEG = -30000.0


class Sched:
    def __init__(self, nc, n_dma_sems=24):
        self.nc = nc
        self.e = dict(pe=nc.tensor, act=nc.scalar, dve=nc.vector, pool=nc.gpsimd, sp=nc.sync)
        self.sem = {k: nc.alloc_semaphore(name=f"sem_{k}") for k in ('pe', 'act', 'dve', 'pool')}
        self.cnt = {k: 0 for k in self.sem}
        self.dsem = [nc.alloc_semaphore(name=f"dsem{i}") for i in range(n_dma_sems)]
        self.dcnt = [0] * n_dma_sems
        self.dnext = 0
        self.waited = {k: {} for k in self.e}
        self.acc = {}
        self.nins = 0

    @staticmethod
    def box(ap):
        dims = ap.ap
        name = ap.tensor.name
        sz = _DSZ.get(ap.dtype, 4)
        if str(ap.space) == 'DRAM':
            lo = ap.offset
            hi = lo + sum((c - 1) * abs(s) for s, c in dims) + 1
            return name, (0, 1, lo * sz, hi * sz)
        pstep, pcount = dims[0]
        sp = ap.start_partition()
        flo = ap.offset - sp * pstep
        fhi = flo + sum((c - 1) * abs(s) for s, c in dims[1:]) + 1
        return name, (sp, sp + pcount, flo * sz, fhi * sz)

    @staticmethod
    def _ov(a, b):
        return a[0] < b[1] and b[0] < a[1] and a[2] < b[3] and b[2] < a[3]

    @staticmethod
    def _contains(a, b):
        return a[0] <= b[0] and b[1] <= a[1] and a[2] <= b[2] and b[3] <= a[3]

    def _deps(self, boxes):
        toks = set()
        for name, bx, isw in boxes:
            for (rbx, risw, rtok) in self.acc.get(name, ()):
                if (isw or risw) and self._ov(bx, rbx):
                    toks.add(rtok)
        return toks

    def _record(self, boxes, tok):
        for name, bx, isw in boxes:
            lst = self.acc.setdefault(name, [])
            if isw:
                lst[:] = [r for r in lst if not self._contains(bx, r[0])]
            else:
                lst[:] = [r for r in lst if not (not r[1] and r[2][0] == tok[0] and self._contains(bx, r[0]))]
            lst.append((bx, isw, tok))

    def _wait(self, eng, tok):
        key, val = tok
        if key == eng and eng == 'pe':
            return
        if self.waited[eng].get(key, 0) >= val:
            return
        self.waited[eng][key] = val
        semobj = self.sem[key] if isinstance(key, str) else self.dsem[key[1]]
        self.e[eng].wait_ge(semobj, val)
        self.nins += 1

    def _boxes(self, reads, writes):
        out = []
        for r in reads:
            if r is None or isinstance(r, (int, float)):
                continue
            n, b = self.box(r)
            out.append((n, b, False))
        for w in writes:
            n, b = self.box(w)
            out.append((n, b, True))
        return out

    def op(self, eng, fn, *args, reads=(), writes=(), **kw):
        boxes = self._boxes(reads, writes)
        for t in sorted(self._deps(boxes), key=lambda t: (str(t[0]), t[1])):
            self._wait(eng, t)
        ins = fn(*args, **kw)
        self.cnt[eng] += 1
        self.nins += 1
        ins.then_inc(self.sem[eng], 1)
        self._record(boxes, (eng, self.cnt[eng]))
        return ins

    def mm(self, out, lhsT, rhs, start=True, stop=True):
        return self.op('pe', self.nc.tensor.matmul, out, lhsT, rhs, start=start, stop=stop,
                       skip_group_check=True, reads=[lhsT, rhs], writes=[out])

    def transpose(self, out, in_, ident):
        return self.op('pe', self.nc.tensor.transpose, out, in_, ident, reads=[in_, ident], writes=[out])

    def act(self, out, in_, func, bias=None, scale=None, accum_out=None):
        kw = {}
        rd = [in_]
        wr = [out]
        if bias is not None:
            kw['bias'] = bias
            rd.append(bias)
        if scale is not None:
            kw['scale'] = scale
            rd.append(scale)
        if accum_out is not None:
            kw['accum_out'] = accum_out
            wr.append(accum_out)
        return self.op('act', self.nc.scalar.activation, out, in_, func, reads=rd, writes=wr, **kw)

    def tt(self, eng, out, in0, in1, op):
        return self.op(eng, self.e[eng].tensor_tensor, out, in0, in1, op, reads=[in0, in1], writes=[out])

    def ts(self, eng, out, in0, s1, s2, op0, op1=None, accum_out=None):
        kw = {}
        wr = [out]
        if op1 is not None:
            kw['op1'] = op1
        if accum_out is not None:
            kw['accum_out'] = accum_out
            wr.append(accum_out)
        return self.op(eng, self.e[eng].tensor_scalar, out, in0, s1, s2, op0, reads=[in0, s1, s2], writes=wr, **kw)

    def stt(self, eng, out, in0, scalar, in1, op0, op1):
        return self.op(eng, self.e[eng].scalar_tensor_tensor, out, in0, scalar, in1, op0, op1,
                       reads=[in0, scalar, in1], writes=[out])

    def copy(self, eng, out, in_):
        if eng == 'act':
            return self.op('act', self.nc.scalar.copy, out, in_, reads=[in_], writes=[out])
        return self.op(eng, self.e[eng].tensor_copy, out, in_, reads=[in_], writes=[out])

    def memset(self, eng, ap, val):
        return self.op(eng, self.e[eng].memset, ap, val, reads=[], writes=[ap])

    def dma(self, q, out, in_):
        boxes = self._boxes([in_], [out])
        toks = self._deps(boxes)
        i = self.dnext
        self.dnext = (self.dnext + 1) % len(self.dsem)
        if self.dcnt[i] > 0:
            toks.add((('d', i), 16 * self.dcnt[i]))
        for t in sorted(toks, key=lambda t: (str(t[0]), t[1])):
            self._wait(q, t)
        ins = self.e[q].dma_start(out=out, in_=in_)
        self.dcnt[i] += 1
        self.nins += 1
        ins.then_inc(self.dsem[i], 16)
        self._record(boxes, (('d', i), 16 * self.dcnt[i]))

    def finish(self):
        for i, c in enumerate(self.dcnt):
            if c:
                self._wait('sp', (('d', i), 16 * c))
        for k, c in self.cnt.items():
            if c:
                self._wait('sp', (k, c))


class PsumPool:
    def __init__(self, nc):
        self.t = nc.alloc_psum_tensor("psum_all", [128, 8 * 512], F32)
        self.rr = {}

    def bank(self, role, banks):
        i = self.rr.get(role, 0)
        self.rr[role] = i + 1
        b = banks[i % len(banks)]
        return self.t[:, b * 512:(b + 1) * 512]

    def wide(self, role, pairs):
        i = self.rr.get(role, 0)
        self.rr[role] = i + 1
        b = pairs[i % len(pairs)]
        return self.t[:, b * 512:(b + 2) * 512]


class Prog:
    def __init__(self, NM, moe, mixers=(0, 1, 2, 3)):
        self.NM = NM
        self.T = NM * 512
        self.SB = NM * 2048
        self.moe = moe
        self.mixers = mixers
        self.nc = bass.Bass("TRN2", target_bir_lowering=False)
        self.S = Sched(self.nc)
        self.ps = PsumPool(self.nc)
        self.din = {}
        self._n = 0

    def inp(self, name, shape, dt=F32):
        t = self.nc.dram_tensor(name, list(shape), dt, kind="ExternalInput")
        self.din[name] = (tuple(shape), dt)
        return t.ap()

    def outp(self, name, shape, dt=F32):
        return self.nc.dram_tensor(name, list(shape), dt, kind="ExternalOutput").ap()

    def scratch(self, name, shape, dt):
        return self.nc.dram_tensor(name, list(shape), dt, kind="Internal").ap()

    def sb(self, name, shape, dt):
        return self.nc.alloc_sbuf_tensor(name, list(shape), dt)

    def layer_norm(self, s, g_bc, b_bc, out_ap, tmp):
        S, nc = self.S, self.nc
        st = tmp['st']
        mv = tmp['mv']
        for j in range(2):
            S.op('dve', nc.vector.bn_stats, out=st[:, j, :], in_=s[:, j * 512:(j + 1) * 512],
                 reads=[s[:, j * 512:(j + 1) * 512]], writes=[st[:, j, :]])
        S.op('dve', nc.vector.bn_aggr, out=mv[:, 0:2], in_=st[:, :, :], reads=[st[:, :, :]], writes=[mv[:, 0:2]])
        S.ts('pool', mv[:, 2:3], mv[:, 1:2], LN_EPS, None, ALU.add)
        S.tt('pool', mv[:, 3:4], mv[:, 2:3], self.neghalf[:, 0:1], ALU.pow)
        S.ts('dve', s[:, :], s[:, :], mv[:, 0:1], mv[:, 3:4], ALU.subtract, ALU.mult)
        S.tt('pool', s[:, :], s[:, :], g_bc[:, :], ALU.mult)
        S.tt('pool', out_ap, s[:, :], b_bc[:, :], ALU.add)

    def setup_common(self):
        S, nc = self.S, self.nc
        self.ident = self.sb("ident", [128, 128], F32)
        S.dma('sp', self.ident[:, :], self.inp("c_ident", [128, 128]))
        self.neghalf = self.sb("neghalf", [128, 1], F32)
        S.memset('pool', self.neghalf[:, :], -0.5)
        self.ones_bf = self.sb("ones_bf", [128, 512], BF16)
        S.memset('pool', self.ones_bf[:, :], 1.0)
        self.lnp_d = self.inp("ln_params", [128, 4, 1024])
        self.lntmp = dict(st=self.sb("ln_st", [128, 2, 6], F32), mv=self.sb("ln_mv", [128, 4], F32))

    def mix_tail(self, m, mergedT, wout, xmid_d, xmT_d, x_own):
        S, nc = self.S, self.nc
        for tt in range(4):
            tok = m * 512 + tt * 128
            xt = self.tl_x[tt % len(self.tl_x)]
            S.dma('sp', xt[:, :], x_own[tok:tok + 128, :])
            s = self.tl_s[tt % len(self.tl_s)]
            if mergedT is not None:
                yps = self.ps.wide('y', [0, 2])
                for half in range(2):
                    for k in range(8):
                        S.mm(yps[:, half * 512:(half + 1) * 512], mergedT[:, k, tt * 128:(tt + 1) * 128],
                             wout[:, k, half * 512:(half + 1) * 512], start=(k == 0), stop=(k == 7))
                S.stt('dve', s[:, :], xt[:, :], ALPHA, yps, ALU.mult, ALU.add)
            else:
                S.ts('dve', s[:, :], xt[:, :], ALPHA, None, ALU.mult)
            xm = self.tl_xm[tt % len(self.tl_xm)]
            self.layer_norm(s, self.lnp[:, 0, :], self.lnp[:, 1, :], xm[:, :], self.lntmp)
            S.dma('sp', xmid_d[tok:tok + 128, :], xm[:, :])
            xmt = self.tl_xmt[tt % len(self.tl_xmt)]
            for half in range(2):
                tp = self.ps.bank('tp', [4, 5])
                for j in range(4):
                    fc = half * 4 + j
                    S.mm(tp[:, j * 128:(j + 1) * 128], xm[:, fc * 128:(fc + 1) * 128], self.ident[:, :])
                S.copy('act', xmt[:, half * 4:(half + 1) * 4, :],
                       tp.rearrange("p (j t) -> p j t", j=4))
            S.dma('sp', xmT_d[:, :, tok:tok + 128], xmt[:, :, :])

    def stage_f(self, xmid_d, xmT_d, y_out):
        S, nc = self.S, self.nc
        T, NT = self.T, self.T // 128
        moe = self.moe
        NE = NEXP if moe else 1
        FF = D_FFE if moe else D_FF
        GW = 256
        NG = FF // GW
        if moe:
            wg_d = self.inp("moe_w_gate", [NEXP, 1024, FF])
            wu_d = self.inp("moe_w_up", [NEXP, 1024, FF])
            wd_d = self.inp("moe_w_down", [NEXP, FF, 1024])
            wr_d = self.inp("w_router", [1024, NEXP])
            br_d = self.inp("b_router", [1, NEXP])
        else:
            wg_d = self.inp("ffn_w_gate", [1, 1024, FF])
            wu_d = self.inp("ffn_w_up", [1, 1024, FF])
            wd_d = self.inp("ffn_w_down", [1, FF, 1024])
        plew_d = self.inp("ple_w", [256, 1024])
        plegw_d = self.inp("ple_gate_w", [1024, 1024])
        plegb_d = self.inp("ple_gate_b", [1, 1024])
        pT_d = self.inp("pT_own", [256, T])

        HT = min(16, NT)
        xmT = self.sb("f_xmT", [128, 8, HT * 128], BF16)
        acc = self.sb("f_acc", [128, HT, 1024], F32)
        comb = self.sb("f_comb", [128, NT, 8], F32)
        wgb = [self.sb(f"f_wg{i}", [128, 8, GW], BF16) for i in range(2)]
        wub = [self.sb(f"f_wu{i}", [128, 8, GW], BF16) for i in range(2)]
        wdb = [self.sb(f"f_wd{i}", [128, GW // 128, 1024], BF16) for i in range(2)]
        hT = [self.sb(f"f_hT{i}", [128, GW // 128, 512], BF16) for i in range(2)]
        sg = [self.sb(f"f_sg{i}", [128, 512], F32) for i in range(2)]
        plegw = self.sb("f_plegw", [128, 8, 1024], BF16)
        plew = self.sb("f_plew", [128, 2, 1024], BF16)
        plegb = self.sb("f_plegb", [1, 1024], BF16)
        pT = self.sb("f_pT", [128, 2, HT * 128], BF16)
        S.dma('pool', plegw[:, :, :], plegw_d.rearrange("(k p) n -> p k n", p=128))
        S.dma('pool', plew[:, :, :], plew_d.rearrange("(k p) n -> p k n", p=128))
        S.dma('pool', plegb[:, :], plegb_d)
        sig = [self.sb(f"f_sig{i}", [128, 1024], F32) for i in range(1)]

        if moe:
            wr = self.sb("f_wr", [128, 8, NEXP], BF16)
            brt = self.sb("f_br", [1, NEXP], BF16)
            S.dma('pool', wr[:, :, :], wr_d.rearrange("(k p) n -> p k n", p=128))
            S.dma('pool', brt[:, :], br_d)
            lg = self.sb("f_lg", [128, 8], F32)
            m8 = self.sb("f_m8", [128, 8], F32)
            ex = self.sb("f_ex", [128, 8], F32)
            msk = self.sb("f_msk", [128, 8], F32)
            den = self.sb("f_den", [128, 2], F32)
        else:
            S.memset('pool', comb[:, :, :], 1.0)

        def router(h0):
            for tl in range(HT):
                t = h0 + tl
                lp = self.ps.bank('rt', [6, 7])
                for k in range(8):
                    S.mm(lp[:, 0:NEXP], xmT[:, k, tl * 128:(tl + 1) * 128], wr[:, k, :], start=(k == 0), stop=False)
                S.mm(lp[:, 0:NEXP], self.ones_bf[0:1, 0:128], brt[0:1, :], start=False, stop=True)
                S.copy('dve', lg[:, :], lp[:, 0:NEXP])
                S.op('dve', nc.vector.max, out=m8[:, :], in_=lg[:, :], reads=[lg[:, :]], writes=[m8[:, :]])
                S.ts('dve', msk[:, :], lg[:, :], m8[:, 1:2], None, ALU.is_ge)
                S.ts('dve', den[:, 0:1], m8[:, 0:1], -1.0, None, ALU.mult)
                S.act(ex[:, :], lg[:, :], AF.Exp, bias=den[:, 0:1])
                S.tt('dve', ex[:, :], ex[:, :], msk[:, :], ALU.mult)
                S.op('dve', nc.vector.tensor_reduce, out=den[:, 1:2], in_=ex[:, :], axis=AX.X, op=ALU.add,
                     reads=[ex[:, :]], writes=[den[:, 1:2]])
                S.op('dve', nc.vector.reciprocal, out=den[:, 1:2], in_=den[:, 1:2], reads=[den[:, 1:2]], writes=[den[:, 1:2]])
                S.ts('dve', comb[:, t, :], ex[:, :], den[:, 1:2], None, ALU.mult)

        gi = 0
        for h0 in range(0, NT, HT):
            S.dma('sp', xmT[:, :, :], xmT_d[:, :, h0 * 128:(h0 + HT) * 128])
            S.dma('pool', pT[:, :, :], pT_d[:, h0 * 128:(h0 + HT) * 128].rearrange("(k p) n -> p k n", p=128))
            if moe:
                router(h0)
            for tl in range(HT):
                t = h0 + tl
                gps = self.ps.wide('y', [0, 2])
                pps = self.ps.wide('pp', [4, 6])
                for half in range(2):
                    cs = slice(half * 512, (half + 1) * 512)
                    for k in range(8):
                        S.mm(gps[:, cs], xmT[:, k, tl * 128:(tl + 1) * 128], plegw[:, k, cs], start=(k == 0), stop=False)
                    S.mm(gps[:, cs], self.ones_bf[0:1, 0:128], plegb[0:1, cs], start=False, stop=True)
                    for k in range(2):
                        S.mm(pps[:, cs], pT[:, k, tl * 128:(tl + 1) * 128], plew[:, k, cs], start=(k == 0), stop=(k == 1))
                sg_ = sig[0]
                S.act(sg_[:, :], gps, AF.Sigmoid)
                S.tt('dve', acc[:, tl, :], sg_[:, :], pps, ALU.mult)
            nchunk = HT // 4
            for e in range(NE):
                for g in range(NG):
                    b = gi % 2
                    gi += 1
                    S.dma('pool', wgb[b][:, :, :], wg_d[e, :, g * GW:(g + 1) * GW].rearrange("(k p) n -> p k n", p=128))
                    S.dma('pool', wub[b][:, :, :], wu_d[e, :, g * GW:(g + 1) * GW].rearrange("(k p) n -> p k n", p=128))
                    S.dma('pool', wdb[b][:, :, :], wd_d[e, g * GW:(g + 1) * GW, :].rearrange("(k p) n -> p k n", p=128))
                    for c in range(nchunk):
                        tok0 = (c * 4) * 128
                        hb = hT[c % 2]
                        for j in range(GW // 128):
                            hg = self.ps.bank('hg', [0, 1])
                            hu = self.ps.bank('hu', [2, 3])
                            for k in range(8):
                                S.mm(hg, wgb[b][:, k, j * 128:(j + 1) * 128], xmT[:, k, tok0:tok0 + 512], start=(k == 0), stop=(k == 7))
                            for k in range(8):
                                S.mm(hu, wub[b][:, k, j * 128:(j + 1) * 128], xmT[:, k, tok0:tok0 + 512], start=(k == 0), stop=(k == 7))
                            s_ = sg[j % 2]
                            S.act(s_[:, :], hg, AF.Silu)
                            S.tt('dve', hb[:, j, :], s_[:, :], hu, ALU.mult)
                        for tl4 in range(4):
                            tl = c * 4 + tl4
                            fps = self.ps.wide('pp', [4, 6])
                            for half in range(2):
                                cs = slice(half * 512, (half + 1) * 512)
                                for j in range(GW // 128):
                                    S.mm(fps[:, cs], hb[:, j, tl4 * 128:(tl4 + 1) * 128], wdb[b][:, j, cs],
                                         start=(j == 0), stop=(j == GW // 128 - 1))
                            S.stt('dve', acc[:, tl, :], fps, comb[:, h0 + tl, e:e + 1], acc[:, tl, :], ALU.mult, ALU.add)
            for tl in range(HT):
                t = h0 + tl
                xm = self.tl_xm[tl % len(self.tl_xm)]
                S.dma('sp', xm[:, :], xmid_d[t * 128:(t + 1) * 128, :])
                s = self.tl_s[tl % len(self.tl_s)]
                S.stt('dve', s[:, :], xm[:, :], ALPHA, acc[:, tl, :], ALU.mult, ALU.add)
                o = self.tl_x[tl % len(self.tl_x)]
                self.layer_norm(s, self.lnp[:, 0, :], self.lnp[:, 1, :], o[:, :], self.lntmp)
                S.dma('sp', y_out[t * 128:(t + 1) * 128, :], o[:, :])

    def load_lnp(self, a):
        self.lnp = self.sb(f"lnp{a}", [128, 2, 1024], F32)
        self.S.dma('sp', self.lnp[:, :, :], self.lnp_d[:, a:a + 2, :])

    def alloc_tiles(self):
        self.tl_x = [self.sb(f"tl_x{i}", [128, 1024], F32) for i in range(1)]
        self.tl_s = [self.sb(f"tl_s{i}", [128, 1024], F32) for i in range(1)]
        self.tl_xm = [self.sb(f"tl_xm{i}", [128, 1024], F32) for i in range(1)]
        self.tl_xmt = [self.sb(f"tl_xmt{i}", [128, 8, 128], BF16) for i in range(2)]

    def build(self):
        S = self.S
        T = self.T
        self.stk = None
        self.setup_common()
        x_own = self.inp("x_own", [T, 1024])
        y_out = self.outp("y", [T, 1024])
        xmid_d = self.scratch("xmid_d", [T, 1024], F32)
        xmT_d = self.scratch("xmT_d", [128, 8, T], BF16)
        from contextlib import ExitStack
        if self.mixers:
            self.stage_m(x_own, xmid_d, xmT_d)
        else:
            self.stk = ExitStack()
            self.alloc_tiles()
            self.load_lnp(0)
            for m in range(self.NM):
                self.mix_tail(m, None, None, xmid_d, xmT_d, x_own)
            barrier(self)
            self.stk.close()
        self.stk = ExitStack()
        self.alloc_tiles()
        self.load_lnp(2)
        self.stage_f(xmid_d, xmT_d, y_out)
        S.finish()
        return self.nc


def own_positions(NM, qt):
    return np.concatenate([np.arange(512) + 2048 * m + 512 * qt for m in range(NM)])


def prep_core_inputs(prog, layer, xl, inp, core, consts):
    NM = prog.NM
    b, qt = core // 4, core % 4
    pos = own_positions(NM, qt)
    d = {}
    need = prog.din
    j = layer // 2
    for name in need:
        if name == "x_own":
            d[name] = np.ascontiguousarray(xl[b, pos, :])
        elif name == "pT_own":
            d[name] = np.ascontiguousarray(inp['p'][layer, b, pos, :].T)
        elif name == "ln_params":
            rows = np.stack([inp['ln_mix_g'][layer], inp['ln_mix_b'][layer], inp['ln_ffn_g'][layer], inp['ln_ffn_b'][layer]])
            d[name] = np.ascontiguousarray(np.broadcast_to(rows[None], (128, 4, 1024)))
        elif name in ("ffn_w_gate", "ffn_w_up", "ffn_w_down"):
            d[name] = np.ascontiguousarray(inp[name][j:j + 1])
        elif name in ("moe_w_gate", "moe_w_up", "moe_w_down", "w_router"):
            d[name] = np.ascontiguousarray(inp[name][j])
        elif name == "b_router":
            d[name] = np.ascontiguousarray(inp[name][j][None, :])
        elif name in ("ple_w", "ple_gate_w"):
            d[name] = np.ascontiguousarray(inp[name][layer])
        elif name == "ple_gate_b":
            d[name] = np.ascontiguousarray(inp[name][layer][None, :])
        elif name in consts:
            d[name] = consts[name]
        else:
            d[name] = prep_mixer_input(prog, name, layer, xl, inp, b, qt)
        shape, dt = need[name]
        assert tuple(d[name].shape) == tuple(shape), (name, d[name].shape, shape)
    return d


def make_consts(prog):
    c = {"c_ident": np.eye(128, dtype=np.float32)}
    c.update(make_mixer_consts(prog))
    return c


_PROGS = {}
DEBUG = False
LAST = {}


def run_layer(layer, xl, inp, NM, moe, mixers=(0, 1, 2, 3)):
    key = (NM, moe, tuple(mixers))
    if key not in _PROGS:
        p = Prog(NM, moe, mixers)
        p.debug = DEBUG
        p.build()
        _PROGS[key] = (p, make_consts(p))
    prog, consts = _PROGS[key]
    in_maps = [prep_core_inputs(prog, layer, xl, inp, c, consts) for c in range(8)]
    res = run_bass_kernel_spmd(prog.nc, in_maps, core_ids=list(range(8)))
    LAST['res'] = res
    B, S_, _ = xl.shape
    out = np.empty_like(xl)
    for c in range(8):
        b, qt = c // 4, c % 4
        out[b, own_positions(NM, qt), :] = res.results[c]["y"]
    return out


def kernel(**inputs):
    inp = {k: np.asarray(v) for k, v in inputs.items()}
    x = inp['x'].astype(np.float32, copy=False)
    B, S_, _ = x.shape
    NM = S_ // 2048
    depth = inp['w_in'].shape[0]
    for layer in range(depth):
        x = run_layer(layer, x, inp, NM, moe=(layer % 2 == 1))
    return x


U_QA, U_KA, U_QC, U_QD, U_KWC, U_KD, U_KCC, U_KSC, U_VCC, U_GN = 0, 18, 36, 42, 48, 50, 52, 54, 56, 58
C_CONV, C_VA, C_VWC, C_VD, C_VSC, NCOLS = 4864, 6016, 7168, 7296, 7424, 7552
WIN = 2560
DILS = ((128, 1), (512, 4), (2048, 16))
BIGB = 1000.0


def win_perm():
    sizes = [1152, 1152, 1152, 384, 384, 384, 384, 128, 128, 128, 128, 128, 128, 18, 384, 128, 128]
    names = ['qa', 'ka', 'va', 'gb', 'gc', 'hb', 'qc', 'kcc', 'vcc', 'ksc', 'vsc', 'kwc', 'vwc', 'gn', 'qd', 'kd', 'vd']
    off = dict(zip(names, np.cumsum([0] + sizes[:-1]).tolist()))
    r = lambda a, n: list(range(a, a + n))
    cols = []
    cols += r(off['qa'], 1152) + r(off['ka'], 1152) + r(off['qc'], 384) + r(off['qd'], 384)
    cols += r(off['kwc'], 128) + r(off['kd'], 128) + r(off['kcc'], 128) + r(off['ksc'], 128) + r(off['vcc'], 128)
    for hq in range(6):
        for br in range(3):
            cols += [off['gn'] + br * 6 + hq] * 64
    cols += r(off['gb'], 1152)
    cols += r(off['va'], 1152) + r(off['vwc'], 128) + r(off['vd'], 128) + r(off['vsc'], 128)
    assert len(cols) == NCOLS
    return np.array(cols)


def _sb(self, name, shape, dt):
    used = self.__dict__.setdefault('_names', {})
    used[name] = used.get(name, 0) + 1
    if used[name] > 1:
        name = f"{name}_r{used[name]}"
    if getattr(self, 'stk', None) is not None:
        return self.stk.enter_context(self.nc.sbuf_tensor(name, list(shape), dt))
    return self.nc.alloc_sbuf_tensor(name, list(shape), dt)


def barrier(self):
    S = self.S
    for eng in ('pe', 'act', 'dve', 'pool', 'sp'):
        for i, c in enumerate(S.dcnt):
            if c:
                S._wait(eng, (('d', i), 16 * c))
        for k, c in S.cnt.items():
            if c and k != eng:
                S._wait(eng, (k, c))
    S.acc.clear()


def get_mask(self, allowed):
    nk, n = allowed.shape
    key = (nk, n, allowed.tobytes())
    if key not in self.mask_idx:
        off = self.mask_used
        assert off + n <= self.MCAP, "mask bank full"
        self.mask_np[:nk, off:off + n] = allowed.astype(np.float32)
        self.mask_idx[key] = off
        self.mask_used += n
    off = self.mask_idx[key]
    return self.maskbank[0:nk, off:off + n]


def wplan(self, groups):
    self.wg, self.wi, self.wissued = groups, 0, 0


def wget(self, c0, n):
    i = self.wi
    assert self.wg[i] == (c0, n), (i, self.wg[i], c0, n)
    self.wi += 1
    while self.wissued < min(len(self.wg), i + 2):
        j = self.wissued
        cc, nn = self.wg[j]
        self.S.dma('pool', self.wbufs[j % 3][:, :, 0:nn], self.w_in_d[:, cc:cc + nn].rearrange("(k p) n -> p k n", p=128))
        self.wissued += 1
    return self.wbufs[i % 3]


def evac(self, dst, src):
    self._n += 1
    self.S.copy('act' if self._n % 2 else 'dve', dst, src)


def proj_fm(self, w, wcol, dst, rhs_fn, ncols, rows=64, dst2=None):
    S = self.S
    for c0 in range(0, ncols, 512):
        n = min(512, ncols - c0)
        ps = self.ps.bank('pj', [6, 7])
        if dst2 is not None:
            for k in range(8):
                S.mm(ps[:, 0:n], w[:, k, wcol:wcol + 128], rhs_fn(k, c0, n), start=(k == 0), stop=(k == 7))
            S.copy('act', dst[0:64, c0:c0 + n], ps[0:64, 0:n])
            S.copy('dve', dst2[0:64, c0:c0 + n], ps[64:128, 0:n])
            continue
        for k in range(8):
            S.mm(ps[0:rows, 0:n], w[:, k, wcol:wcol + rows], rhs_fn(k, c0, n), start=(k == 0), stop=(k == 7))
        evac(self, dst[0:rows, c0:c0 + n], ps[0:rows, 0:n])


PV_DEPTH = 3


def pv_defer(self, fn):
    q = self.__dict__.setdefault('pvq', [])
    q.append(fn)
    while len(q) > PV_DEPTH:
        q.pop(0)()


def pv_flush(self):
    q = self.__dict__.setdefault('pvq', [])
    while q:
        q.pop(0)()


def band_attn(self, o_ps_fn, qT, nq, uq0, kT_fn, v_fn, uk0, nk, wd, first):
    S = self.S
    nt = (nk + 127) // 128
    for i in range(nt):
        nki = min(128, nk - 128 * i)
        k_lo = uk0 + 128 * i
        k_hi = k_lo + nki - 1
        c0 = max(k_lo, uq0) - uq0
        c1 = min(k_hi + wd, uq0 + nq - 1) - uq0 + 1
        if c1 <= c0:
            continue
        n = c1 - c0
        kk = np.arange(k_lo, k_lo + nki)[:, None]
        qq = np.arange(uq0 + c0, uq0 + c1)[None, :]
        allowed = (kk <= qq) & (kk >= qq - wd)
        s_ps = self.ps.bank('sc', [0, 1, 2])
        S.mm(s_ps[0:nki, 0:n], kT_fn(i, nki), qT[:, c0:c1])
        p = self.pbufs[self._pn % len(self.pbufs)]
        self._pn += 1
        S.act(p[0:nki, 0:n], s_ps[0:nki, 0:n], AF.Exp, scale=0.125)
        if not allowed.all():
            bad = np.where(~allowed.all(axis=0))[0]
            a, b = int(bad.min()), int(bad.max()) + 1
            mk = get_mask(self, allowed[:, a:b])
            S.tt('dve', p[0:nki, a:b], p[0:nki, a:b], mk, ALU.mult)
        pv_defer(self, lambda o_=o_ps_fn(c0, c1), v_=v_fn(i, nki), p_=p[0:nki, 0:n], st_=first[0]:
                 S.mm(o_, v_, p_, start=st_, stop=False))
        first[0] = False


def vsel(vt, h, nb):
    return vt[:, h, :]


def carve(ar, off, p0, p1, shape):
    n = int(np.prod(shape))
    v = ar[p0:p1, off:off + n]
    if len(shape) == 2:
        v = v.rearrange("p (a b) -> p a b", a=shape[0])
    elif len(shape) == 3:
        v = v.rearrange("p (a b c) -> p a b c", a=shape[0], b=shape[1])
    return v, off + n


def normalize(self, o_src, dst, extra=None, clamp=False):
    S, nc = self.S, self.nc
    rl = self.rlb[self._n % 2]
    self._n += 1
    if extra is not None:
        S.ts('dve', rl[:, :], o_src[64:128, :], extra, None, ALU.add)
        src = rl[:, :]
    elif clamp:
        S.ts('dve', rl[:, :], o_src[64:128, :], 1e-30, None, ALU.max)
        src = rl[:, :]
    else:
        S.ts('dve', rl[:, :], o_src[64:128, :], 0.0, None, ALU.add)
        src = rl[:, :]
    S.op('dve', nc.vector.reciprocal, out=rl[:, :], in_=src, reads=[src], writes=[rl[:, :]])
    S.tt('dve', dst, o_src[0:64, :], rl[:, :], ALU.mult)


def stage_m_setup(self):
    S, nc = self.S, self.nc
    NS, NKT = self.SB // 64, self.SB // 2048
    self.MCAP = 2048
    self.mask_np = np.zeros((128, self.MCAP), np.float32)
    self.mask_idx, self.mask_used = {}, 0
    self.maskbank = self.sb("maskbank", [128, self.MCAP], BF16)
    S.dma('pool', self.maskbank[:, :], self.inp("c_masks", [128, self.MCAP]))
    self.convw = self.sb("convw", [128, 3, 3], F32)
    S.dma('sp', self.convw[:, :, :], self.inp("conv_w_l", [128, 3, 3]))
    self.sinkexp = self.sb("sinkexp", [128, 6], F32)
    S.dma('sp', self.sinkexp[:, :], self.inp("sinks_rep", [128, 6]))
    S.act(self.sinkexp[:, :], self.sinkexp[:, :], AF.Exp)
    self.bmg = self.sb("bmg", [128, 4, 8], F32)
    S.dma('sp', self.bmg[:, :, :], self.inp("b_merge_l", [128, 4, 8]))
    self.pbufs = [self.sb(f"pbuf{i}", [128, 512], BF16) for i in range(4)]
    self._pn = 0
    self.wbufs = [self.sb(f"wbuf{i}", [128, 8, 384], BF16) for i in range(3)]
    self.oT = self.sb("oT", [64, 18, 512], BF16)
    self.oTb = self.sb("oTb", [128, 3, 512], BF16)
    self.xw = self.sb("xw", [128, 8, 1536], BF16)
    self.arb = self.sb("arb", [128, 29184], BF16)
    self.arf = self.sb("arf", [128, 6144], F32)
    self.load_lnp(0)
    self.rlb = [self.sb(f"rlb{i}", [64, 512], F32) for i in range(2)]
    if 2 in self.mixers:
        self.Eq = self.sb("Eq", [128, 32, 128], BF16)
        S.dma('pool', self.Eq[:, :, :], self.inp("c_Eq", [128, 32, 128]))
        self.ovl = self.sb("ovl", [128, NKT, NS + 1], BF16)
        S.dma('pool', self.ovl[:, :, :], self.inp("c_ovl", [128, NKT, NS + 1]))
        self.dqk = self.sb("dqk", [128, 2, 512], F32)
        S.dma('sp', self.dqk[:, :, :], self.inp("c_dqk", [128, 2, 512]))
        self.thr = self.sb("thr", [128, 18], F32)
        S.dma('sp', self.thr[:, :], self.inp("pc_thr", [128, 18]))
        self.negselT = self.sb("negselT", [64, 2, self.NS // 64, 512], BF16)
        self.ibias = self.sb("ibias", [128, 4, NS], F32)
        self.selw = self.sb("selw", [128, NS], F32)
        self.m8 = self.sb("m8", [128, 24], F32)


def stage_k(self):
    from contextlib import ExitStack
    S, nc = self.S, self.nc
    SB, NKT = self.SB, self.NKT
    xTf = self.inp("xT_full", [1024, SB])
    self.kslc_d = self.scratch("kslc_d", [2, 64, SB], BF16)
    self.vslc_d = self.scratch("vslc_d", [SB, 128], BF16)
    kcmp_d = self.scratch("kcmp_d", [4, 64, SB + 32], BF16)
    w1_d = self.inp("cmp_w1_l", [64, 2, 32, 128])
    pos_d = self.inp("cmp_pos_l", [64, 2, 32])
    b1_d = self.inp("cmp_b1_l", [128, 2])
    w2_d = self.inp("cmp_w2_l", [128, 2, 64])
    b2k_d = self.inp("cmp_b2k_l", [64, 1])
    b2v_d = self.inp("cmp_b2v_l", [1, 64])
    self.stk = ExitStack()
    wk = self.sb("k_w", [128, 8, 512], BF16)
    S.dma('pool', wk[:, :, 0:384], self.w_in_d[:, 64 * U_KCC:64 * U_KCC + 384].rearrange("(k p) n -> p k n", p=128))
    S.dma('pool', wk[:, :, 384:512], self.w_in_d[:, C_VSC:C_VSC + 128].rearrange("(k p) n -> p k n", p=128))
    xp = [self.sb(f"k_xp{i}", [128, 8, 512], BF16) for i in range(2)]
    ut = [self.sb(f"k_ut{i}", [64, 6, 512], BF16) for i in range(2)]
    vt = [self.sb(f"k_vt{i}", [128, 4, 128], BF16) for i in range(2)]
    zt = self.sb("k_zero", [64, 32], BF16)
    S.memset('pool', zt[:, :], 0.0)
    for s in range(4):
        S.dma('sp', kcmp_d[s, :, SB:SB + 32], zt[:, :])
    for j in range(SB // 512):
        x_ = xp[j % 2]
        S.dma('pool', x_[:, :, :], xTf[:, 512 * j:512 * j + 512].rearrange("(k p) n -> p k n", p=128))
        u_ = ut[j % 2]
        for u in range(0, 6, 2):
            proj_fm(self, wk, 64 * u, u_[:, u, :], lambda k, c0, n: x_[:, k, c0:c0 + n], 512, dst2=u_[:, u + 1, :])
        for s, u in enumerate((0, 1, 4, 5)):
            S.dma('sp', kcmp_d[s, :, 512 * j:512 * j + 512], u_[:, u, :])
        for kv in range(2):
            S.dma('sp', self.kslc_d[kv, :, 512 * j:512 * j + 512], u_[:, 2 + kv, :])
        v_ = vt[j % 2]
        for t in range(4):
            ps = self.ps.bank('pj', [6, 7])
            for k in range(8):
                S.mm(ps[:, 0:128], x_[:, k, 128 * t:128 * t + 128], wk[:, k, 384:512], start=(k == 0), stop=(k == 7))
            evac(self, v_[:, t, :], ps[:, 0:128])
        S.dma('sp', self.vslc_d[512 * j:512 * j + 512, :].rearrange("(t p) c -> p t c", p=128), v_[:, :, :])
    w1 = self.sb("k_w1", [64, 2, 32, 128], BF16)
    S.dma('pool', w1[:, :, :, :], w1_d)
    pos = self.sb("k_pos", [64, 2, 32], BF16)
    S.dma('pool', pos[:, :, :], pos_d)
    b1 = self.sb("k_b1", [128, 2], F32)
    S.dma('sp', b1[:, :], b1_d)
    w2 = self.sb("k_w2", [128, 2, 64], BF16)
    S.dma('pool', w2[:, :, :], w2_d)
    b2k = self.sb("k_b2k", [64, 1], F32)
    S.dma('sp', b2k[:, :], b2k_d)
    b2v = self.sb("k_b2v", [1, 64], BF16)
    S.dma('pool', b2v[:, :], b2v_d)
    beff = self.sb("k_beff", [128, 2], F32)
    for wh in range(2):
        ps = self.ps.bank('pj', [6, 7])
        for p_ in range(32):
            S.mm(ps[:, 0:1], w1[:, wh, p_, :], pos[:, wh, p_:p_ + 1], start=(p_ == 0), stop=(p_ == 31))
        S.tt('dve', beff[:, wh:wh + 1], ps[:, 0:1], b1[:, wh:wh + 1], ALU.add)
    kk = [self.sb(f"k_kk{i}", [64, 8192 + 32], BF16) for i in range(2)]
    hx = self.sb("k_hx", [128, 512], F32)
    hu = self.sb("k_hu", [128, 512], F32)
    hT = self.sb("k_hT", [128, 512], BF16)
    S.memset('pool', self.vc[:, :, :, 64:128], 1.0)
    it = 0
    for wh in range(2):
        for kv in range(2):
            s = wh * 2 + kv
            for bg in range((NKT * 128 + 511) // 512):
                nb = min(512, NKT * 128 - 512 * bg)
                k_ = kk[it % 2]
                it += 1
                ntok = nb * 16 + 16
                S.dma('sp', k_[:, 0:ntok], kcmp_d[s, :, 8192 * bg:8192 * bg + ntok])
                hp = self.ps.bank('sc', [0, 1, 2])
                for p_ in range(32):
                    S.mm(hp[:, 0:nb], w1[:, wh, p_, :], k_[:, p_:p_ + 16 * nb:16], start=(p_ == 0), stop=(p_ == 31))
                S.act(hx[:, 0:nb], hp[:, 0:nb], AF.Identity, bias=beff[:, wh:wh + 1])
                S.tt('dve', hu[:, 0:nb], hx[:, 0:nb], hx[:, 0:nb], ALU.mult)
                S.ts('dve', hu[:, 0:nb], hu[:, 0:nb], 0.044715, 1.0, ALU.mult, ALU.add)
                S.tt('dve', hu[:, 0:nb], hu[:, 0:nb], hx[:, 0:nb], ALU.mult)
                S.act(hu[:, 0:nb], hu[:, 0:nb], AF.Sigmoid, scale=1.5957691216057308)
                S.tt('dve', hT[:, 0:nb], hx[:, 0:nb], hu[:, 0:nb], ALU.mult)
                if wh == 0:
                    kp = self.ps.bank('pj', [6, 7])
                    S.mm(kp[0:64, 0:nb], w2[:, 0, :], hT[:, 0:nb])
                    S.act(self.kcT[:, kv, 512 * bg:512 * bg + nb], kp[0:64, 0:nb], AF.Identity, bias=b2k[:, 0:1])
                else:
                    for t in range(nb // 128):
                        vp = self.ps.bank('pj', [6, 7])
                        S.mm(vp[:, 0:64], hT[:, 128 * t:128 * t + 128], w2[:, 1, :], start=True, stop=False)
                        S.mm(vp[:, 0:64], self.ones_bf[0:1, 0:128], b2v[0:1, :], start=False, stop=True)
                        evac(self, self.vc[:, bg * 4 + t, kv, 0:64], vp[:, 0:64])
    barrier(self)
    self.stk.close()
    self.stk = None


Prog.sb = _sb
Prog.stage_k = stage_k
Prog.stage_m_setup = stage_m_setup


def stage_m(self, x_own, xmid_d, xmT_d):
    from contextlib import ExitStack
    S, nc = self.S, self.nc
    NM, NS, NKT, NH = self.NM, self.SB // 64, self.SB // 2048, (self.SB // 64 + 127) // 128
    mix = self.mixers
    self.stk = ExitStack()
    self.NS, self.NKT, self.NH = NS, NKT, NH
    self.w_in_d = self.inp("w_in_u", [1024, NCOLS])
    if 2 in mix:
        self.kcT = self.sb("kcT", [64, 2, NKT * 128], BF16)
        self.vc = self.sb("vc", [128, NKT, 2, 128], BF16)
        stk_m = self.stk
        stage_k(self)
        self.stk = stk_m
    stage_m_setup(self)
    xTw_d = self.inp("xT_win", [NM, 1024, WIN])
    pad_d = self.inp("padrow", [NM, 1, WIN])
    wg_d = self.inp("w_mgate_l", [8, 128, 4, 8, 128])
    wb_d = self.inp("w_branch_l", [8, 64, 18, 128])
    wbc_d = self.inp("w_branchc_l", [8, 128, 3, 128])
    wout_d = self.inp("w_out_l", [1024, 1024])
    if 2 in mix:
        ibias_d = self.inp("pc_impbias", [NM, 128, 4, NS])
    arb, arf, xw = self.arb, self.arf, self.xw
    o = 0
    qbuf, o = carve(arb, o, 0, 65, (6, 512))
    kbuf, o = carve(arb, o, 0, 65, (2, WIN))
    vbuf, o = carve(arb, o, 0, 128, (32, 2, 128))
    o_x = o
    xh, _ = carve(arb, o_x, 0, 128, (8, 1024))
    gbuf, o = carve(arb, o, 0, 64, (9, 512))
    ET, _ = carve(arb, o, 0, 128, (8, 512))
    kslab = []
    vslab = []
    for i in range(2):
        a, o = carve(arb, o, 0, 64, (2048,))
        kslab.append(a)
    for i in range(2):
        a, o = carve(arb, o, 0, 128, (16, 2, 64))
        vslab.append(a)
    assert o <= 29184, o
    f = 0
    Oacc, f = carve(arf, f, 0, 128, (6, 512))
    onsa, _ = carve(arf, 0, 0, 64, (6, 512))
    cva, _ = carve(arf, 0, 0, 128, (514,))
    cvb, _ = carve(arf, 514, 0, 128, (514,))
    imp, f = carve(arf, f, 0, 128, (4, 2, NS))
    tmpn = []
    for i in range(2):
        a, f = carve(arf, f, 0, 64, (512,))
        tmpn.append(a)
    assert f <= 6144, f
    o = 0
    wgb, wbb, wbcb = [], [], []
    for i in range(2):
        a, o = carve(arb, o, 0, 128, (4, 8, 128))
        wgb.append(a)
    for i in range(2):
        a, o = carve(arb, o, 0, 64, (18, 128))
        wbb.append(a)
    for i in range(2):
        a, o = carve(arb, o, 0, 128, (3, 128))
        wbcb.append(a)
    mergedT, o = carve(arb, o, 0, 128, (8, 512))
    wout, o = carve(arb, o, 0, 128, (8, 1024))
    assert o <= 27136, o
    f = 0
    gsb, f = carve(arf, f, 0, 128, (512,))
    macc, f = carve(arf, f, 0, 128, (512,))
    mtmp, f = carve(arf, f, 0, 128, (512,))
    tls = []
    for i in range(4):
        a, f = carve(arf, f, 0, 128, (1024,))
        tls.append(a)
    self.tl_x, self.tl_s, self.tl_xm = tls[0:1], tls[1:2], tls[2:4]
    self.tl_xmt = [self.sb(f"tl_xmt{i}", [128, 8, 128], BF16) for i in range(2)]
    assert f <= 6144, f

    def xcol(k, a, b, step=1):
        if a >= 1024:
            return xw[:, k, a - 1024:b - 1024:step]
        assert b <= 1024 + step - 1, (a, b)
        return xh[:, k, a:min(b, 1024):step]

    xrhs = lambda k, c0, n: xcol(k, 2048 + c0, 2048 + c0 + n)

    for m in range(NM):
        S.dma('pool', xw[:, :, :], xTw_d[m][:, 1024:WIN].rearrange("(k p) n -> p k n", p=128))
        for j in range(2):
            S.dma('pool', kbuf[64:65, j, :], pad_d[m])
        S.memset('pool', qbuf[64:65, :, :], 1.0)
        S.memset('pool', vbuf[:, :, :, 64:128], 1.0)
        for i in range(2):
            S.memset('pool', vslab[i][:, :, 1, :], 1.0)
        groups = []
        if 1 in mix:
            groups += [(C_CONV, 384), (C_CONV + 384, 384), (C_CONV + 768, 384)]
        if 0 in mix:
            for g in range(3):
                groups += [(64 * (U_QA + 6 * g), 384), (64 * (U_KA + 6 * g), 384), (C_VA + 384 * g, 384)]
        if 3 in mix:
            groups += [(64 * U_QD, 384), (64 * U_KD, 128), (C_VD, 128)]
        if 2 in mix:
            groups += [(64 * U_QC, 384), (64 * U_KWC, 128), (C_VWC, 128)]
            for kv in range(2):
                groups += [(64 * (U_GN + 9 * kv), 384), (64 * (U_GN + 9 * kv + 6), 192)]
        wplan(self, groups)

        if 1 in mix:
            wgb_ = wget(self, C_CONV, 384)
            for c in range(3):
                p3 = self.ps.bank('pj', [6, 7])
                for k in range(8):
                    S.mm(p3[:, :], wgb_[:, k, 128 * c:128 * c + 128], xcol(k, 2048, 2560), start=(k == 0), stop=(k == 7))
                evac(self, self.oTb[:, c, :], p3[:, :])
            wgc_ = wget(self, C_CONV + 384, 384)
            whb = wget(self, C_CONV + 768, 384)
            for c in range(3):
                u = cva
                for (c0, n, dcol) in ((2046, 2, 0), (2048, 512, 2)):
                    p1 = self.ps.bank('pj', [6, 7])
                    p2 = self.ps.bank('pj', [6, 7])
                    for k in range(8):
                        S.mm(p1[:, 0:n], wgc_[:, k, 128 * c:128 * c + 128], xcol(k, c0, c0 + n), start=(k == 0), stop=(k == 7))
                    for k in range(8):
                        S.mm(p2[:, 0:n], whb[:, k, 128 * c:128 * c + 128], xcol(k, c0, c0 + n), start=(k == 0), stop=(k == 7))
                    S.copy('act', cvb[:, dcol:dcol + n], p1[:, 0:n])
                    S.tt('dve', u[:, dcol:dcol + n], cvb[:, dcol:dcol + n], p2[:, 0:n], ALU.mult)
                S.ts('dve', cvb[:, 0:512], u[:, 0:512], self.convw[:, c, 0:1], None, ALU.mult)
                S.stt('dve', cvb[:, 0:512], u[:, 1:513], self.convw[:, c, 1:2], cvb[:, 0:512], ALU.mult, ALU.add)
                S.stt('dve', cvb[:, 0:512], u[:, 2:514], self.convw[:, c, 2:3], cvb[:, 0:512], ALU.mult, ALU.add)
                S.tt('pool', self.oTb[:, c, :], cvb[:, 0:512], self.oTb[:, c, :], ALU.mult)

        if 0 in mix:
            for g in range(3):
                D = DILS[g][1]
                lo = 2048 - DILS[g][0]
                nq = 512 // D
                uq0 = 2048 // D
                uk0 = uq0 - 128
                nk = 128 + nq
                ntile = (nk + 127) // 128
                if lo < 1024:
                    S.dma('pool', xh[:, :, :], xTw_d[m][:, 0:1024].rearrange("(k p) n -> p k n", p=128))
                wq = wget(self, 64 * (U_QA + 6 * g), 384)
                for h in range(0, 6, 2):
                    proj_fm(self, wq, 64 * h, qbuf[:, h, :], xrhs, 512, dst2=qbuf[:, h + 1, :])
                wk = wget(self, 64 * (U_KA + 6 * g), 384)
                wv = wget(self, C_VA + 384 * g, 384)
                for hp in range(3):
                    proj_fm(self, wk, 64 * (2 * hp), kbuf[:, 0, lo:WIN],
                            lambda k, c0, n: xcol(k, lo + c0, lo + c0 + n), WIN - lo, dst2=kbuf[:, 1, lo:WIN])
                    for r in range(D):
                        for i in range(ntile):
                            nki = min(128, nk - 128 * i)
                            st = r + D * (uk0 + 128 * i)
                            vp = self.ps.bank('pj', [6, 7])
                            en = st + D * (nki - 1) + 1
                            if st < 1024 < en:
                                n1 = (1024 - st + D - 1) // D
                                parts = [(0, n1, st, st + D * (n1 - 1) + 1), (n1, nki, st + D * n1, en)]
                            else:
                                parts = [(0, nki, st, en)]
                            for (r0, r1, a_, b_) in parts:
                                assert r0 in (0, 32, 64), r0
                                for k in range(8):
                                    S.mm(vp[r0:r1, 0:128], xcol(k, a_, b_, D), wv[:, k, 128 * hp:128 * hp + 128],
                                         start=(k == 0), stop=(k == 7))
                            evac(self, vbuf[0:nki, r * ntile + i, :, 0:64], vp[0:nki, 0:128].rearrange("p (a b) -> p a b", a=2))
                    for j in range(2):
                        h = 2 * hp + j
                        o_ps = self.ps.bank('o', [3, 4, 5])
                        first = [True]
                        for r in range(D):
                            band_attn(self,
                                      lambda c0, c1: o_ps[:, r + D * c0:r + D * (c1 - 1) + 1:D],
                                      qbuf[:, h, r:512:D], nq, uq0,
                                      lambda i, n: kbuf[:, j, r + D * (uk0 + 128 * i):r + D * (uk0 + 128 * i) + D * (n - 1) + 1:D],
                                      lambda i, n: vsel(vbuf[0:n, r * ntile + i, :, :], j, 2),
                                      uk0, nk, 128, first)
                        pv_flush(self)
                        if g == 0:
                            S.copy('act', Oacc[:, h, :], o_ps)
                        else:
                            S.tt('dve', Oacc[:, h, :], Oacc[:, h, :], o_ps, ALU.add)
            for h in range(6):
                normalize(self, Oacc[:, h, :], self.oT[:, h, :])

        if 3 in mix:
            lo = 2048 - 128
            wq = wget(self, 64 * U_QD, 384)
            for h in range(0, 6, 2):
                proj_fm(self, wq, 64 * h, qbuf[:, h, :], xrhs, 512, dst2=qbuf[:, h + 1, :])
            wk = wget(self, 64 * U_KD, 128)
            proj_fm(self, wk, 0, kbuf[:, 0, lo:WIN], lambda k, c0, n: xcol(k, lo + c0, lo + c0 + n), WIN - lo, dst2=kbuf[:, 1, lo:WIN])
            wv = wget(self, C_VD, 128)
            for i in range(5):
                vp = self.ps.bank('pj', [6, 7])
                for k in range(8):
                    S.mm(vp[:, 0:128], xcol(k, lo + 128 * i, lo + 128 * i + 128), wv[:, k, 0:128], start=(k == 0), stop=(k == 7))
                evac(self, vbuf[:, i, :, 0:64], vp[:, 0:128].rearrange("p (a b) -> p a b", a=2))
            for h in range(6):
                kv = h // 3
                o_ps = self.ps.bank('o', [3, 4, 5])
                band_attn(self, lambda c0, c1: o_ps[:, c0:c1], qbuf[:, h, :], 512, 2048,
                          lambda i, n: kbuf[:, kv, lo + 128 * i:lo + 128 * i + n],
                          lambda i, n: vsel(vbuf[0:n, i, :, :], kv, 2), lo, 640, 127, [True])
                pv_flush(self)
                normalize(self, o_ps, self.oT[:, 12 + h, :], extra=self.sinkexp[64:128, h:h + 1])

        if 2 in mix:
            nsa_chunk(self, m, locals())

        if getattr(self, 'debug', False) and m == NM - 1:
            dbg = self.outp("dbg_oT", [64, 18, 512], BF16)
            S.dma('sp', dbg, self.oT[:, :, :])
        S.dma('pool', wout[:, :, :], wout_d.rearrange("(k p) n -> p k n", p=128))
        def load_fc(fc_):
            b_ = fc_ % 2
            S.dma('pool', wgb[b_][:, :, :, :], wg_d[fc_])
            S.dma('pool', wbb[b_][:, :, :], wb_d[fc_])
            S.dma('pool', wbcb[b_][:, :, :], wbc_d[fc_])
        load_fc(0)
        for fc in range(8):
            b = fc % 2
            if fc + 1 < 8:
                load_fc(fc + 1)
            nmix = len(mix)
            for idx, mi in enumerate(mix):
                bp = self.ps.bank('sc', [0, 1, 2])
                if mi == 1:
                    for c in range(3):
                        S.mm(bp[:, :], wbcb[b][:, c, :], self.oTb[:, c, :], start=(c == 0), stop=(c == 2))
                else:
                    sl = {0: 0, 2: 6, 3: 12}[mi]
                    for h in range(6):
                        S.mm(bp[:, :], wbb[b][:, sl + h, :], self.oT[:, sl + h, :], start=(h == 0), stop=(h == 5))
                gp = self.ps.bank('o', [3, 4, 5])
                for k in range(8):
                    S.mm(gp[:, :], wgb[b][:, mi, k, :], xcol(k, 2048, 2560), start=(k == 0), stop=(k == 7))
                S.act(gsb[:, :], gp[:, :], AF.Sigmoid, bias=self.bmg[:, mi, fc:fc + 1])
                last = (idx == nmix - 1)
                if idx == 0:
                    S.tt('dve', mergedT[:, fc, :] if last else macc[:, :], gsb[:, :], bp[:, :], ALU.mult)
                else:
                    S.tt('dve', mtmp[:, :], gsb[:, :], bp[:, :], ALU.mult)
                    S.tt('pool', mergedT[:, fc, :] if last else macc[:, :], macc[:, :], mtmp[:, :], ALU.add)
        self.mix_tail(m, mergedT, wout, xmid_d, xmT_d, x_own)
    barrier(self)
    self.stk.close()
    self.stk = None


Prog.stage_m = stage_m


_PERM = win_perm()


def prep_mixer_input(prog, name, layer, xl, inp, b, qt):
    NM, SB = prog.NM, prog.SB
    NS = SB // 64
    f32 = np.float32
    if name == "w_in_u":
        return np.ascontiguousarray(inp['w_in'][layer][:, _PERM])
    if name == "xT_win":
        out = np.zeros((NM, 1024, WIN), f32)
        for m in range(NM):
            t0 = 2048 * m + 512 * qt
            lo = t0 - 2048
            a = max(lo, 0)
            out[m, :, a - lo:] = xl[b, a:t0 + 512, :].T
        return out
    if name == "padrow":
        out = np.zeros((NM, 1, WIN), f32)
        for m in range(NM):
            pos = 2048 * m + 512 * qt - 2048 + np.arange(WIN)
            out[m, 0, pos < 0] = NEG
        return out
    if name == "xT_full":
        return np.ascontiguousarray(xl[b].T)
    if name == "conv_w_l":
        return np.ascontiguousarray(inp['conv_w'][layer].reshape(3, 3, 128).transpose(2, 1, 0))
    if name == "sinks_rep":
        return np.ascontiguousarray(np.broadcast_to(inp['sinks'][layer][None, :], (128, 6)))
    if name == "b_merge_l":
        return np.ascontiguousarray(inp['b_merge_gate'][layer].reshape(4, 8, 128).transpose(2, 0, 1))
    if name == "w_mgate_l":
        w = inp['w_merge_gate'][layer].reshape(4, 8, 128, 8, 128)
        return np.ascontiguousarray(w.transpose(3, 2, 0, 1, 4))
    if name == "w_branch_l":
        w = inp['w_branch'][layer][[0, 2, 3]].reshape(3, 6, 64, 8, 128)
        return np.ascontiguousarray(w.transpose(3, 2, 0, 1, 4).reshape(8, 64, 18, 128))
    if name == "w_branchc_l":
        w = inp['w_branch'][layer][1].reshape(3, 128, 8, 128)
        return np.ascontiguousarray(w.transpose(2, 1, 0, 3))
    if name == "w_out_l":
        return np.ascontiguousarray(inp['w_out'][layer])
    if name == "cmp_w1_l":
        w = inp['cmp_w1'][layer].reshape(2, 32, 64, 128)
        return np.ascontiguousarray(w.transpose(2, 0, 1, 3))
    if name == "cmp_pos_l":
        return np.ascontiguousarray(inp['cmp_pos'][layer].transpose(2, 0, 1))
    if name == "cmp_b1_l":
        return np.ascontiguousarray(inp['cmp_b1'][layer].T)
    if name == "cmp_w2_l":
        return np.ascontiguousarray(inp['cmp_w2'][layer].transpose(1, 0, 2))
    if name == "cmp_b2k_l":
        return np.ascontiguousarray(inp['cmp_b2'][layer][0][:, None])
    if name == "cmp_b2v_l":
        return np.ascontiguousarray(inp['cmp_b2'][layer][1][None, :])
    if name == "pc_thr":
        row = np.zeros(18, f32)
        row[0:16] = 128 * np.arange(16) - 512 * qt
        row[16] = -2048 + 31 - 512 * qt
        row[17] = 31 - 512 * qt
        return np.ascontiguousarray(np.broadcast_to(row[None, :], (128, 18)))
    if name == "pc_impbias":
        out = np.zeros((NM, 128, 4, NS), f32)
        jj = np.arange(NS)[None, :]
        for m in range(NM):
            for qi in range(4):
                t = 2048 * m + 512 * qt + 128 * qi + np.arange(128)
                cur = (t // 64)[:, None]
                forced = (jj == 0) | (jj == cur) | (jj == cur - 1)
                out[m, :, qi, :] = np.where(forced, BIGB, np.where(jj <= cur, 0.0, -BIGB))
        return out
    raise KeyError(name)


def make_mixer_consts(prog):
    c = {}
    if not prog.mixers:
        return c
    c["c_masks"] = prog.mask_np
    if 2 in prog.mixers:
        NS, NKT = prog.SB // 64, prog.SB // 2048
        p = np.arange(128)[:, None, None]
        r = np.arange(16)[None, :, None]
        j = np.arange(128)[None, None, :]
        e1 = ((p % 32) == 2 * r + (j >= 64)).astype(np.float32)
        e2 = e1 * ((p % 64) >= 32)
        c["c_Eq"] = np.ascontiguousarray(np.concatenate([e1, e2], axis=1))
        cc = (np.arange(NKT)[None, :, None] * 128 + np.arange(128)[:, None, None])
        jb = np.arange(NS)[None, None, :]
        ov = ((16 * cc < 64 * jb + 64) & (16 * cc + 31 >= 64 * jb)).astype(np.float32)
        c["c_ovl"] = np.concatenate([ov, np.ones((128, NKT, 1), np.float32)], axis=2)
        q = np.arange(512)[None, :]
        jl = np.arange(128)[:, None]
        c["c_dqk"] = np.ascontiguousarray(np.stack([q - jl, q - 16 * jl], axis=1).astype(np.float32))
    return c


def nsa_chunk(self, m, L):
    S, nc = self.S, self.nc
    NS, NKT, NH = self.NS, self.NKT, self.NH
    qbuf, kbuf, vbuf, gbuf, ET = L['qbuf'], L['kbuf'], L['vbuf'], L['gbuf'], L['ET']
    kslab, vslab, onsa, imp, tmpn = L['kslab'], L['vslab'], L['onsa'], L['imp'], L['tmpn']
    xcol, xrhs, ibias_d = L['xcol'], L['xrhs'], L['ibias_d']
    lo = 2048 - 512
    S.dma('sp', self.ibias[:, :, :], ibias_d[m])
    wq = wget(self, 64 * U_QC, 384)
    for h in range(0, 6, 2):
        proj_fm(self, wq, 64 * h, qbuf[:, h, :], xrhs, 512, dst2=qbuf[:, h + 1, :])
    wk = wget(self, 64 * U_KWC, 128)
    proj_fm(self, wk, 0, kbuf[:, 0, lo:WIN], lambda k, c0, n: xcol(k, lo + c0, lo + c0 + n), WIN - lo, dst2=kbuf[:, 1, lo:WIN])
    wv = wget(self, C_VWC, 128)
    for i in range(8):
        vp = self.ps.bank('pj', [6, 7])
        for k in range(8):
            S.mm(vp[:, 0:128], xcol(k, lo + 128 * i, lo + 128 * i + 128), wv[:, k, 0:128], start=(k == 0), stop=(k == 7))
        evac(self, vbuf[:, i, :, 0:64], vp[:, 0:128].rearrange("p (a b) -> p a b", a=2))
    nkt = min(m + 1, NKT)
    for kv in range(2):
        wga = wget(self, 64 * (U_GN + 9 * kv), 384)
        wgb_ = wget(self, 64 * (U_GN + 9 * kv + 6), 192)
        for u in range(9):
            w_, wc = (wga, 64 * u) if u < 6 else (wgb_, 64 * (u - 6))
            ps = self.ps.bank('pj', [6, 7])
            for k in range(8):
                S.mm(ps[0:64, :], w_[:, k, wc:wc + 64], xcol(k, 2048, 2560), start=(k == 0), stop=(k == 7))
            S.act(gbuf[:, u, :], ps[0:64, :], AF.Sigmoid)
        for hl in range(3):
            hq = 3 * kv + hl
            o_ps = self.ps.bank('o', [3, 4, 5])
            band_attn(self, lambda c0, c1: o_ps[:, c0:c1], qbuf[:, hq, :], 512, 2048,
                      lambda i, n: kbuf[:, kv, lo + 128 * i:lo + 128 * i + n],
                      lambda i, n: vsel(vbuf[0:n, i, :, :], kv, 2), lo, 1024, 511, [True])
            pv_flush(self)
            t_ = tmpn[0]
            normalize(self, o_ps, t_[:, :])
            S.tt('pool', onsa[:, hq, :], t_[:, :], gbuf[:, 3 * hl + 2, :], ALU.mult)
            o_ps = self.ps.bank('o', [3, 4, 5])
            for kt in range(nkt):
                s_ps = self.ps.bank('sc', [0, 1, 2])
                S.mm(s_ps[:, :], self.kcT[:, kv, 128 * kt:128 * kt + 128], qbuf[0:64, hq, :])
                S.act(ET[:, kt, :], s_ps[:, :], AF.Exp, scale=0.125)
                if kt >= m - 1:
                    S.stt('dve', ET[:, kt, :], self.dqk[:, 1, :], self.thr[:, 16 + (kt - (m - 1)):17 + (kt - (m - 1))],
                          ET[:, kt, :], ALU.is_ge, ALU.mult)
                pv_defer(self, lambda o_=o_ps[:, :], v_=self.vc[:, kt, kv, :], p_=ET[:, kt, :], st_=(kt == 0), sp_=(kt == nkt - 1):
                         S.mm(o_, v_, p_, start=st_, stop=sp_))
            pv_flush(self)
            t_ = tmpn[1]
            normalize(self, o_ps, t_[:, :], clamp=True)
            S.tt('pool', t_[:, :], t_[:, :], gbuf[:, 3 * hl + 0, :], ALU.mult)
            S.tt('pool', onsa[:, hq, :], onsa[:, hq, :], t_[:, :], ALU.add)
            for qi in range(4):
                ip = self.ps.bank('pj', [6, 7])
                for kt in range(nkt):
                    S.mm(ip[:, 0:NS + 1], ET[:, kt, 128 * qi:128 * qi + 128], self.ovl[:, kt, :], start=(kt == 0), stop=(kt == nkt - 1))
                rd = self.m8[:, 16 + qi:17 + qi]
                S.ts('dve', rd, ip[:, NS:NS + 1], 1e-30, None, ALU.max)
                S.op('dve', nc.vector.reciprocal, out=rd, in_=rd, reads=[rd], writes=[rd])
                if hl == 0:
                    S.ts('dve', imp[:, qi, kv, :], ip[:, 0:NS], rd, None, ALU.mult)
                else:
                    S.stt('dve', imp[:, qi, kv, :], ip[:, 0:NS], rd, imp[:, qi, kv, :], ALU.mult, ALU.add)
        for qi in range(4):
            ib = imp[:, qi, kv, :]
            S.tt('dve', ib, ib, self.ibias[:, qi, :], ALU.add)
            S.op('dve', nc.vector.max, out=self.m8[:, 0:8], in_=ib, reads=[ib], writes=[self.m8[:, 0:8]])
            S.op('dve', nc.vector.match_replace, out=self.selw[:, :], in_to_replace=self.m8[:, 0:8], in_values=ib,
                 imm_value=-3.0 * BIGB, reads=[self.m8[:, 0:8], ib], writes=[self.selw[:, :]])
            S.op('dve', nc.vector.max, out=self.m8[:, 8:16], in_=self.selw[:, :], reads=[self.selw[:, :]], writes=[self.m8[:, 8:16]])
            S.ts('dve', self.m8[:, 20:21], self.m8[:, 15:16], -0.5 * BIGB, None, ALU.max)
            S.ts('dve', self.selw[:, :], ib, self.m8[:, 20:21], None, ALU.is_ge)
            S.ts('dve', self.selw[:, :], self.selw[:, :], -1.0, -NEG, ALU.add, ALU.mult)
            for quarter in range(NS // 64):
                tp = self.ps.bank('pj', [6, 7])
                S.mm(tp[0:64, 0:128], self.selw[:, 64 * quarter:64 * quarter + 64], self.ident[:, :])
                evac(self, self.negselT[:, kv, quarter, 128 * qi:128 * qi + 128], tp[0:64, 0:128])
        obanks = [self.ps.t[:, (3 + hl) * 512:(4 + hl) * 512] for hl in range(3)]
        nslab = m + 1
        for i in range(nslab):
            ks, vs = kslab[i % 2], vslab[i % 2]
            S.dma('sp', ks[:, :], self.kslc_d[kv, :, 2048 * i:2048 * i + 2048])
            S.dma('sp', vs[:, :, 0, :], self.vslc_d[2048 * i:2048 * i + 2048, 64 * kv:64 * kv + 64].rearrange("(t p) c -> p t c", p=128))
            for hl in range(3):
                hq = 3 * kv + hl
                for t in range(16):
                    kt = 16 * i + t
                    row = (2 * kt) % 64
                    quarter = (2 * kt) // 64
                    q32, r = row // 32, (row % 32) // 2
                    s_ps = self.ps.bank('sc', [0, 1, 2])
                    S.mm(s_ps[:, :], ks[:, 128 * t:128 * t + 128], qbuf[0:64, hq, :], start=True, stop=False)
                    S.mm(s_ps[:, :], self.Eq[32 * q32:32 * q32 + 32, r, :], self.negselT[32 * q32:32 * q32 + 32, kv, quarter, :],
                         start=False, stop=True)
                    p = self.pbufs[self._pn % len(self.pbufs)]
                    self._pn += 1
                    S.act(p[:, :], s_ps[:, :], AF.Exp, scale=0.125)
                    if i == m:
                        S.stt('dve', p[:, :], self.dqk[:, 0, :], self.thr[:, t:t + 1], p[:, :], ALU.is_ge, ALU.mult)
                    pv_defer(self, lambda o_=obanks[hl], v_=vs[:, t, :, :].rearrange("p a b -> p (a b)"), p_=p[:, :], st_=(kt == 0), sp_=(kt == 16 * nslab - 1):
                             S.mm(o_, v_, p_, start=st_, stop=sp_))
        pv_flush(self)
        for hl in range(3):
            hq = 3 * kv + hl
            t_ = tmpn[hl % 2]
            normalize(self, obanks[hl], t_[:, :])
            S.tt('pool', t_[:, :], t_[:, :], gbuf[:, 3 * hl + 1, :], ALU.mult)
            S.tt('pool', self.oT[:, 6 + hq, :], onsa[:, hq, :], t_[:, :], ALU.add)
```
